# Optimizing a Trainium2 kernel written in Bass

```python
import math
import jax
import jax.numpy as jnp
from jax import lax
import numpy as np

D_MODEL = 1024
BATCH = 16
SEQ = 2048
DEPTH = 4

GRID_W = 64
CTX_LEN = 256
N_MIXERS = 4
Q_BLOCK = 128
RMS_EPS = 1e-6
ROPE_BASE = 10000.0

DA_HEADS = 8
DA_HEAD_DIM = 64
DA_V_DIM = 2 * DA_HEAD_DIM

GM_CHUNK = 128
GM_WIDTH = D_MODEL
GM_GROUPS = 8
GM_GROUP_DIM = GM_WIDTH // GM_GROUPS

SSM_INNER = 2 * D_MODEL
SSM_HEAD_DIM = 64
SSM_HEADS = SSM_INNER // SSM_HEAD_DIM
SSM_GROUPS = 4
SSM_HEADS_PER_GROUP = SSM_HEADS // SSM_GROUPS
SSM_STATE = 128
SSM_CONV = 5
SSM_CHUNK = 128
SSM_CONV_DIM = SSM_INNER + 2 * SSM_GROUPS * SSM_STATE

MLA_HEADS = 16
MLA_Q_RANK = 384
MLA_KV_RANK = 256
MLA_NOPE = 64
MLA_ROPE = 32
MLA_V = 64
MLA_QK = MLA_NOPE + MLA_ROPE

MOE_GROUPS = 4
MOE_PER_GROUP = 8
MOE_EXPERTS = MOE_GROUPS * MOE_PER_GROUP
MOE_TOP_K = 2
MOE_FF = 512

N_DA = (DEPTH + 3) // N_MIXERS
N_GM = (DEPTH + 2) // N_MIXERS
N_SSM = (DEPTH + 1) // N_MIXERS
N_MLA = DEPTH // N_MIXERS

kernel_name = 'hybrid_interleaved_diffusion_trunk'


def rms_norm(x, g):
    xf = x.astype(jnp.float32)
    y = xf * lax.rsqrt(jnp.mean(xf * xf, axis=-1, keepdims=True) + RMS_EPS)
    return (y * g.astype(jnp.float32)).astype(x.dtype)


def modulate(x, shift, scale):
    return x * (1 + scale) + shift


def axial_rope_tables(n_tokens, rot_dim):
    rows = n_tokens // GRID_W
    row = jnp.repeat(jnp.arange(rows, dtype=jnp.float32), GRID_W)
    col = jnp.tile(jnp.arange(GRID_W, dtype=jnp.float32), rows)
    n_freq = rot_dim // 4
    inv_freq = ROPE_BASE ** (-jnp.arange(n_freq, dtype=jnp.float32) / n_freq)
    ang = jnp.concatenate([row[:, None] * inv_freq, col[:, None] * inv_freq], axis=-1)
    return jnp.cos(ang), jnp.sin(ang)


def apply_rope(x, cos, sin):
    half = x.shape[-1] // 2
    x1, x2 = x[..., :half], x[..., half:]
    c = cos[None, :, None, :].astype(x.dtype)
    s = sin[None, :, None, :].astype(x.dtype)
    return jnp.concatenate([x1 * c - x2 * s, x1 * s + x2 * c], axis=-1)


def over_query_blocks(fn, q):
    b, t = q.shape[:2]
    nb = t // Q_BLOCK
    qb = jnp.moveaxis(q.reshape((b, nb, Q_BLOCK) + q.shape[2:]), 1, 0)
    out = lax.map(fn, qb)
    return jnp.moveaxis(out, 0, 1).reshape(b, t, out.shape[-1])


def softmax_attention(q, k, v, scale):
    b, tq = q.shape[:2]
    s = jnp.einsum('bqhd,bkhd->bhqk', q, k).astype(jnp.float32) * scale
    p = jax.nn.softmax(s, axis=-1).astype(v.dtype)
    return jnp.einsum('bhqk,bkhe->bqhe', p, v).reshape(b, tq, -1)


def diff_attention(xl, xc, w_in, w_out, q_g, k_g, lam_q1, lam_k1, lam_q2, lam_k2, sub_g, layer_idx, want_ctx):
    H, d = DA_HEADS, DA_HEAD_DIM
    f32 = jnp.float32

    def project(x):
        b, t, _ = x.shape
        q, k, v = jnp.split(x @ w_in, 3, axis=-1)
        q = rms_norm(q.reshape(b, t, 2 * H, d), q_g)
        k = rms_norm(k.reshape(b, t, 2 * H, d), k_g)
        return q, k, v.reshape(b, t, H, DA_V_DIM)

    ql, kl, vl = project(xl)
    qc, kc, vc = project(xc)
    cos, sin = axial_rope_tables(xl.shape[1], d)
    ql, kl = apply_rope(ql, cos, sin), apply_rope(kl, cos, sin)

    lam_init = 0.8 - 0.6 * math.exp(-0.3 * layer_idx)
    lam = (jnp.exp(jnp.sum(lam_q1.astype(f32) * lam_k1.astype(f32)))
           - jnp.exp(jnp.sum(lam_q2.astype(f32) * lam_k2.astype(f32))) + lam_init)
    scale = d ** -0.5

    def attend(q, k, v):
        b, tq = q.shape[:2]
        tk = k.shape[1]
        q = q.reshape(b, tq, H, 2, d)
        k = k.reshape(b, tk, H, 2, d)
        s = jnp.einsum('bqhmd,bkhmd->bhmqk', q, k).astype(f32) * scale
        p = jax.nn.softmax(s, axis=-1)
        a = (p[:, :, 0] - lam * p[:, :, 1]).astype(v.dtype)
        o = jnp.einsum('bhqk,bkhe->bqhe', a, v)
        o = rms_norm(o, sub_g) * (1.0 - lam_init)
        return o.reshape(b, tq, H * DA_V_DIM)

    k_all = jnp.concatenate([kc, kl], axis=1)
    v_all = jnp.concatenate([vc, vl], axis=1)
    out_l = over_query_blocks(lambda qb: attend(qb, k_all, v_all), ql) @ w_out
    out_c = attend(qc, kc, vc) @ w_out if want_ctx else None
    return out_l, out_c


def chunk_gmlp(xl, xc, w_in, v_g, w_s, b_s, w_out, want_ctx):
    def mix(x):
        b, t, _ = x.shape
        u, v = jnp.split(jax.nn.gelu(x @ w_in, approximate=False), 2, axis=-1)
        v = rms_norm(v, v_g).reshape(b, t // GM_CHUNK, GM_CHUNK, GM_GROUPS, GM_GROUP_DIM)
        s = jnp.einsum('gpq,bcqgd->bcpgd', w_s, v) + b_s.T[None, None, :, :, None]
        return (u * s.reshape(b, t, GM_WIDTH)) @ w_out
    return mix(xl), (mix(xc) if want_ctx else None)


def depthwise_conv_centred(x, w, bias):
    pad = (SSM_CONV - 1) // 2
    y = lax.conv_general_dilated(x, w[:, None, :].astype(x.dtype), window_strides=(1,),
                                 padding=[(pad, pad)], dimension_numbers=('NWC', 'WIO', 'NWC'),
                                 feature_group_count=x.shape[-1])
    return y + bias


def segsum(a):
    t = a.shape[-1]
    cs = jnp.cumsum(a, axis=-1)
    diff = cs[..., :, None] - cs[..., None, :]
    mask = jnp.arange(t)[:, None] >= jnp.arange(t)[None, :]
    return jnp.where(mask, diff, -jnp.inf)


def ssd_chunked(x, a, bm, cm, h0):
    b, t = x.shape[:2]
    nc = t // SSM_CHUNK
    x = x.reshape((b, nc, SSM_CHUNK) + x.shape[2:])
    bm = bm.reshape((b, nc, SSM_CHUNK) + bm.shape[2:])
    cm = cm.reshape((b, nc, SSM_CHUNK) + cm.shape[2:])
    a = jnp.moveaxis(a.reshape((b, nc, SSM_CHUNK) + a.shape[2:]), 2, -1)
    a_cum = jnp.cumsum(a, axis=-1)
    decay_in = jnp.exp(segsum(a))
    cb = jnp.einsum('bclgn,bcsgn->bcgls', cm, bm)
    y_diag = jnp.einsum('bcgels,bcsgep->bclgep', cb[:, :, :, None] * decay_in, x)
    decay_to_end = jnp.exp(a_cum[..., -1:] - a_cum)
    xd = x * jnp.moveaxis(decay_to_end, -1, 2)[..., None]
    states = jnp.einsum('bclgn,bclgep->bcgepn', bm, xd)
    states = jnp.concatenate([h0[:, None].astype(states.dtype), states], axis=1)
    chunk_a = jnp.pad(jnp.moveaxis(a_cum[..., -1], 1, -1), [(0, 0), (0, 0), (0, 0), (1, 0)])
    decay_chunk = jnp.exp(segsum(chunk_a))
    states = jnp.einsum('bgezc,bcgepn->bzgepn', decay_chunk, states)
    y_off = (jnp.einsum('bclgn,bcgepn->bclgep', cm, states[:, :-1])
             * jnp.moveaxis(jnp.exp(a_cum), -1, 2)[..., None])
    y = (y_diag + y_off).reshape((b, t) + y_diag.shape[3:])
    return y, states[:, -1]


def mamba2_bidir(xl, xc, w_in, conv_w, conv_b, dt_bias, a_log, d_skip, out_g, w_out, want_ctx):
    G, E, P, N = SSM_GROUPS, SSM_HEADS_PER_GROUP, SSM_HEAD_DIM, SSM_STATE
    f32 = jnp.float32
    A = -jnp.exp(a_log.astype(f32)).reshape(2, G, E)

    def prepare(x):
        b, t, _ = x.shape
        z, xbc, dt = jnp.split(x @ w_in, [SSM_INNER, SSM_INNER + SSM_CONV_DIM], axis=-1)
        xbc = jax.nn.silu(depthwise_conv_centred(xbc, conv_w, conv_b))
        xs, bm, cm = jnp.split(xbc, [SSM_INNER, SSM_INNER + G * N], axis=-1)
        dt = jax.nn.softplus(dt.astype(f32).reshape(b, t, 2, SSM_HEADS) + dt_bias.astype(f32))
        return (z, xs.reshape(b, t, G, E, P), bm.reshape(b, t, G, N), cm.reshape(b, t, G, N),
                dt.reshape(b, t, 2, G, E))

    def run(xs, bm, cm, dt, direction, h0):
        dtd = dt[:, :, direction]
        a = dtd * A[direction]
        xdt = xs * dtd[..., None]
        if direction == 1:
            xdt, a, bm, cm = (jnp.flip(v, axis=1) for v in (xdt, a, bm, cm))
        y, h_last = ssd_chunked(xdt, a, bm, cm, h0)
        if direction == 1:
            y = jnp.flip(y, axis=1)
        return y, h_last

    def finish(y_f, y_b, xs, z):
        b, t = xs.shape[:2]
        y = y_f + y_b + d_skip.reshape(G, E)[:, :, None] * xs
        y = rms_norm(y.reshape(b, t, SSM_INNER) * jax.nn.silu(z), out_g)
        return y @ w_out

    zc, xsc, bc, cc, dtc = prepare(xc)
    zl, xsl, bl, cl, dtl = prepare(xl)
    h0 = jnp.zeros((xc.shape[0], G, E, P, N), f32)
    yc_f, hc_f = run(xsc, bc, cc, dtc, 0, h0)
    yc_b, hc_b = run(xsc, bc, cc, dtc, 1, h0)
    yl_f, _ = run(xsl, bl, cl, dtl, 0, hc_f)
    yl_b, _ = run(xsl, bl, cl, dtl, 1, hc_b)
    out_l = finish(yl_f, yl_b, xsl, zl)
    out_c = finish(yc_f, yc_b, xsc, zc) if want_ctx else None
    return out_l, out_c


def mla_attention(xl, xc, w_in, q_norm_g, kv_norm_g, w_uq, w_ukv, q_g, k_g, w_out, want_ctx):
    H = MLA_HEADS

    def project(x):
        b, t, _ = x.shape
        cq, ckv, k_pe = jnp.split(x @ w_in, [MLA_Q_RANK, MLA_Q_RANK + MLA_KV_RANK], axis=-1)
        q = (rms_norm(cq, q_norm_g) @ w_uq).reshape(b, t, H, MLA_QK)
        kv = (rms_norm(ckv, kv_norm_g) @ w_ukv).reshape(b, t, H, MLA_NOPE + MLA_V)
        k_nope, v = jnp.split(kv, [MLA_NOPE], axis=-1)
        k = jnp.concatenate([k_nope, jnp.broadcast_to(k_pe[:, :, None, :], (b, t, H, MLA_ROPE))], axis=-1)
        return rms_norm(q, q_g), rms_norm(k, k_g), v

    ql, kl, vl = project(xl)
    qc, kc, vc = project(xc)
    cos, sin = axial_rope_tables(xl.shape[1], MLA_ROPE)

    def rope_tail(z):
        return jnp.concatenate([z[..., :MLA_NOPE], apply_rope(z[..., MLA_NOPE:], cos, sin)], axis=-1)

    ql, kl = rope_tail(ql), rope_tail(kl)
    scale = MLA_QK ** -0.5
    k_all = jnp.concatenate([kc, kl], axis=1)
    v_all = jnp.concatenate([vc, vl], axis=1)
    out_l = over_query_blocks(lambda qb: softmax_attention(qb, k_all, v_all, scale), ql) @ w_out
    out_c = softmax_attention(qc, kc, vc, scale) @ w_out if want_ctx else None
    return out_l, out_c


def hier_moe(x, w_group, b_group, w_router, b_router, w1, w3, w2):
    f32 = jnp.float32
    n_tok = x.shape[0]
    g_logits = (x @ w_group).astype(f32) + b_group
    _, g_idx = lax.top_k(g_logits, 1)
    p_group = jnp.take_along_axis(jax.nn.softmax(g_logits, axis=-1), g_idx, axis=-1)
    e_logits = ((x @ w_router).astype(f32) + b_router).reshape(n_tok, MOE_GROUPS, MOE_PER_GROUP)
    sel = jnp.broadcast_to(g_idx[:, :, None], (n_tok, 1, MOE_PER_GROUP))
    e_logits = jnp.take_along_axis(e_logits, sel, axis=1)[:, 0]
    e_top, e_idx = lax.top_k(e_logits, MOE_TOP_K)
    weights = jax.nn.softmax(e_top, axis=-1) * p_group
    expert = g_idx * MOE_PER_GROUP + e_idx
    gates = jnp.sum(jax.nn.one_hot(expert, MOE_EXPERTS, dtype=f32) * weights[..., None], axis=1)
    y = jnp.zeros_like(x)
    for e in range(MOE_EXPERTS):
        hid = jax.nn.silu(x @ w1[e]) * (x @ w3[e])
        y = y + gates[:, e:e + 1].astype(x.dtype) * (hid @ w2[e])
    return y


def setup_inputs(seed: int = 0) -> dict:
    key = jax.random.key(seed)
    ks = iter(jax.random.split(key, 64))
    f32 = jnp.float32
    D = D_MODEL

    def nrm(shape, scale):
        return scale * jax.random.normal(next(ks), shape, f32)

    def gain(shape):
        return 1.0 + 0.05 * jax.random.normal(next(ks), shape, f32)

    u = jax.random.uniform(next(ks), (N_SSM, 2, SSM_HEADS), f32)
    dt0 = jnp.exp(u * (math.log(0.1) - math.log(0.001)) + math.log(0.001))
    ssm_dt_bias = dt0 + jnp.log(-jnp.expm1(-dt0))
    ssm_a_log = jnp.log(jax.random.uniform(next(ks), (N_SSM, 2, SSM_HEADS), f32, 1.0, 16.0))

    return {
        'x': nrm((BATCH, SEQ, D), 1.0),
        'c': nrm((BATCH, D), 1.0),
        'ctx': nrm((BATCH, CTX_LEN, D), 1.0),
        'c_ctx': nrm((D,), 1.0),
        'ada_w': nrm((DEPTH, D, 6 * D), 0.5 * D ** -0.5),
        'ada_b': nrm((DEPTH, 6 * D), 0.01),
        'norm1_g': gain((DEPTH, D)),
        'norm2_g': gain((DEPTH, D)),
        'da_w_in': nrm((N_DA, D, 3 * 2 * DA_HEADS * DA_HEAD_DIM), D ** -0.5),
        'da_w_out': nrm((N_DA, DA_HEADS * DA_V_DIM, D), (DA_HEADS * DA_V_DIM) ** -0.5),
        'da_q_g': gain((N_DA, DA_HEAD_DIM)),
        'da_k_g': gain((N_DA, DA_HEAD_DIM)),
        'da_lam_q1': nrm((N_DA, DA_HEAD_DIM), 0.1),
        'da_lam_k1': nrm((N_DA, DA_HEAD_DIM), 0.1),
        'da_lam_q2': nrm((N_DA, DA_HEAD_DIM), 0.1),
        'da_lam_k2': nrm((N_DA, DA_HEAD_DIM), 0.1),
        'da_sub_g': gain((N_DA, DA_V_DIM)),
        'gm_w_in': nrm((N_GM, D, 2 * GM_WIDTH), D ** -0.5),
        'gm_v_g': gain((N_GM, GM_WIDTH)),
        'gm_w_s': nrm((N_GM, GM_GROUPS, GM_CHUNK, GM_CHUNK), GM_CHUNK ** -0.5),
        'gm_b_s': 1.0 + nrm((N_GM, GM_GROUPS, GM_CHUNK), 0.1),
        'gm_w_out': nrm((N_GM, GM_WIDTH, D), GM_WIDTH ** -0.5),
        'ssm_w_in': nrm((N_SSM, D, SSM_INNER + SSM_CONV_DIM + 2 * SSM_HEADS), D ** -0.5),
        'ssm_conv_w': nrm((N_SSM, SSM_CONV, SSM_CONV_DIM), SSM_CONV ** -0.5),
        'ssm_conv_b': nrm((N_SSM, SSM_CONV_DIM), 0.01),
        'ssm_dt_bias': ssm_dt_bias,
        'ssm_a_log': ssm_a_log,
        'ssm_d': 1.0 + nrm((N_SSM, SSM_HEADS), 0.1),
        'ssm_out_g': gain((N_SSM, SSM_INNER)),
        'ssm_w_out': nrm((N_SSM, SSM_INNER, D), SSM_INNER ** -0.5),
        'mla_w_in': nrm((N_MLA, D, MLA_Q_RANK + MLA_KV_RANK + MLA_ROPE), D ** -0.5),
        'mla_q_norm_g': gain((N_MLA, MLA_Q_RANK)),
        'mla_kv_norm_g': gain((N_MLA, MLA_KV_RANK)),
        'mla_w_uq': nrm((N_MLA, MLA_Q_RANK, MLA_HEADS * MLA_QK), MLA_Q_RANK ** -0.5),
        'mla_w_ukv': nrm((N_MLA, MLA_KV_RANK, MLA_HEADS * (MLA_NOPE + MLA_V)), MLA_KV_RANK ** -0.5),
        'mla_q_g': gain((N_MLA, MLA_QK)),
        'mla_k_g': gain((N_MLA, MLA_QK)),
        'mla_w_out': nrm((N_MLA, MLA_HEADS * MLA_V, D), (MLA_HEADS * MLA_V) ** -0.5),
        'moe_w_group': nrm((DEPTH, D, MOE_GROUPS), D ** -0.5),
        'moe_b_group': nrm((DEPTH, MOE_GROUPS), 0.01),
        'moe_w_router': nrm((DEPTH, D, MOE_EXPERTS), D ** -0.5),
        'moe_b_router': nrm((DEPTH, MOE_EXPERTS), 0.01),
        'moe_w1': nrm((DEPTH, MOE_EXPERTS, D, MOE_FF), D ** -0.5),
        'moe_w3': nrm((DEPTH, MOE_EXPERTS, D, MOE_FF), D ** -0.5),
        'moe_w2': nrm((DEPTH, MOE_EXPERTS, MOE_FF, D), MOE_FF ** -0.5),
    }


def reference(x, c, ctx, c_ctx, ada_w, ada_b, norm1_g, norm2_g,
              da_w_in, da_w_out, da_q_g, da_k_g, da_lam_q1, da_lam_k1, da_lam_q2, da_lam_k2, da_sub_g,
              gm_w_in, gm_v_g, gm_w_s, gm_b_s, gm_w_out,
              ssm_w_in, ssm_conv_w, ssm_conv_b, ssm_dt_bias, ssm_a_log, ssm_d, ssm_out_g, ssm_w_out,
              mla_w_in, mla_q_norm_g, mla_kv_norm_g, mla_w_uq, mla_w_ukv, mla_q_g, mla_k_g, mla_w_out,
              moe_w_group, moe_b_group, moe_w_router, moe_b_router, moe_w1, moe_w3, moe_w2):
    h, hc = x, ctx
    b, s, dm = x.shape
    for i in range(DEPTH):
        kind, j = i % N_MIXERS, i // N_MIXERS
        want_ctx = i < DEPTH - 1
        mod_l = jax.nn.silu(c) @ ada_w[i] + ada_b[i]
        mod_c = jax.nn.silu(c_ctx) @ ada_w[i] + ada_b[i]
        sh1, sc1, g1, sh2, sc2, g2 = jnp.split(mod_l[:, None, :], 6, axis=-1)
        csh1, csc1, cg1, csh2, csc2, cg2 = jnp.split(mod_c, 6, axis=-1)

        xl = modulate(rms_norm(h, norm1_g[i]), sh1, sc1)
        xc = modulate(rms_norm(hc, norm1_g[i]), csh1, csc1)
        if kind == 0:
            dl, dc = diff_attention(xl, xc, da_w_in[j], da_w_out[j], da_q_g[j], da_k_g[j], da_lam_q1[j],
                                    da_lam_k1[j], da_lam_q2[j], da_lam_k2[j], da_sub_g[j], i, want_ctx)
        elif kind == 1:
            dl, dc = chunk_gmlp(xl, xc, gm_w_in[j], gm_v_g[j], gm_w_s[j], gm_b_s[j], gm_w_out[j], want_ctx)
        elif kind == 2:
            dl, dc = mamba2_bidir(xl, xc, ssm_w_in[j], ssm_conv_w[j], ssm_conv_b[j], ssm_dt_bias[j],
                                  ssm_a_log[j], ssm_d[j], ssm_out_g[j], ssm_w_out[j], want_ctx)
        else:
            dl, dc = mla_attention(xl, xc, mla_w_in[j], mla_q_norm_g[j], mla_kv_norm_g[j], mla_w_uq[j],
                                   mla_w_ukv[j], mla_q_g[j], mla_k_g[j], mla_w_out[j], want_ctx)
        h = h + g1 * dl
        if want_ctx:
            hc = hc + cg1 * dc

        xl = modulate(rms_norm(h, norm2_g[i]), sh2, sc2).reshape(-1, dm)
        moe_args = (moe_w_group[i], moe_b_group[i], moe_w_router[i], moe_b_router[i],
                    moe_w1[i], moe_w3[i], moe_w2[i])
        if want_ctx:
            xc = modulate(rms_norm(hc, norm2_g[i]), csh2, csc2).reshape(-1, dm)
            y = hier_moe(jnp.concatenate([xl, xc], axis=0), *moe_args)
            h = h + g2 * y[:b * s].reshape(b, s, dm)
            hc = hc + cg2 * y[b * s:].reshape(hc.shape)
        else:
            h = h + g2 * hier_moe(xl, *moe_args).reshape(b, s, dm)
    return h
```

```python
import os
import numpy as np
import concourse.bass as bass
import concourse.mybir as mybir

F32 = mybir.dt.float32
BF16 = mybir.dt.bfloat16
AF = mybir.ActivationFunctionType
ALU = mybir.AluOpType
AX = mybir.AxisListType

ENGS = ("tensor", "vector", "scalar", "gpsimd", "sync")
N_DMA_SEMS = 8


class _Op:
    __slots__ = ("eng", "fn", "deps", "chan", "is_dma", "needs_inc", "val", "idx")


class FW:
    def __init__(self, nc, same_engine_sync=True):
        self.nc = nc
        self.ops = []
        self.state = {}
        self.same_engine_sync = same_engine_sync
        self.dma_rr = {e: 0 for e in ENGS}
        self.chan_last = {}
        self.bar = {}

    def barrier(self):
        self.bar = dict(self.chan_last)

    def _entries(self, res):
        name, idx = res if isinstance(res, tuple) else (res, None)
        name = getattr(name, "name", name)
        ent = self.state.setdefault(name, {})
        return name, idx, ent

    def _collect(self, res, is_write, deps):
        name, idx, ent = self._entries(res)
        if idx is None:
            targets = list(ent.values())
        else:
            targets = [ent.get(idx), ent.get(None)]
        for e in targets:
            if e is None:
                continue
            if e[0] is not None:
                deps.add(e[0])
            if is_write:
                deps.update(e[1].values())

    def _update(self, res, is_write, op):
        name, idx, ent = self._entries(res)
        if is_write:
            if idx is None:
                ent.clear()
                ent[None] = [op.idx, {}]
            else:
                ent[idx] = [op.idx, {}]
        else:
            e = ent.get(idx)
            if e is None:
                e = ent[idx] = [None, {}]
            e[1][op.chan] = op.idx

    def op(self, eng, fn, r=(), w=(), dma=False):
        o = _Op()
        o.idx = len(self.ops)
        o.eng = eng
        o.fn = fn
        o.is_dma = dma
        if dma:
            k = self.dma_rr[eng]
            self.dma_rr[eng] = (k + 1) % N_DMA_SEMS
            o.chan = "dma_%s_%d" % (eng, k)
        else:
            o.chan = eng
        o.needs_inc = dma
        o.val = None
        deps = set()
        for res in r:
            self._collect(res, False, deps)
        for res in w:
            self._collect(res, True, deps)
        best = {}
        for d in deps:
            c = self.ops[d].chan
            if c not in best or best[c] < d:
                best[c] = d
        for c, d in self.bar.items():
            if c not in best or best[c] < d:
                best[c] = d
        if dma and o.chan in self.chan_last:
            d = self.chan_last[o.chan]
            if o.chan not in best or best[o.chan] < d:
                best[o.chan] = d
        o.deps = best
        self.chan_last[o.chan] = o.idx
        for res in r:
            self._update(res, False, o)
        for res in w:
            self._update(res, True, o)
        self.ops.append(o)
        return o

    def pe(self, fn, r=(), w=()):
        return self.op("tensor", fn, r, w)

    def dve(self, fn, r=(), w=()):
        return self.op("vector", fn, r, w)

    def act(self, fn, r=(), w=()):
        return self.op("scalar", fn, r, w)

    def pool(self, fn, r=(), w=()):
        return self.op("gpsimd", fn, r, w)

    def dma(self, eng, out, in_, r=(), w=(), **kw):
        return self.op(eng, lambda e: e.dma_start(out=out, in_=in_, **kw), r, w, dma=True)

    def _init_emit(self):
        import contextlib
        self._st = contextlib.ExitStack()
        chans = list(ENGS[:4]) + ["dma_%s_%d" % (e, k) for e in ("sync", "scalar", "gpsimd") for k in range(N_DMA_SEMS)]
        self.sems = {c: self._st.enter_context(self.nc.semaphore("s_" + c)) for c in chans}
        self.cnt = {c: 0 for c in chans}
        self.inc_idx = {c: [] for c in chans}
        self.inc_val = {c: [] for c in chans}
        self.waited = {e: {} for e in ENGS}
        self.flushed = 0

    def _skip_same(self, o, p):
        return p.chan == o.eng and not p.is_dma and (o.eng == "tensor" or not self.same_engine_sync)

    def flush(self):
        import bisect
        if not hasattr(self, "sems"):
            self._init_emit()
        ops = self.ops
        batch = ops[self.flushed:]
        if not batch:
            return
        start = self.flushed
        last_in_chan = {}
        for o in batch:
            last_in_chan[o.chan] = o
            for c, d in o.deps.items():
                p = ops[d]
                if d >= start and not self._skip_same(o, p):
                    p.needs_inc = True
        for o in last_in_chan.values():
            o.needs_inc = True
        for o in batch:
            if o.needs_inc:
                self.cnt[o.chan] += 16 if o.is_dma else 1
                o.val = self.cnt[o.chan]
                self.inc_idx[o.chan].append(o.idx)
                self.inc_val[o.chan].append(o.val)
        for o in batch:
            eng = getattr(self.nc, o.eng)
            waited = self.waited[o.eng]
            for c, d in o.deps.items():
                p = ops[d]
                if self._skip_same(o, p):
                    continue
                k = bisect.bisect_left(self.inc_idx[p.chan], d)
                val = self.inc_val[p.chan][k]
                if waited.get(p.chan, 0) >= val:
                    continue
                eng.wait_ge(self.sems[p.chan], val)
                waited[p.chan] = val
            ins = o.fn(eng)
            if o.needs_inc:
                ins.then_inc(self.sems[o.chan], 16 if o.is_dma else 1)
            o.fn = None
        self.flushed = len(ops)

    def emit(self, final_wait_ops=()):
        self.flush()
        eng = self.nc.sync
        for p in final_wait_ops:
            eng.wait_ge(self.sems[p.chan], p.val)
        self.sem_max = dict(self.cnt)
        self._st.close()


import contextlib
import math
import numpy as np
import concourse.bass as bass
import concourse.mybir as mybir

D = 1024
KC = 8
EPS = 1e-6


class T:
    def __init__(self, h, name):
        self.h = h
        self.name = name

    def __getitem__(self, k):
        return self.h[k]


class Cfg:
    def __init__(self, **kw):
        self.S_LAT = 2048
        self.S_CTX = 256
        self.NB = 2
        self.GROUPS = 4
        self.PER = 8
        self.kinds = [0, 1, 2, 3]
        self.want_ctx = [True, True, True, False]
        self.layer_ids = [0, 1, 2, 3]
        self.moe = True
        self.__dict__.update(kw)
        self.NTC = self.S_CTX // 128
        self.NTL = self.S_LAT // 128
        self.NT = self.NTC + self.NTL
        self.E = self.GROUPS * self.PER
        self.DEPTH = len(self.kinds)


class B:
    def __init__(self, nc, cfg):
        self.nc = nc
        self.cfg = cfg
        self.fw = FW(nc)
        self.uid = 0
        self.stacks = []
        self.dram = {}
        self.ps_rr = 0

    @contextlib.contextmanager
    def scope(self):
        st = contextlib.ExitStack()
        self.stacks.append(st)
        try:
            with st:
                yield
                self.fw.flush()
        finally:
            self.fw.flush()
            self.stacks.pop()
            self.fw.barrier()

    def sb(self, name, shape, dt):
        self.uid += 1
        nm = "%s_%d" % (name, self.uid)
        h = self.stacks[-1].enter_context(self.nc.sbuf_tensor(nm, list(shape), dt))
        return T(h, nm)

    def din(self, name, shape, dt=F32):
        ap = self.nc.dram_tensor(name, list(shape), dt, kind="ExternalInput").ap()
        self.dram[name] = ap
        return ap

    def psum(self):
        p = self.ps[self.ps_rr]
        self.ps_rr = (self.ps_rr + 1) % len(self.ps)
        return p

    def psumb(self):
        p = self.psb[self.psb_rr]
        self.psb_rr = (self.psb_rr + 1) % len(self.psb)
        return p

    def bcast_rows(self, dst, dst_cols, row_ap_f32, n, evac=None):
        fw = self.fw
        for c0 in range(0, n, 512):
            cw = min(512, n - c0)
            p = self.psum()
            fw.pe(lambda e, p=p, c0=c0, cw=cw: e.matmul(p[:, 0:cw], lhsT=self.ones_row[0:1, 0:128],
                                                      rhs=row_ap_f32[1][0:1, c0:c0 + cw], start=True, stop=True),
                  r=[self.ones_row, row_ap_f32[0]], w=[p])
            d0 = dst_cols + c0
            if evac is None:
                fw.act(lambda e, p=p, d0=d0, cw=cw: e.copy(out=dst[:, d0:d0 + cw], in_=p[:, 0:cw]), r=[p], w=[dst])
            else:
                evac(p, d0, cw)


def rstd_op(b, out, in_, scale, r, w):
    fw = b.fw
    fw.act(lambda e: e.activation(out=out, in_=in_, func=AF.Sqrt, bias=b.eps_col[0:out.shape[0], 0:1], scale=scale),
           r=list(r) + [b.eps_col], w=w)
    fw.dve(lambda e: e.reciprocal(out=out, in_=out), r=w, w=w)


def build_consts(b):
    nc, fw = b.nc, b.fw
    cfg = b.cfg
    ident_d = b.din("c_ident", [128, 128])
    tri_d = b.din("c_tri", [4, 128, 128])
    b.din("c_rope64", [cfg.S_CTX + cfg.S_LAT, 64])
    b.din("c_rope32", [cfg.S_CTX + cfg.S_LAT, 32])
    b.identb = b.sb("identb", [128, 128], BF16)
    b.identf = b.sb("identf", [128, 128], F32)
    b.ones_row = b.sb("ones_row", [1, 512], F32)
    b.ones_f = b.sb("ones_f", [128, 128], F32)
    b.tri = b.sb("tri", [128, 4, 128], F32)
    fw.dma("gpsimd", b.identb[:], ident_d, w=[b.identb])
    fw.dma("sync", b.identf[:], ident_d, w=[b.identf])
    fw.dma("sync", b.tri[:], tri_d.rearrange("a p q -> p a q"), w=[b.tri])
    fw.pool(lambda e: e.memset(b.ones_row[:], 1.0), w=[b.ones_row])
    fw.pool(lambda e: e.memset(b.ones_f[:], 1.0), w=[b.ones_f])
    b.eps_col = b.sb("eps_col", [128, 1], F32)
    fw.pool(lambda e: e.memset(b.eps_col[:], EPS), w=[b.eps_col])
    st = b.stacks[-1]
    b.ps = []
    for j in range(6):
        h = st.enter_context(nc.psum_tensor("psf%d" % j, [128, 512], F32))
        b.ps.append(T(h, "psf%d" % j))
    b.psb = []
    for j in range(2):
        h = st.enter_context(nc.psum_tensor("psb%d" % j, [128, 1024], BF16))
        b.psb.append(T(h, "psb%d" % j))
    b.psb_rr = 0


def load_cond(b, bi):
    with b.scope():
        _load_cond(b, bi)


def _load_cond(b, bi):
    fw = b.fw
    c_d, cctx_d = b.dram["c"], b.dram["c_ctx"]
    crow = b.sb("crow", [1, 2 * D], F32)
    fw.dma("sync", crow[0:1, 0:D], c_d[bi:bi + 1, :], w=[(crow, 0)])
    fw.dma("sync", crow[0:1, D:2 * D], cctx_d[0:1, :], w=[(crow, 1)])
    p = b.psum()
    for k in range(2):
        for kc in range(KC):
            fw.pe(lambda e, k=k, kc=kc: e.matmul(p[:, k * KC + kc:k * KC + kc + 1],
                                                lhsT=crow[0:1, k * D + kc * 128:k * D + (kc + 1) * 128],
                                                rhs=b.ones_row[0:1, 0:1], start=True, stop=True),
                  r=[crow, b.ones_row], w=[p])
    ccol = b.sb("ccol", [128, 2 * KC], F32)
    fw.act(lambda e: e.activation(out=ccol[:], in_=p[:, 0:2 * KC], func=AF.Silu), r=[p], w=[ccol])
    for k in range(2):
        fw.dve(lambda e, k=k: e.tensor_copy(out=b.scB[k][:],
                                           in_=ccol[:, k * KC:(k + 1) * KC].unsqueeze(2).to_broadcast([128, KC, 128])),
               r=[ccol], w=[b.scB[k]])


def mod_bcast(b, li, sec, dsts, add_one=False):
    fw = b.fw
    ada_w, ada_b = b.dram["ada_w"], b.dram["ada_b"]
    with b.scope():
        _mod_bcast(b, li, sec, dsts, add_one)


def _mod_bcast(b, li, sec, dsts, add_one):
    fw = b.fw
    ada_w, ada_b = b.dram["ada_w"], b.dram["ada_b"]
    brow = b.sb("brow", [1, D], F32)
    QW = 256
    wsts = [b.sb("wst%d" % j, [128, KC, QW], F32) for j in range(2)]
    fw.dma("sync", brow[:], ada_b[li:li + 1, sec * D:(sec + 1) * D], w=[brow])
    for q in range(D // QW):
        wst = wsts[q % 2]
        c0 = sec * D + q * QW
        fw.dma("sync", wst[:], ada_w[li, :, c0:c0 + QW].rearrange("(kc p) f -> p kc f", p=128), w=[wst])
        for k in range(2):
            if dsts[k] is None:
                continue
            p = b.psum()
            for kc in range(KC):
                fw.pe(lambda e, p=p, k=k, kc=kc, wst=wst: e.matmul(p[:, 0:QW], lhsT=b.scB[k][:, kc, :], rhs=wst[:, kc, :],
                                                                  start=(kc == 0), stop=False),
                      r=[b.scB[k], wst], w=[p])
            fw.pe(lambda e, p=p, q=q: e.matmul(p[:, 0:QW], lhsT=b.ones_row[0:1, 0:128], rhs=brow[0:1, q * QW:(q + 1) * QW],
                                              start=False, stop=True),
                  r=[b.ones_row, brow], w=[p])
            d = dsts[k]
            if add_one:
                fw.act(lambda e, p=p, d=d, q=q: e.activation(out=d[:, q * QW:(q + 1) * QW], in_=p[:, 0:QW],
                                                            func=AF.Identity, bias=b.one_col[:, 0:1], scale=1.0),
                       r=[p, b.one_col], w=[(d, q)])
            else:
                fw.act(lambda e, p=p, d=d, q=q: e.copy(out=d[:, q * QW:(q + 1) * QW], in_=p[:, 0:QW]),
                       r=[p], w=[(d, q)])


def norm_mod(b, li, which, tiles):
    fw, cfg = b.fw, b.cfg
    gname = "norm1_g" if which == 1 else "norm2_g"
    with b.scope():
        A = [b.sb("A%d" % k, [128, D], F32) for k in range(2)]
        S = [b.sb("S%d" % k, [128, D], F32) for k in range(2)]
        G = b.sb("Gn", [128, D], F32)
        grow = b.sb("grow", [1, D], F32)
        need_ctx = any(t < cfg.NTC for t in tiles)
        sec0 = 0 if which == 1 else 3
        fw.dma("sync", grow[:], b.dram[gname][li:li + 1, :], w=[grow])
        b.bcast_rows(G, 0, (grow, grow), D)
        dA = [A[0], A[1] if need_ctx else None]
        dS = [S[0], S[1] if need_ctx else None]
        mod_bcast(b, li, sec0 + 1, dA, add_one=True)
        mod_bcast(b, li, sec0 + 0, dS)
        for k in range(2):
            if dA[k] is not None:
                fw.dve(lambda e, k=k: e.tensor_mul(out=A[k][:], in0=A[k][:], in1=G[:]), r=[A[k], G], w=[A[k]])
        junk = b.sb("junk", [128, D], BF16)
        ss = b.sb("ss", [128, cfg.NT], F32)
        rstd = b.sb("rstd", [128, cfg.NT], F32)
        fw.pool(lambda e: e.memset(ss[:], 0.0), w=[ss])
        tmp = [b.sb("nt%d" % j, [128, D], F32) for j in range(2)]
        xnb = [b.sb("xnb%d" % j, [128, D], BF16) for j in range(2)]
        for j, t in enumerate(tiles):
            k = 1 if t < cfg.NTC else 0
            fw.act(lambda e, t=t: e.activation(out=junk[:], in_=b.h[:, t, :], func=AF.Square, accum_out=ss[:, t:t + 1]),
                   r=[(b.h, t)], w=[junk, (ss, t)])
            rstd_op(b, rstd[:, t:t + 1], ss[:, t:t + 1], 1.0 / D, [(ss, t)], [(rstd, t)])
            tm, xb = tmp[j % 2], xnb[j % 2]
            fw.dve(lambda e, t=t, tm=tm, k=k: e.scalar_tensor_tensor(out=tm[:], in0=b.h[:, t, :], scalar=rstd[:, t:t + 1],
                                                                    in1=A[k][:], op0=ALU.mult, op1=ALU.mult),
                   r=[(b.h, t), (rstd, t), A[k]], w=[tm])
            fw.pool(lambda e, tm=tm, xb=xb, k=k: e.tensor_tensor(out=xb[:], in0=tm[:], in1=S[k][:], op=ALU.add),
                    r=[tm, S[k]], w=[xb])
            pb = b.psumb()
            for kc in range(KC):
                fw.pe(lambda e, pb=pb, kc=kc, xb=xb: e.transpose(out=pb[:, kc * 128:(kc + 1) * 128],
                                                                in_=xb[:, kc * 128:(kc + 1) * 128], identity=b.identb[:]),
                      r=[xb, b.identb], w=[pb])
            fw.act(lambda e, pb=pb, t=t: e.copy(out=b.xnT[:, :, t * 128:(t + 1) * 128],
                                               in_=pb[:, :].rearrange("p (k q) -> p k q", k=KC)),
                   r=[pb], w=[(b.xnT, t)])


def gate_tiles(b, li, sec, need_ctx):
    Gt = [b.sb("Gt0", [128, D], F32), b.sb("Gt1", [128, D], F32) if need_ctx else None]
    mod_bcast(b, li, sec, Gt)
    return Gt


def resid_add(b, t, p, hf, Gk):
    fw = b.fw
    tm = b.rtmp[b.rtmp_rr]
    b.rtmp_rr = (b.rtmp_rr + 1) % len(b.rtmp)
    sl = slice(hf * 512, (hf + 1) * 512)
    fw.dve(lambda e: e.tensor_tensor(out=tm[:], in0=p[:, :], in1=Gk[:, sl], op=ALU.mult), r=[p, Gk], w=[tm])
    fw.pool(lambda e: e.tensor_tensor(out=b.h[:, t, sl], in0=b.h[:, t, sl], in1=tm[:], op=ALU.add),
            r=[tm, (b.h, t)], w=[(b.h, t)])


def moe(b, li, tiles):
    fw, cfg = b.fw, b.cfg
    E, NT = cfg.E, cfg.NT
    NG, PER = cfg.GROUPS, cfg.PER
    NL = NG + E
    need_ctx = any(t < cfg.NTC for t in tiles)
    with b.scope():
        Gt = gate_tiles(b, li, 5, need_ctx)
        gates = b.sb("gates", [128, NT, E], F32)
        with b.scope():
            _router(b, li, tiles, gates)
        _experts(b, li, tiles, gates, Gt)


def _router(b, li, tiles, gates):
    fw, cfg = b.fw, b.cfg
    E, NT = cfg.E, cfg.NT
    NG, PER = cfg.GROUPS, cfg.PER
    NL = NG + E
    if True:
        wgr = b.sb("wgr", [128, KC, NL], BF16)
        brow = b.sb("brow_r", [1, NL], F32)
        fw.dma("gpsimd", wgr[:, :, 0:NG], b.dram["moe_w_group"][li].rearrange("(kc p) f -> p kc f", p=128), w=[(wgr, 0)])
        fw.dma("gpsimd", wgr[:, :, NG:NL], b.dram["moe_w_router"][li].rearrange("(kc p) f -> p kc f", p=128), w=[(wgr, 1)])
        fw.dma("sync", brow[0:1, 0:NG], b.dram["moe_b_group"][li:li + 1, :], w=[(brow, 0)])
        fw.dma("sync", brow[0:1, NG:NL], b.dram["moe_b_router"][li:li + 1, :], w=[(brow, 1)])
        L = b.sb("L", [128, NT, NL], F32)
        fw.pool(lambda e: e.memset(L[:], 0.0), w=[L])
        for t in tiles:
            p = b.psum()
            for kc in range(KC):
                fw.pe(lambda e, p=p, kc=kc, t=t: e.matmul(p[:, 0:NL], lhsT=b.xnT[:, kc, t * 128:(t + 1) * 128], rhs=wgr[:, kc, :],
                                                         start=(kc == 0), stop=False), r=[(b.xnT, t), wgr], w=[p])
            fw.pe(lambda e, p=p: e.matmul(p[:, 0:NL], lhsT=b.ones_row[0:1, 0:128], rhs=brow[0:1, :], start=False, stop=True),
                  r=[b.ones_row, brow], w=[p])
            fw.act(lambda e, p=p, t=t: e.copy(out=L[:, t, :], in_=p[:, 0:NL]), r=[p], w=[L])
        def v(name, shape):
            return b.sb(name, shape, F32)
        gmax = v("gmax", [128, NT]); eg = v("eg", [128, NT, NG]); gsum = v("gsum", [128, NT]); pg = v("pg", [128, NT])
        goh = v("goh", [128, NT, NG]); Lm = v("Lm", [128, NT, E]); m1 = v("m1", [128, NT]); oh1 = v("oh1", [128, NT, E])
        m2 = v("m2", [128, NT]); oh2 = v("oh2", [128, NT, E]); w1 = v("w1", [128, NT]); w2 = v("w2", [128, NT])
        pen = v("pen", [128, NT, NG])
        Lg = L[:, :, 0:NG]
        Le = L[:, :, NG:NL]
        dv = fw.dve
        dv(lambda e: e.tensor_reduce(out=gmax[:], in_=Lg, axis=AX.X, op=ALU.max), r=[L], w=[gmax])
        dv(lambda e: e.tensor_tensor(out=eg[:], in0=Lg, in1=gmax[:].unsqueeze(2).to_broadcast([128, NT, NG]), op=ALU.subtract),
           r=[L, gmax], w=[eg])
        dv(lambda e: e.tensor_tensor(out=goh[:], in0=Lg, in1=gmax[:].unsqueeze(2).to_broadcast([128, NT, NG]), op=ALU.is_equal),
           r=[L, gmax], w=[goh])
        fw.act(lambda e: e.activation(out=eg[:], in_=eg[:], func=AF.Exp), r=[eg], w=[eg])
        dv(lambda e: e.tensor_reduce(out=gsum[:], in_=eg[:], axis=AX.X, op=ALU.add), r=[eg], w=[gsum])
        dv(lambda e: e.reciprocal(out=pg[:], in_=gsum[:]), r=[gsum], w=[pg])
        dv(lambda e: e.tensor_scalar(out=pen[:], in0=goh[:], scalar1=-1.0, scalar2=1e30, op0=ALU.add, op1=ALU.mult),
           r=[goh], w=[pen])
        dv(lambda e: e.tensor_tensor(out=Lm[:].rearrange("p t (g e) -> p t g e", g=NG),
                                     in0=Le.rearrange("p t (g e) -> p t g e", g=NG),
                                     in1=pen[:].unsqueeze(3).to_broadcast([128, NT, NG, PER]), op=ALU.add),
           r=[L, pen], w=[Lm])
        dv(lambda e: e.tensor_reduce(out=m1[:], in_=Lm[:], axis=AX.X, op=ALU.max), r=[Lm], w=[m1])
        dv(lambda e: e.tensor_tensor(out=oh1[:], in0=Lm[:], in1=m1[:].unsqueeze(2).to_broadcast([128, NT, E]), op=ALU.is_equal),
           r=[Lm, m1], w=[oh1])
        dv(lambda e: e.scalar_tensor_tensor(out=Lm[:], in0=oh1[:], scalar=-1e30, in1=Lm[:], op0=ALU.mult, op1=ALU.add),
           r=[oh1, Lm], w=[Lm])
        dv(lambda e: e.tensor_reduce(out=m2[:], in_=Lm[:], axis=AX.X, op=ALU.max), r=[Lm], w=[m2])
        dv(lambda e: e.tensor_tensor(out=oh2[:], in0=Lm[:], in1=m2[:].unsqueeze(2).to_broadcast([128, NT, E]), op=ALU.is_equal),
           r=[Lm, m2], w=[oh2])
        dv(lambda e: e.tensor_tensor(out=w2[:], in0=m2[:], in1=m1[:], op=ALU.subtract), r=[m1, m2], w=[w2])
        fw.act(lambda e: e.activation(out=w2[:], in_=w2[:], func=AF.Sigmoid), r=[w2], w=[w2])
        dv(lambda e: e.tensor_scalar(out=w1[:], in0=w2[:], scalar1=-1.0, scalar2=1.0, op0=ALU.mult, op1=ALU.add), r=[w2], w=[w1])
        dv(lambda e: e.tensor_mul(out=w1[:], in0=w1[:], in1=pg[:]), r=[w1, pg], w=[w1])
        dv(lambda e: e.tensor_mul(out=w2[:], in0=w2[:], in1=pg[:]), r=[w2, pg], w=[w2])
        dv(lambda e: e.tensor_tensor(out=oh1[:], in0=oh1[:], in1=w1[:].unsqueeze(2).to_broadcast([128, NT, E]), op=ALU.mult),
           r=[oh1, w1], w=[oh1])
        dv(lambda e: e.tensor_tensor(out=oh2[:], in0=oh2[:], in1=w2[:].unsqueeze(2).to_broadcast([128, NT, E]), op=ALU.mult),
           r=[oh2, w2], w=[oh2])
        dv(lambda e: e.tensor_add(out=gates[:], in0=oh1[:], in1=oh2[:]), r=[oh1, oh2], w=[gates])


def _experts(b, li, tiles, gates, Gt):
    fw, cfg = b.fw, b.cfg
    E, NT = cfg.E, cfg.NT
    if True:
        FF = 512
        w1b = [b.sb("w1b%d" % j, [128, KC, FF], BF16) for j in range(2)]
        w3b = [b.sb("w3b%d" % j, [128, KC, FF], BF16) for j in range(2)]
        w2b = [b.sb("w2b%d" % j, [128, 4, D], BF16) for j in range(2)]
        hid = [b.sb("hid%d" % j, [128, 4, 512], BF16) for j in range(2)]
        s1 = [b.sb("s1_%d" % j, [128, 512], BF16) for j in range(2)]
        mtmp = [b.sb("mtmp%d" % j, [128, 512], F32) for j in range(2)]
        blocks = []
        i0 = 0
        while i0 < len(tiles):
            blocks.append(tiles[i0:i0 + 4])
            i0 += 4
        cnt = 0
        for ex in range(E):
            j = ex % 2
            fw.dma("gpsimd", w1b[j][:], b.dram["moe_w1"][li, ex].rearrange("(kc p) f -> p kc f", p=128), w=[w1b[j]])
            fw.dma("gpsimd", w3b[j][:], b.dram["moe_w3"][li, ex].rearrange("(kc p) f -> p kc f", p=128), w=[w3b[j]])
            fw.dma("gpsimd", w2b[j][:], b.dram["moe_w2"][li, ex].rearrange("(kc p) f -> p kc f", p=128), w=[w2b[j]])
            for blk in blocks:
                t0 = blk[0]
                nt = len(blk) * 128
                c0 = t0 * 128
                hd = hid[cnt % 2]
                cnt += 1
                for fc in range(4):
                    p1 = b.psum()
                    p3 = b.psum()
                    for kc in range(KC):
                        fw.pe(lambda e, p1=p1, kc=kc, fc=fc, c0=c0, nt=nt, j=j: e.matmul(
                            p1[:, 0:nt], lhsT=w1b[j][:, kc, fc * 128:(fc + 1) * 128], rhs=b.xnT[:, kc, c0:c0 + nt],
                            start=(kc == 0), stop=(kc == KC - 1)), r=[w1b[j], b.xnT], w=[p1])
                    for kc in range(KC):
                        fw.pe(lambda e, p3=p3, kc=kc, fc=fc, c0=c0, nt=nt, j=j: e.matmul(
                            p3[:, 0:nt], lhsT=w3b[j][:, kc, fc * 128:(fc + 1) * 128], rhs=b.xnT[:, kc, c0:c0 + nt],
                            start=(kc == 0), stop=(kc == KC - 1)), r=[w3b[j], b.xnT], w=[p3])
                    sj = s1[fc % 2]
                    fw.act(lambda e, p1=p1, sj=sj, nt=nt: e.activation(out=sj[:, 0:nt], in_=p1[:, 0:nt], func=AF.Silu),
                           r=[p1], w=[sj])
                    fw.dve(lambda e, p3=p3, sj=sj, nt=nt, hd=hd, fc=fc: e.tensor_tensor(
                        out=hd[:, fc, 0:nt], in0=p3[:, 0:nt], in1=sj[:, 0:nt], op=ALU.mult), r=[p3, sj], w=[(hd, fc)])
                for ti, t in enumerate(blk):
                    k = 1 if t < cfg.NTC else 0
                    for hf in range(2):
                        py = b.psum()
                        for fc in range(4):
                            fw.pe(lambda e, py=py, fc=fc, ti=ti, hf=hf, hd=hd, j=j: e.matmul(
                                py[:, :], lhsT=hd[:, fc, ti * 128:(ti + 1) * 128], rhs=w2b[j][:, fc, hf * 512:(hf + 1) * 512],
                                start=(fc == 0), stop=(fc == 3)), r=[hd, w2b[j]], w=[py])
                        tm = mtmp[(ti * 2 + hf) % 2]
                        sl = slice(hf * 512, (hf + 1) * 512)
                        fw.dve(lambda e, py=py, tm=tm, t=t, ex=ex, k=k, sl=sl: e.scalar_tensor_tensor(
                            out=tm[:], in0=py[:, :], scalar=gates[:, t, ex:ex + 1], in1=Gt[k][:, sl], op0=ALU.mult, op1=ALU.mult),
                            r=[py, gates, Gt[k]], w=[tm])
                        fw.pool(lambda e, tm=tm, t=t, sl=sl: e.tensor_tensor(out=b.h[:, t, sl], in0=b.h[:, t, sl], in1=tm[:], op=ALU.add),
                                r=[tm, (b.h, t)], w=[(b.h, t)])


MIXERS = {}


def build_program(cfg):
    nc = bass.Bass("TRN2", target_bir_lowering=False)
    b = B(nc, cfg)
    fw = b.fw
    NB, NT, NTC = cfg.NB, cfg.NT, cfg.NTC
    x_d = b.din("x", [NB, cfg.S_LAT, D])
    ctx_d = b.din("ctx", [NB, cfg.S_CTX, D])
    b.din("c", [NB, D])
    b.din("c_ctx", [1, D])
    for name, shape in cfg.wshapes.items():
        b.din(name, shape)
    out_d = nc.dram_tensor("out", [NB, cfg.S_LAT, D], F32, kind="ExternalOutput").ap()
    finals = []
    with b.scope():
        build_consts(b)
        b.one_col = b.ones_f
        b.h = b.sb("h", [128, NT, D], F32)
        b.scB = [b.sb("scB%d" % k, [128, KC, 128], F32) for k in range(2)]
        for bi in range(NB):
            with b.scope():
                fw.dma("sync", b.h[:, 0:NTC, :], ctx_d[bi].rearrange("(t p) d -> p t d", p=128), w=[b.h])
                fw.dma("sync", b.h[:, NTC:NT, :], x_d[bi].rearrange("(t p) d -> p t d", p=128), w=[b.h])
                load_cond(b, bi)
                for li in range(cfg.DEPTH):
                    kind = cfg.kinds[li]
                    wc = cfg.want_ctx[li]
                    with b.scope():
                        b.rtmp = [b.sb("rtmp%d" % j, [128, 512], F32) for j in range(2)]
                        b.rtmp_rr = 0
                        all_tiles = list(range(NT))
                        out_tiles = all_tiles if wc else list(range(NTC, NT))
                        if kind >= 0:
                            MIXERS[kind](b, li, all_tiles, out_tiles)
                        if cfg.moe:
                            with b.scope():
                                b.xnT = b.sb("xnT", [128, KC, NT * 128], BF16)
                                norm_mod(b, li, 2, out_tiles)
                                moe(b, li, out_tiles)
                finals.append(fw.dma("sync", out_d[bi].rearrange("(t p) d -> p t d", p=128), b.h[:, NTC:NT, :], r=[b.h]))
        fw.emit(final_wait_ops=finals)
    return nc, b


def host_tables(cfg):
    out = {}
    for name, rot in (("rope64", 64), ("rope32", 32)):
        n = cfg.S_LAT
        rows = n // 64
        row = np.repeat(np.arange(rows, dtype=np.float32), 64)
        col = np.tile(np.arange(64, dtype=np.float32), rows)
        nf = rot // 4
        inv = (10000.0 ** (-np.arange(nf, dtype=np.float32) / nf)).astype(np.float32)
        ang = np.concatenate([row[:, None] * inv, col[:, None] * inv], axis=-1)
        cs = np.concatenate([np.cos(ang), np.sin(ang)], axis=-1).astype(np.float32)
        ctxp = np.concatenate([np.ones((cfg.S_CTX, rot // 2), np.float32), np.zeros((cfg.S_CTX, rot // 2), np.float32)], axis=-1)
        out["c_" + name] = np.concatenate([ctxp, cs], axis=0)
    return out


def load_w(b, name, dram_ap, kchunks, n, dt=BF16, eng="gpsimd"):
    t = b.sb(name, [128, kchunks, n], dt)
    b.fw.dma(eng, t[:], dram_ap.rearrange("(kc p) f -> p kc f", p=128), w=[t])
    return t


def lin_tok(b, xT, t, W, n0, n, p, kchunks=KC, rx=None):
    fw = b.fw
    for kc in range(kchunks):
        fw.pe(lambda e, kc=kc: e.matmul(p[:, 0:n], lhsT=xT[:, kc, t * 128:(t + 1) * 128], rhs=W[:, kc, n0:n0 + n],
                                        start=(kc == 0), stop=(kc == kchunks - 1)),
              r=[(xT, t) if rx is None else rx, W], w=[p])


def transposes(b, src, nblk, dst_ap_fn, r, w):
    fw = b.fw
    pb = b.psumb()
    for k in range(nblk):
        fw.pe(lambda e, k=k: e.transpose(out=pb[:, k * 128:(k + 1) * 128], in_=src[:, k * 128:(k + 1) * 128], identity=b.identb[:]),
              r=[src, b.identb], w=[pb])
    fw.act(lambda e: e.copy(out=dst_ap_fn(), in_=pb[:, 0:nblk * 128].rearrange("p (k q) -> p k q", k=nblk)), r=[pb], w=w)


def row_bcast_tile(b, name, dram_row_ap, n, dt=F32):
    t = b.sb(name, [128, n], dt)
    with b.scope():
        row = b.sb(name + "_row", [1, n], F32)
        b.fw.dma("sync", row[:], dram_row_ap, w=[row])
        b.bcast_rows(t, 0, (row, row), n)
    return t


def out_proj_resid(b, li, t, srcT, W, kchunks, Gk, rsrc):
    fw = b.fw
    for hf in range(2):
        p = b.psum()
        for kc in range(kchunks):
            fw.pe(lambda e, kc=kc, p=p, hf=hf: e.matmul(p[:, :], lhsT=srcT[:, kc, :], rhs=W[:, kc, hf * 512:(hf + 1) * 512],
                                                       start=(kc == 0), stop=(kc == kchunks - 1)), r=[rsrc, W], w=[p])
        resid_add(b, t, p, hf, Gk)


def gmlp(b, li, all_tiles, out_tiles):
    fw, cfg = b.fw, b.cfg
    dr = b.dram
    with b.scope():
        b.xnT = b.sb("xnT", [128, KC, cfg.NT * 128], BF16)
        norm_mod(b, li, 1, out_tiles)
        need_ctx = any(t < cfg.NTC for t in out_tiles)
        Gt = gate_tiles(b, li, 2, need_ctx)
        w_in = load_w(b, "gm_win", dr["gm_w_in"][0], KC, 2 * D)
        w_out = load_w(b, "gm_wout", dr["gm_w_out"][0], KC, D)
        vg = row_bcast_tile(b, "gm_vg", dr["gm_v_g"][0:1, :], D)
        ws_raw = b.sb("ws_raw", [128, 8, 128], BF16)
        fw.dma("gpsimd", ws_raw[:], dr["gm_w_s"][0].rearrange("g p q -> p g q"), w=[ws_raw])
        wsT = b.sb("wsT", [128, 8, 128], BF16)
        transposes(b, ws_raw[:].rearrange("p g q -> p (g q)"), 8, lambda: wsT[:], [ws_raw], [wsT])
        bs_raw = b.sb("bs_raw", [8, 128], F32)
        fw.dma("sync", bs_raw[:], dr["gm_b_s"][0], w=[bs_raw])
        pbs = b.psum()
        fw.pe(lambda e: e.matmul(pbs[:, 0:8], lhsT=bs_raw[:, :], rhs=b.identf[0:8, 0:8], start=True, stop=True),
              r=[bs_raw, b.identf], w=[pbs])
        bsT = b.sb("bsT", [128, 8], F32)
        fw.act(lambda e: e.copy(out=bsT[:], in_=pbs[:, 0:8]), r=[pbs], w=[bsT])
        u = [b.sb("gm_u%d" % j, [128, D], F32) for j in range(1)]
        v = [b.sb("gm_v%d" % j, [128, D], F32) for j in range(1)]
        vb = [b.sb("gm_vb%d" % j, [128, D], BF16) for j in range(1)]
        us = [b.sb("gm_us%d" % j, [128, D], BF16) for j in range(1)]
        usT = [b.sb("gm_usT%d" % j, [128, KC, 128], BF16) for j in range(1)]
        ss = b.sb("gm_ss", [128, cfg.NT], F32)
        fw.pool(lambda e: e.memset(ss[:], 0.0), w=[ss])
        for j, t in enumerate(out_tiles):
            k = 1 if t < cfg.NTC else 0
            uj, vj, vbj, usj, usTj = u[0], v[0], vb[0], us[0], usT[0]
            for blk in range(4):
                p = b.psum()
                lin_tok(b, b.xnT, t, w_in, blk * 512, 512, p)
                dst = uj if blk < 2 else vj
                c0 = (blk % 2) * 512
                fw.act(lambda e, p=p, dst=dst, c0=c0: e.activation(out=dst[:, c0:c0 + 512], in_=p[:, :], func=AF.Gelu),
                       r=[p], w=[(dst, blk % 2)])
            fw.act(lambda e, vj=vj, vbj=vbj, t=t: e.activation(out=vbj[:], in_=vj[:], func=AF.Square, accum_out=ss[:, t:t + 1]),
                   r=[vj], w=[vbj, (ss, t)])
            rstd_op(b, ss[:, t:t + 1], ss[:, t:t + 1], 1.0 / D, [(ss, t)], [(ss, t)])
            fw.dve(lambda e, vj=vj, vbj=vbj, t=t: e.scalar_tensor_tensor(out=vbj[:], in0=vj[:], scalar=ss[:, t:t + 1], in1=vg[:],
                                                                        op0=ALU.mult, op1=ALU.mult), r=[vj, (ss, t), vg], w=[vbj])
            for hf in range(2):
                p = b.psum()
                for g4 in range(4):
                    g = hf * 4 + g4
                    fw.pe(lambda e, p=p, g=g, g4=g4, vbj=vbj: e.matmul(p[:, g4 * 128:(g4 + 1) * 128], lhsT=wsT[:, g, :],
                                                                      rhs=vbj[:, g * 128:(g + 1) * 128], start=True, stop=True),
                          r=[wsT, vbj], w=[p])
                tmp = b.rtmp[b.rtmp_rr]
                b.rtmp_rr = (b.rtmp_rr + 1) % len(b.rtmp)
                fw.dve(lambda e, p=p, tmp=tmp, hf=hf: e.tensor_tensor(
                    out=tmp[:].rearrange("p (g d) -> p g d", g=4), in0=p[:, :].rearrange("p (g d) -> p g d", g=4),
                    in1=bsT[:, hf * 4:(hf + 1) * 4].unsqueeze(2).to_broadcast([128, 4, 128]), op=ALU.add), r=[p, bsT], w=[tmp])
                fw.pool(lambda e, tmp=tmp, uj=uj, usj=usj, hf=hf: e.tensor_tensor(
                    out=usj[:, hf * 512:(hf + 1) * 512], in0=tmp[:], in1=uj[:, hf * 512:(hf + 1) * 512], op=ALU.mult),
                    r=[tmp, uj], w=[(usj, hf)])
            transposes(b, usj, KC, lambda usTj=usTj: usTj[:], [usj], [usTj])
            out_proj_resid(b, li, t, usTj, w_out, KC, Gt[k], usTj)


MIXERS[1] = gmlp


def attn_core(b, qT, kT, v_aug, vd, qtiles, ktiles, scale, Osb, r_q, r_k, r_v):
    fw = b.fw
    nq = len(qtiles) * 128
    q0 = qtiles[0] * 128
    w1 = vd + 1
    per_bank = 1
    pexp = b.pexp
    obanks = [b.ps[0], b.ps[1], b.ps[2], b.ps[3]]
    assert len(qtiles) <= 4
    for ki, kt in enumerate(ktiles):
        ps_s = b.ps[4 + (b.sc_rr % 2)]
        b.sc_rr += 1
        fw.pe(lambda e, ps_s=ps_s, kt=kt: e.matmul(ps_s[:, 0:nq], lhsT=kT(kt * 128, 128), rhs=qT(q0, nq), start=True, stop=True),
              r=[r_q, r_k], w=[ps_s])
        pe_ = pexp[ki % 2]
        fw.act(lambda e, ps_s=ps_s, pe_=pe_: e.activation(out=pe_[:, 0:nq], in_=ps_s[:, 0:nq], func=AF.Exp, scale=scale),
               r=[ps_s], w=[pe_])
        for qi in range(len(qtiles)):
            ob = obanks[qi // per_bank]
            oc = (qi % per_bank) * w1
            fw.pe(lambda e, ob=ob, oc=oc, qi=qi, pe_=pe_, kt=kt, ki=ki: e.matmul(ob[:, oc:oc + w1], lhsT=pe_[:, qi * 128:(qi + 1) * 128],
                                                                       rhs=v_aug(kt), start=(ki == 0), stop=(ki == len(ktiles) - 1)),
                  r=[pe_, r_v], w=[ob])
    for qi in range(len(qtiles)):
        ob = obanks[qi // per_bank]
        oc = (qi % per_bank) * w1
        fw.act(lambda e, ob=ob, oc=oc, qi=qi: e.copy(out=Osb[:, qi, 0:w1], in_=ob[:, oc:oc + w1]), r=[ob], w=[(Osb, qi)])


def qblocks(cfg, out_tiles):
    blocks = []
    ctx_q = [t for t in out_tiles if t < cfg.NTC]
    if ctx_q:
        blocks.append((ctx_q, list(range(cfg.NTC))))
    lat = [t for t in out_tiles if t >= cfg.NTC]
    return blocks, lat


def rope_apply(b, dst, src, cs_t, ngrp, half, r, w):
    fw = b.fw
    t1, t2 = b.rope_tmp
    cos = cs_t[:, 0:half].unsqueeze(1).to_broadcast([128, ngrp, half])
    sin = cs_t[:, half:2 * half].unsqueeze(1).to_broadcast([128, ngrp, half])
    x1 = src[:, :, 0, :]
    x2 = src[:, :, 1, :]
    v1 = t1[:, 0:ngrp * half].rearrange("p (g h) -> p g h", g=ngrp)
    v2 = t2[:, 0:ngrp * half].rearrange("p (g h) -> p g h", g=ngrp)
    fw.dve(lambda e: e.tensor_tensor(out=v1, in0=x1, in1=cos, op=ALU.mult), r=r, w=[t1])
    fw.pool(lambda e: e.tensor_tensor(out=v2, in0=x2, in1=sin, op=ALU.mult), r=r, w=[t2])
    fw.dve(lambda e: e.tensor_tensor(out=dst[:, :, 0, :], in0=v1, in1=v2, op=ALU.subtract), r=[t1, t2], w=w)
    fw.dve(lambda e: e.tensor_tensor(out=v1, in0=x1, in1=sin, op=ALU.mult), r=r, w=[t1])
    fw.pool(lambda e: e.tensor_tensor(out=v2, in0=x2, in1=cos, op=ALU.mult), r=r, w=[t2])
    fw.dve(lambda e: e.tensor_tensor(out=dst[:, :, 1, :], in0=v1, in1=v2, op=ALU.add), r=[t1, t2], w=w)


def group_rms(b, x, ngrp, gd, ssq, r, w_ssq):
    fw = b.fw
    sq = b.sq_tmp
    fw.pool(lambda e: e.tensor_tensor(out=sq[:, 0:ngrp * gd], in0=x[:, 0:ngrp * gd], in1=x[:, 0:ngrp * gd], op=ALU.mult), r=r, w=[sq])
    fw.dve(lambda e: e.tensor_reduce(out=ssq[:, 0:ngrp], in_=sq[:, 0:ngrp * gd].rearrange("p (g d) -> p g d", g=ngrp),
                                     axis=AX.X, op=ALU.add), r=[sq], w=w_ssq)
    rstd_op(b, ssq[:, 0:ngrp], ssq[:, 0:ngrp], 1.0 / gd, w_ssq, w_ssq)


def diff_attn(b, li, all_tiles, out_tiles):
    fw, cfg = b.fw, b.cfg
    dr = b.dram
    NT = cfg.NT
    lam_init = 0.8 - 0.6 * math.exp(-0.3 * cfg.layer_ids[li])
    with b.scope():
        b.xnT = b.sb("xnT", [128, KC, NT * 128], BF16)
        norm_mod(b, li, 1, all_tiles)
        need_ctx = any(t < cfg.NTC for t in out_tiles)
        Gt = gate_tiles(b, li, 2, need_ctx)
        lr = b.sb("lamrow", [1, 4, 64], F32)
        for j, nm in enumerate(("da_lam_q1", "da_lam_k1", "da_lam_q2", "da_lam_k2")):
            fw.dma("sync", lr[0:1, j, :], dr[nm][0:1, :], w=[(lr, j)])
        lp = b.sb("lamp", [1, 2, 64], F32)
        ls = b.sb("lams", [1, 4], F32)
        fw.dve(lambda e: e.tensor_tensor(out=lp[0:1, :, :], in0=lr[0:1, 0:4:2, :], in1=lr[0:1, 1:4:2, :], op=ALU.mult), r=[lr], w=[lp])
        fw.dve(lambda e: e.tensor_reduce(out=ls[0:1, 0:2], in_=lp[0:1, :, :], axis=AX.X, op=ALU.add), r=[lp], w=[ls])
        fw.act(lambda e: e.activation(out=ls[0:1, 0:2], in_=ls[0:1, 0:2], func=AF.Exp), r=[ls], w=[ls])
        fw.dve(lambda e: e.tensor_tensor(out=ls[0:1, 2:3], in0=ls[0:1, 0:1], in1=ls[0:1, 1:2], op=ALU.subtract), r=[ls], w=[ls])
        fw.dve(lambda e: e.tensor_scalar(out=ls[0:1, 3:4], in0=ls[0:1, 2:3], scalar1=lam_init, scalar2=-1.0, op0=ALU.add, op1=ALU.mult),
               r=[ls], w=[ls])
        nlam = b.sb("nlam", [128, 1], F32)
        b.bcast_rows(nlam, 0, (ls, T(ls[0:1, 3:4], ls.name)), 1)
        grow = b.sb("da_grow", [1, 256], F32)
        for j in range(4):
            fw.dma("sync", grow[0:1, j * 64:(j + 1) * 64], dr["da_q_g" if j < 2 else "da_k_g"][0:1, :], w=[(grow, j)])
        qkg = b.sb("da_qkg", [128, 256], F32)
        b.bcast_rows(qkg, 0, (grow, grow), 256)
        cs = b.sb("da_cs", [128, NT, 64], F32)
        fw.dma("sync", cs[:], dr["c_rope64"].rearrange("(t p) c -> p t c", p=128), w=[cs])
        w_out = load_w(b, "da_wout", dr["da_w_out"][0], 8, D)
        sgrow = b.sb("da_sgrow", [1, 128], F32)
        fw.dma("sync", sgrow[:], dr["da_sub_g"][0:1, :], w=[sgrow])
        pc = b.psum()
        fw.pe(lambda e: e.matmul(pc[:, 0:1], lhsT=sgrow[0:1, :], rhs=b.ones_row[0:1, 0:1], start=True, stop=True),
              r=[sgrow, b.ones_row], w=[pc])
        sgcol = b.sb("da_sgcol", [128, 1], F32)
        fw.act(lambda e: e.activation(out=sgcol[:], in_=pc[:, 0:1], func=AF.Copy, scale=(1.0 - lam_init)), r=[pc], w=[sgcol])
        fw.dve(lambda e: e.tensor_scalar(out=w_out[:], in0=w_out[:], scalar1=sgcol[:, 0:1], scalar2=None, op0=ALU.mult),
               r=[w_out, sgcol], w=[w_out])
        b.pexp = [b.sb("pexp%d" % j, [128, 512], BF16) for j in range(2)]
        b.sc_rr = 0
        b.rope_tmp = [b.sb("ropet%d" % j, [128, 512], F32) for j in range(2)]
        b.sq_tmp = b.sb("sqtmp", [128, 256], F32)
        qkT = b.sb("da_qkT", [128, 2, NT * 128], BF16)
        v_aug = b.sb("da_vaug", [128, NT, 132], BF16)
        fw.pool(lambda e: e.memset(v_aug[:, :, 128:129], 1.0), w=[v_aug])
        W_h = [b.sb("da_Wh%d" % j, [128, KC, 384], BF16) for j in range(1)]
        qk = b.sb("da_qk", [128, 256], F32)
        qkn = b.sb("da_qkn", [128, 256], F32)
        qkr = b.sb("da_qkr", [128, 256], BF16)
        ssq = b.sb("da_ssq", [128, 4], F32)
        Osb = [b.sb("da_O%d" % j, [128, 4, 132], F32) for j in range(2)]
        rr = b.sb("da_rr", [128, 2], F32)
        o = b.sb("da_o", [128, 128], F32)
        ob = b.sb("da_ob", [128, 128], BF16)
        oss = b.sb("da_oss", [128, 1], F32)
        junk = b.sb("da_junk", [128, 128], F32)
        oT = b.sb("da_oT", [128, 1, 128], BF16)
        w_in = dr["da_w_in"][0]
        blocks, lat = qblocks(cfg, out_tiles)
        for i0 in range(0, len(lat), 4):
            blocks.append((lat[i0:i0 + 4], all_tiles))
        for hd in range(8):
            Wh = W_h[0]
            for j in range(3):
                fw.dma("gpsimd", Wh[:, :, j * 128:(j + 1) * 128],
                       w_in[:, j * 1024 + hd * 128:j * 1024 + (hd + 1) * 128].rearrange("(kc p) f -> p kc f", p=128), w=[(Wh, j)])
            for t in all_tiles:
                p = b.psum()
                lin_tok(b, b.xnT, t, Wh, 0, 384, p)
                fw.act(lambda e, p=p: e.copy(out=qk[:], in_=p[:, 0:256]), r=[p], w=[qk])
                fw.act(lambda e, p=p, t=t: e.copy(out=v_aug[:, t, 0:128], in_=p[:, 256:384]), r=[p], w=[(v_aug, t)])
                group_rms(b, qk, 4, 64, ssq, [qk], [ssq])
                fw.dve(lambda e: e.tensor_tensor(out=qkn[:].rearrange("p (g d) -> p g d", g=4), in0=qk[:].rearrange("p (g d) -> p g d", g=4),
                                                 in1=ssq[:, 0:4].unsqueeze(2).to_broadcast([128, 4, 64]), op=ALU.mult), r=[qk, ssq], w=[qkn])
                fw.pool(lambda e: e.tensor_tensor(out=qkn[:], in0=qkn[:], in1=qkg[:], op=ALU.mult), r=[qkn, qkg], w=[qkn])
                rope_apply(b, qkr[:].rearrange("p (g two h) -> p g two h", g=4, two=2), qkn[:].rearrange("p (g two h) -> p g two h", g=4, two=2),
                           cs[:, t, :], 4, 32, [qkn, cs], [qkr])
                transposes(b, qkr, 2, lambda t=t: qkT[:, :, t * 128:(t + 1) * 128], [qkr], [(qkT, t)])
            for (qts, kts) in blocks:
                for comp in range(2):
                    sl = slice(comp * 64, (comp + 1) * 64)
                    attn_core(b, lambda c0, n, sl=sl: qkT[sl, 0, c0:c0 + n], lambda c0, n, sl=sl: qkT[sl, 1, c0:c0 + n],
                              lambda kt: v_aug[:, kt, 0:129], 128, qts, kts, 0.125, Osb[comp], qkT, qkT, v_aug)
                for qi, t in enumerate(qts):
                    k = 1 if t < cfg.NTC else 0
                    fw.dve(lambda e, qi=qi: e.reciprocal(out=rr[:, 0:1], in_=Osb[0][:, qi, 128:129]), r=[(Osb[0], qi)], w=[rr])
                    fw.dve(lambda e, qi=qi: e.reciprocal(out=rr[:, 1:2], in_=Osb[1][:, qi, 128:129]), r=[(Osb[1], qi)], w=[rr])
                    fw.dve(lambda e: e.tensor_tensor(out=rr[:, 1:2], in0=rr[:, 1:2], in1=nlam[:, 0:1], op=ALU.mult), r=[rr, nlam], w=[rr])
                    fw.dve(lambda e, qi=qi: e.tensor_scalar(out=o[:], in0=Osb[0][:, qi, 0:128], scalar1=rr[:, 0:1], scalar2=None, op0=ALU.mult),
                           r=[(Osb[0], qi), rr], w=[o])
                    fw.dve(lambda e, qi=qi: e.scalar_tensor_tensor(out=o[:], in0=Osb[1][:, qi, 0:128], scalar=rr[:, 1:2], in1=o[:],
                                                                  op0=ALU.mult, op1=ALU.add), r=[(Osb[1], qi), rr, o], w=[o])
                    fw.pool(lambda e: e.memset(oss[:], 0.0), w=[oss])
                    fw.act(lambda e: e.activation(out=junk[:], in_=o[:], func=AF.Square, accum_out=oss[:, 0:1]), r=[o, oss], w=[junk, oss])
                    rstd_op(b, oss[:, 0:1], oss[:, 0:1], 1.0 / 128, [oss], [oss])
                    fw.dve(lambda e: e.tensor_scalar(out=ob[:], in0=o[:], scalar1=oss[:, 0:1], scalar2=None, op0=ALU.mult), r=[o, oss], w=[ob])
                    transposes(b, ob, 1, lambda: oT[:], [ob], [oT])
                    out_proj_resid(b, li, t, oT, T(w_out[:, hd:hd + 1, :], w_out.name), 1, Gt[k], oT)


MIXERS[0] = diff_attn


def mla(b, li, all_tiles, out_tiles):
    fw, cfg = b.fw, b.cfg
    dr = b.dram
    NT = cfg.NT
    with b.scope():
        need_ctx = any(t < cfg.NTC for t in out_tiles)
        Gt = gate_tiles(b, li, 2, need_ctx)
        cqnT = b.sb("cqnT", [128, 3, NT * 128], BF16)
        ckvnT = b.sb("ckvnT", [128, 2, NT * 128], BF16)
        kpe = b.sb("kpe", [128, NT, 32], F32)
        b.sq_tmp = b.sb("sqtmp", [128, 512], F32)
        ssq = b.sb("ml_ssq", [128, 8], F32)
        with b.scope():
            b.xnT = b.sb("xnT", [128, KC, NT * 128], BF16)
            norm_mod(b, li, 1, all_tiles)
            w_in = load_w(b, "ml_win", dr["mla_w_in"][0], KC, 672)
            qng = row_bcast_tile(b, "ml_qng", dr["mla_q_norm_g"][0:1, :], 384)
            kvng = row_bcast_tile(b, "ml_kvng", dr["mla_kv_norm_g"][0:1, :], 256)
            cq = b.sb("ml_cq", [128, 384], F32)
            ckv = b.sb("ml_ckv", [128, 288], F32)
            cqb = b.sb("ml_cqb", [128, 384], BF16)
            ckvb = b.sb("ml_ckvb", [128, 256], BF16)
            for t in all_tiles:
                p0 = b.psum()
                lin_tok(b, b.xnT, t, w_in, 0, 384, p0)
                p1 = b.psum()
                lin_tok(b, b.xnT, t, w_in, 384, 288, p1)
                fw.act(lambda e, p0=p0: e.copy(out=cq[:], in_=p0[:, 0:384]), r=[p0], w=[cq])
                fw.act(lambda e, p1=p1: e.copy(out=ckv[:], in_=p1[:, 0:288]), r=[p1], w=[ckv])
                fw.pool(lambda e, t=t: e.tensor_copy(out=kpe[:, t, :], in_=ckv[:, 256:288]), r=[ckv], w=[(kpe, t)])
                group_rms(b, cq, 1, 384, ssq, [cq], [ssq])
                fw.dve(lambda e: e.scalar_tensor_tensor(out=cqb[:], in0=cq[:], scalar=ssq[:, 0:1], in1=qng[:], op0=ALU.mult, op1=ALU.mult),
                       r=[cq, ssq, qng], w=[cqb])
                transposes(b, cqb, 3, lambda t=t: cqnT[:, :, t * 128:(t + 1) * 128], [cqb], [(cqnT, t)])
                group_rms(b, ckv, 1, 256, ssq, [ckv], [ssq])
                fw.dve(lambda e: e.scalar_tensor_tensor(out=ckvb[:], in0=ckv[:, 0:256], scalar=ssq[:, 0:1], in1=kvng[:], op0=ALU.mult, op1=ALU.mult),
                       r=[ckv, ssq, kvng], w=[ckvb])
                transposes(b, ckvb, 2, lambda t=t: ckvnT[:, :, t * 128:(t + 1) * 128], [ckvb], [(ckvnT, t)])
        with b.scope():
            qg = row_bcast_tile(b, "ml_qg", dr["mla_q_g"][0:1, :], 96)
            kg = row_bcast_tile(b, "ml_kg", dr["mla_k_g"][0:1, :], 96)
            cs = b.sb("ml_cs", [128, NT, 32], F32)
            fw.dma("sync", cs[:], dr["c_rope32"].rearrange("(t p) c -> p t c", p=128), w=[cs])
            b.pexp = [b.sb("pexp%d" % j, [128, 512], BF16) for j in range(2)]
            b.sc_rr = 0
            b.rope_tmp = [b.sb("ropet%d" % j, [128, 512], F32) for j in range(2)]
            qT_g = b.sb("ml_qT", [128, 4, NT * 128], BF16)
            kT_g = b.sb("ml_kT", [128, 4, NT * 128], BF16)
            v_aug = b.sb("ml_vaug", [128, NT, 4, 66], BF16)
            fw.pool(lambda e: e.memset(v_aug[:, :, :, 64:65], 1.0), w=[v_aug])
            wuq = [b.sb("ml_wuq%d" % j, [128, 3, 384], BF16) for j in range(2)]
            wukv = [b.sb("ml_wukv%d" % j, [128, 2, 512], BF16) for j in range(2)]
            wout = [b.sb("ml_wout%d" % j, [128, 2, D], BF16) for j in range(2)]
            qs = b.sb("ml_qs", [128, 384], F32)
            ks = b.sb("ml_ks", [128, 384], F32)
            qr = b.sb("ml_qr", [128, 384], BF16)
            kr = b.sb("ml_kr", [128, 384], BF16)
            Osb = [b.sb("ml_O%d" % j, [128, 4, 68], F32) for j in range(2)]
            rr = b.sb("ml_rr", [128, 1], F32)
            opair = b.sb("ml_opair", [128, 128], BF16)
            oT = b.sb("ml_oT", [128, 1, 128], BF16)
            blocks, lat = qblocks(cfg, out_tiles)
            for i0 in range(0, len(lat), 4):
                blocks.append((lat[i0:i0 + 4], all_tiles))
            for hg in range(4):
                j = hg % 2
                fw.dma("gpsimd", wuq[j][:], dr["mla_w_uq"][0][:, hg * 384:(hg + 1) * 384].rearrange("(kc p) f -> p kc f", p=128), w=[wuq[j]])
                fw.dma("gpsimd", wukv[j][:], dr["mla_w_ukv"][0][:, hg * 512:(hg + 1) * 512].rearrange("(kc p) f -> p kc f", p=128), w=[wukv[j]])
                fw.dma("gpsimd", wout[j][:], dr["mla_w_out"][0][hg * 256:(hg + 1) * 256, :].rearrange("(kc p) f -> p kc f", p=128), w=[wout[j]])
                for t in all_tiles:
                    pq = b.psum()
                    lin_tok(b, cqnT, t, wuq[j], 0, 384, pq, kchunks=3)
                    pkv = b.psum()
                    lin_tok(b, ckvnT, t, wukv[j], 0, 512, pkv, kchunks=2)
                    fw.act(lambda e, pq=pq: e.copy(out=qs[:], in_=pq[:, 0:384]), r=[pq], w=[qs])
                    pkv4 = pkv[:, :].rearrange("p (h d) -> p h d", h=4)
                    fw.act(lambda e, pkv4=pkv4: e.copy(out=ks[:].rearrange("p (h d) -> p h d", h=4)[:, :, 0:64], in_=pkv4[:, :, 0:64]),
                           r=[pkv], w=[(ks, 0)])
                    fw.pool(lambda e, t=t: e.tensor_copy(out=ks[:].rearrange("p (h d) -> p h d", h=4)[:, :, 64:96],
                                                        in_=kpe[:, t, :].unsqueeze(1).to_broadcast([128, 4, 32])), r=[(kpe, t)], w=[(ks, 1)])
                    fw.act(lambda e, pkv4=pkv4, t=t: e.copy(out=v_aug[:, t, :, 0:64], in_=pkv4[:, :, 64:128]), r=[pkv], w=[(v_aug, t)])
                    for (src, gain, dst, dstT) in ((qs, qg, qr, qT_g), (ks, kg, kr, kT_g)):
                        group_rms(b, src, 4, 96, ssq, [src], [ssq])
                        s4 = src[:].rearrange("p (h d) -> p h d", h=4)
                        fw.dve(lambda e, s4=s4: e.tensor_tensor(out=s4, in0=s4, in1=ssq[:, 0:4].unsqueeze(2).to_broadcast([128, 4, 96]), op=ALU.mult),
                               r=[src, ssq], w=[src])
                        fw.pool(lambda e, s4=s4, gain=gain: e.tensor_tensor(out=s4, in0=s4, in1=gain[:].unsqueeze(1).to_broadcast([128, 4, 96]), op=ALU.mult),
                                r=[src, gain], w=[src])
                        d4 = dst[:].rearrange("p (h d) -> p h d", h=4)
                        fw.act(lambda e, s4=s4, d4=d4: e.copy(out=d4[:, :, 0:64], in_=s4[:, :, 0:64]), r=[src], w=[(dst, 0)])
                        rope_apply(b, d4[:, :, 64:96].rearrange("p h (two x) -> p h two x", two=2),
                                   s4[:, :, 64:96].rearrange("p h (two x) -> p h two x", two=2), cs[:, t, :], 4, 16, [src, cs], [(dst, 1)])
                        pb = b.psumb()
                        for h in range(4):
                            fw.pe(lambda e, pb=pb, h=h, dst=dst: e.transpose(out=pb[0:96, h * 128:(h + 1) * 128], in_=dst[:, h * 96:(h + 1) * 96],
                                                                            identity=b.identb[:]), r=[dst, b.identb], w=[pb])
                        fw.act(lambda e, pb=pb, t=t, dstT=dstT: e.copy(out=dstT[0:96, :, t * 128:(t + 1) * 128],
                                                                      in_=pb[0:96, 0:512].rearrange("p (k q) -> p k q", k=4)), r=[pb], w=[(dstT, t)])
                for pair in range(2):
                    for (qts, kts) in blocks:
                        for hh in range(2):
                            h = pair * 2 + hh
                            attn_core(b, lambda c0, n, h=h: qT_g[0:96, h, c0:c0 + n], lambda c0, n, h=h: kT_g[0:96, h, c0:c0 + n],
                                      lambda kt, h=h: v_aug[:, kt, h, 0:65], 64, qts, kts, 96 ** -0.5, Osb[hh], qT_g, kT_g, v_aug)
                        for qi, t in enumerate(qts):
                            k = 1 if t < cfg.NTC else 0
                            for hh in range(2):
                                fw.dve(lambda e, qi=qi, hh=hh: e.reciprocal(out=rr[:, 0:1], in_=Osb[hh][:, qi, 64:65]), r=[(Osb[hh], qi)], w=[rr])
                                fw.dve(lambda e, qi=qi, hh=hh: e.tensor_scalar(out=opair[:, hh * 64:(hh + 1) * 64], in0=Osb[hh][:, qi, 0:64],
                                                                              scalar1=rr[:, 0:1], scalar2=None, op0=ALU.mult),
                                       r=[(Osb[hh], qi), rr], w=[(opair, hh)])
                            transposes(b, opair, 1, lambda: oT[:], [opair], [oT])
                            out_proj_resid(b, li, t, oT, T(wout[j][:, pair:pair + 1, :], wout[j].name), 1, Gt[k], oT)


MIXERS[3] = mla


def mamba(b, li, all_tiles, out_tiles):
    fw, cfg, nc = b.fw, b.cfg, b.nc
    dr = b.dram
    NT, NTC = cfg.NT, cfg.NTC
    S_CTX, S_LAT = cfg.S_CTX, cfg.S_LAT
    w_in = dr["ssm_w_in"][0]
    if not hasattr(b, "yf_d"):
        b.yf_d = nc.dram_tensor("ssm_yf_scratch", [NT * 128, 512], F32, kind="Internal").ap()
        b.yg_d = nc.dram_tensor("ssm_yg_scratch", [NT * 128, 2048], BF16, kind="Internal").ap()
    yf_d, yg_d = b.yf_d, b.yg_d
    with b.scope():
        need_ctx = any(t < NTC for t in out_tiles)
        Gt = gate_tiles(b, li, 2, need_ctx)
        ssqg = b.sb("sm_ssqg", [128, NT, 4], F32)
        fw.pool(lambda e: e.memset(ssqg[:], 0.0), w=[ssqg])
        with b.scope():
            b.xnT = b.sb("xnT", [128, KC, NT * 128], BF16)
            norm_mod(b, li, 1, all_tiles)
            dt_all = b.sb("sm_dt", [128, NT, 64], F32)
            a_all = b.sb("sm_a", [128, NT, 64], F32)
            dskB = b.sb("sm_dsk", [128, 32], F32)
            with b.scope():
                Wdt = load_w(b, "sm_wdt", w_in[:, 5120:5184], KC, 64)
                rows = b.sb("sm_rows", [1, 160], F32)
                fw.dma("sync", rows[0:1, 0:64], dr["ssm_dt_bias"][0:1].rearrange("a d h -> a (d h)"), w=[(rows, 0)])
                fw.dma("sync", rows[0:1, 64:128], dr["ssm_a_log"][0:1].rearrange("a d h -> a (d h)"), w=[(rows, 1)])
                fw.dma("sync", rows[0:1, 128:160], dr["ssm_d"][0:1, :], w=[(rows, 2)])
                fw.act(lambda e: e.activation(out=rows[0:1, 64:128], in_=rows[0:1, 64:128], func=AF.Exp), r=[(rows, 1)], w=[(rows, 1)])
                fw.dve(lambda e: e.tensor_scalar(out=rows[0:1, 64:128], in0=rows[0:1, 64:128], scalar1=-1.0, scalar2=None, op0=ALU.mult),
                       r=[(rows, 1)], w=[(rows, 1)])
                cB = b.sb("sm_cB", [128, 160], F32)
                b.bcast_rows(cB, 0, (rows, rows), 160)
                fw.dve(lambda e: e.tensor_copy(out=dskB[:], in_=cB[:, 128:160]), r=[cB], w=[dskB])
                xr = b.sb("sm_xr", [128, 64], F32)
                ax = b.sb("sm_ax", [128, 64], F32)
                for t in all_tiles:
                    p = b.psum()
                    lin_tok(b, b.xnT, t, Wdt, 0, 64, p)
                    fw.dve(lambda e, p=p: e.tensor_tensor(out=xr[:], in0=p[:, 0:64], in1=cB[:, 0:64], op=ALU.add), r=[p, cB], w=[xr])
                    fw.act(lambda e: e.activation(out=ax[:], in_=xr[:], func=AF.Abs), r=[xr], w=[ax])
                    fw.act(lambda e: e.activation(out=ax[:], in_=ax[:], func=AF.Exp, scale=-1.0), r=[ax], w=[ax])
                    fw.act(lambda e: e.activation(out=ax[:], in_=ax[:], func=AF.Ln, bias=b.ones_f[:, 0:1], scale=1.0), r=[ax, b.ones_f], w=[ax])
                    fw.dve(lambda e, t=t: e.scalar_tensor_tensor(out=dt_all[:, t, :], in0=xr[:], scalar=0.0, in1=ax[:], op0=ALU.max, op1=ALU.add),
                           r=[xr, ax], w=[(dt_all, t)])
                    fw.pool(lambda e, t=t: e.tensor_tensor(out=a_all[:, t, :], in0=dt_all[:, t, :], in1=cB[:, 64:128], op=ALU.mult),
                            r=[(dt_all, t), cB], w=[(a_all, t)])
            for g in range(4):
                with b.scope():
                    _mamba_group(b, li, g, all_tiles, dt_all, a_all, dskB, ssqg, w_in, yf_d, yg_d)
        with b.scope():
            w_out = load_w(b, "sm_wout", dr["ssm_w_out"][0], 16, D)
            OG = row_bcast_tile(b, "sm_og", dr["ssm_out_g"][0:1, :], 2048)
            rstd = b.sb("sm_rstd", [128, NT], F32)
            fw.dve(lambda e: e.tensor_reduce(out=rstd[:], in_=ssqg[:], axis=AX.X, op=ALU.add), r=[ssqg], w=[rstd])
            rstd_op(b, rstd[:], rstd[:], 1.0 / 2048, [rstd], [rstd])
            ygt = [b.sb("sm_ygt%d" % j, [128, 2048], BF16) for j in range(2)]
            ygn = b.sb("sm_ygn", [128, 2048], BF16)
            ygT = [b.sb("sm_ygT%d" % j, [128, 16, 128], BF16) for j in range(2)]
            for j, t in enumerate(out_tiles):
                k = 1 if t < NTC else 0
                yt, yT = ygt[j % 2], ygT[j % 2]
                fw.dma("sync", yt[:], yg_d[t * 128:(t + 1) * 128, :], r=[("yg_d", t)], w=[yt])
                fw.dve(lambda e, yt=yt, t=t: e.scalar_tensor_tensor(out=ygn[:], in0=yt[:], scalar=rstd[:, t:t + 1], in1=OG[:],
                                                                  op0=ALU.mult, op1=ALU.mult), r=[yt, rstd, OG], w=[ygn])
                for hf in range(2):
                    transposes(b, T(ygn[:, hf * 1024:(hf + 1) * 1024], ygn.name), 8,
                               lambda yT=yT, hf=hf: yT[:, hf * 8:(hf + 1) * 8, :], [ygn], [(yT, hf)])
                out_proj_resid(b, li, t, yT, w_out, 16, Gt[k], yT)


def _mamba_group(b, li, g, all_tiles, dt_all, a_all, dskB, ssqg, w_in, yf_d, yg_d):
    fw, cfg = b.fw, b.cfg
    dr = b.dram
    NT, NTC = cfg.NT, cfg.NTC
    S_CTX, S_LAT = cfg.S_CTX, cfg.S_LAT
    NTOK = NT * 128
    xs_tok = b.sb("sm_xs", [128, NT, 512], BF16)
    BT = b.sb("sm_BT", [128, NTOK], BF16)
    CT = b.sb("sm_CT", [128, NTOK], BF16)
    B_tok = b.sb("sm_Btok", [128, NT, 128], BF16)
    with b.scope():
        XR = S_CTX + S_LAT + 8
        xrow = b.sb("sm_xrow", [128, XR], F32)
        acc = b.sb("sm_acc", [128, NTOK], F32)
        conv_o = b.sb("sm_convo", [128, NTOK], BF16)
        fw.pool(lambda e: e.memset(xrow[:], 0.0), w=[xrow])
        segs = [(0, S_CTX, 2), (S_CTX, S_LAT, S_CTX + 6)]
        Wcs = [b.sb("sm_Wc%d" % j, [128, KC, 128], BF16) for j in range(2)]
        cwrow = b.sb("sm_cwrow", [6, 128], F32)
        cw = b.sb("sm_cw", [128, 6], F32)
        chunks = [("xs", g * 512 + cc * 128, cc) for cc in range(4)] + [("B", 2048 + g * 128, 0), ("C", 2560 + g * 128, 0)]
        for ci, (kind, cch, cc) in enumerate(chunks):
            Wc = Wcs[ci % 2]
            fw.dma("gpsimd", Wc[:], w_in[:, 2048 + cch:2048 + cch + 128].rearrange("(kc p) f -> p kc f", p=128), w=[Wc])
            fw.dma("sync", cwrow[0:5, :], dr["ssm_conv_w"][0][:, cch:cch + 128], w=[(cwrow, 0)])
            fw.dma("sync", cwrow[5:6, :], dr["ssm_conv_b"][0:1, cch:cch + 128], w=[(cwrow, 1)])
            pc = b.psum()
            fw.pe(lambda e, pc=pc: e.matmul(pc[:, 0:6], lhsT=cwrow[:, :], rhs=b.identf[0:6, 0:6], start=True, stop=True),
                  r=[cwrow, b.identf], w=[pc])
            fw.act(lambda e, pc=pc: e.copy(out=cw[:], in_=pc[:, 0:6]), r=[pc], w=[cw])
            dst = conv_o if kind == "xs" else (BT if kind == "B" else CT)
            for (tok0, ln, xoff) in segs:
                for c0 in range(0, ln, 512):
                    n = min(512, ln - c0)
                    p = b.psum()
                    for kc in range(KC):
                        fw.pe(lambda e, p=p, kc=kc, Wc=Wc, a0=tok0 + c0, n=n: e.matmul(p[:, 0:n], lhsT=Wc[:, kc, :], rhs=b.xnT[:, kc, a0:a0 + n],
                                                                                     start=(kc == 0), stop=(kc == KC - 1)), r=[Wc, b.xnT], w=[p])
                    fw.act(lambda e, p=p, x0=xoff + c0, n=n: e.copy(out=xrow[:, x0:x0 + n], in_=p[:, 0:n]), r=[p], w=[xrow])
                fw.dve(lambda e, tok0=tok0, ln=ln, xoff=xoff: e.tensor_scalar(out=acc[:, tok0:tok0 + ln], in0=xrow[:, xoff - 2:xoff - 2 + ln],
                                                                             scalar1=cw[:, 0:1], scalar2=None, op0=ALU.mult),
                       r=[xrow, cw], w=[acc])
                for j in range(1, 5):
                    fw.dve(lambda e, j=j, tok0=tok0, ln=ln, xoff=xoff: e.scalar_tensor_tensor(
                        out=acc[:, tok0:tok0 + ln], in0=xrow[:, xoff - 2 + j:xoff - 2 + j + ln], scalar=cw[:, j:j + 1],
                        in1=acc[:, tok0:tok0 + ln], op0=ALU.mult, op1=ALU.add), r=[xrow, cw, acc], w=[acc])
                fw.act(lambda e, tok0=tok0, ln=ln, dst=dst: e.activation(out=dst[:, tok0:tok0 + ln], in_=acc[:, tok0:tok0 + ln], func=AF.Silu,
                                                                        bias=cw[:, 5:6], scale=1.0), r=[acc, cw], w=[dst])
            if kind == "xs":
                for t in all_tiles:
                    transposes(b, T(conv_o[:, t * 128:(t + 1) * 128], conv_o.name), 1,
                               lambda t=t, cc=cc: xs_tok[:, t:t + 1, cc * 128:(cc + 1) * 128], [conv_o], [(xs_tok, t)])
            elif kind == "B":
                for t in all_tiles:
                    transposes(b, T(BT[:, t * 128:(t + 1) * 128], BT.name), 1, lambda t=t: B_tok[:, t:t + 1, :], [BT], [(B_tok, t)])
    with b.scope():
        Wz = load_w(b, "sm_wz", w_in[:, g * 512:(g + 1) * 512], KC, 512)
        S = b.sb("sm_S", [128, 512], F32)
        S_bf = b.sb("sm_Sbf", [128, 512], BF16)
        CBm = b.sb("sm_CBm", [128, 128], F32)
        aU = b.sb("sm_aU", [128, 8, 128], F32)
        Ex = b.sb("sm_Ex", [128, 8, 128], F32)
        MT = b.sb("sm_MT", [128, 8, 128], BF16)
        xdt = b.sb("sm_xdt", [128, 512], BF16)
        xdtd = b.sb("sm_xdtd", [128, 512], BF16)
        ct = b.sb("sm_ct", [128, 24], F32)
        ex = b.sb("sm_ex", [128, 24], F32)
        ytmp = b.sb("sm_ytmp", [128, 512], F32)
        yb = b.sb("sm_yb", [128, 512], F32)
        yfl = b.sb("sm_yfl", [128, 512], F32)
        zs = b.sb("sm_zs", [128, 512], F32)
        ygb = b.sb("sm_ygb", [128, 512], BF16)
        ctx_t = list(range(NTC))
        lat_t = list(range(NTC, NT))
        for d in range(2):
            order = (ctx_t + lat_t) if d == 0 else (ctx_t[::-1] + lat_t[::-1])
            iU_r, iU_l = (0, 1) if d == 0 else (2, 3)
            c0 = d * 32 + g * 8
            fw.pool(lambda e: e.memset(S[:], 0.0), w=[S])
            fw.pool(lambda e: e.memset(S_bf[:], 0.0), w=[S_bf])
            for t in order:
                tsl = slice(t * 128, (t + 1) * 128)
                a8 = a_all[:, t, c0:c0 + 8]
                dt8 = dt_all[:, t, c0:c0 + 8]
                pct = b.psum()
                fw.pe(lambda e, pct=pct, a8=a8, iU_r=iU_r: e.matmul(pct[:, 0:8], lhsT=b.tri[:, iU_r, :], rhs=a8, start=True, stop=True),
                      r=[b.tri, (a_all, t)], w=[pct])
                fw.pe(lambda e, pct=pct, a8=a8: e.matmul(pct[:, 8:16], lhsT=b.ones_f[:, :], rhs=a8, start=True, stop=True),
                      r=[b.ones_f, (a_all, t)], w=[pct])
                fw.act(lambda e, pct=pct: e.copy(out=ct[:, 0:16], in_=pct[:, 0:16]), r=[pct], w=[ct])
                fw.dve(lambda e: e.tensor_tensor(out=ct[:, 16:24], in0=ct[:, 8:16], in1=ct[:, 0:8], op=ALU.subtract), r=[ct], w=[ct])
                fw.act(lambda e: e.activation(out=ex[:], in_=ct[:], func=AF.Exp), r=[ct], w=[ex])
                pcb = b.psum()
                fw.pe(lambda e, pcb=pcb, tsl=tsl: e.matmul(pcb[:, 0:128], lhsT=BT[:, tsl], rhs=CT[:, tsl], start=True, stop=True),
                      r=[BT, CT], w=[pcb])
                fw.dve(lambda e, pcb=pcb, iU_r=iU_r: e.tensor_tensor(out=CBm[:], in0=pcb[:, 0:128], in1=b.tri[:, iU_r, :], op=ALU.mult),
                       r=[pcb, b.tri], w=[CBm])
                fw.dve(lambda e, a8=a8, iU_r=iU_r: e.tensor_tensor(out=aU[:], in0=a8.unsqueeze(2).to_broadcast([128, 8, 128]),
                                                       in1=b.tri[:, iU_r, :].unsqueeze(1).to_broadcast([128, 8, 128]), op=ALU.mult),
                       r=[(a_all, t), b.tri], w=[aU])
                for hf in range(2):
                    pd = b.psum()
                    fw.pe(lambda e, pd=pd, hf=hf, iU_l=iU_l: e.matmul(pd[:, :], lhsT=b.tri[:, iU_l, :],
                                                          rhs=aU[:, hf * 4:(hf + 1) * 4, :].rearrange("p e t -> p (e t)"), start=True, stop=True),
                          r=[b.tri, aU], w=[pd])
                    fw.act(lambda e, pd=pd, hf=hf: e.activation(out=Ex[:, hf * 4:(hf + 1) * 4, :].rearrange("p e t -> p (e t)"), in_=pd[:, :], func=AF.Exp),
                           r=[pd], w=[(Ex, hf)])
                fw.dve(lambda e: e.tensor_tensor(out=MT[:], in0=Ex[:], in1=CBm[:].unsqueeze(1).to_broadcast([128, 8, 128]), op=ALU.mult),
                       r=[Ex, CBm], w=[MT])
                fw.pool(lambda e, t=t, dt8=dt8: e.tensor_tensor(out=xdt[:].rearrange("p (e q) -> p e q", e=8),
                                                               in0=xs_tok[:, t, :].rearrange("p (e q) -> p e q", e=8),
                                                               in1=dt8.unsqueeze(2).to_broadcast([128, 8, 64]), op=ALU.mult),
                        r=[(xs_tok, t), (dt_all, t)], w=[xdt])
                fw.pool(lambda e: e.tensor_tensor(out=xdtd[:].rearrange("p (e q) -> p e q", e=8), in0=xdt[:].rearrange("p (e q) -> p e q", e=8),
                                                  in1=ex[:, 16:24].unsqueeze(2).to_broadcast([128, 8, 64]), op=ALU.mult),
                        r=[xdt, ex], w=[xdtd])
                pyd = b.psum()
                for e8 in range(8):
                    fw.pe(lambda e, pyd=pyd, e8=e8: e.matmul(pyd[:, e8 * 64:(e8 + 1) * 64], lhsT=MT[:, e8, :], rhs=xdt[:, e8 * 64:(e8 + 1) * 64],
                                                            start=True, stop=True), r=[MT, xdt], w=[pyd])
                pyo = b.psum()
                fw.pe(lambda e, pyo=pyo, tsl=tsl: e.matmul(pyo[:, :], lhsT=CT[:, tsl], rhs=S_bf[:, :], start=True, stop=True),
                      r=[CT, S_bf], w=[pyo])
                fw.dve(lambda e, pyo=pyo: e.tensor_tensor(out=ytmp[:].rearrange("p (e q) -> p e q", e=8), in0=pyo[:, :].rearrange("p (e q) -> p e q", e=8),
                                                         in1=ex[:, 0:8].unsqueeze(2).to_broadcast([128, 8, 64]), op=ALU.mult),
                       r=[pyo, ex], w=[ytmp])
                fw.dve(lambda e, pyd=pyd: e.tensor_tensor(out=yb[:], in0=pyd[:, :], in1=ytmp[:], op=ALU.add), r=[pyd, ytmp], w=[yb])
                psn = b.psum()
                fw.pe(lambda e, psn=psn, t=t: e.matmul(psn[:, :], lhsT=B_tok[:, t, :], rhs=xdtd[:, :], start=True, stop=True),
                      r=[(B_tok, t), xdtd], w=[psn])
                fw.dve(lambda e: e.tensor_tensor(out=S[:].rearrange("p (e q) -> p e q", e=8), in0=S[:].rearrange("p (e q) -> p e q", e=8),
                                                 in1=ex[:, 8:16].unsqueeze(2).to_broadcast([128, 8, 64]), op=ALU.mult), r=[S, ex], w=[S])
                fw.dve(lambda e, psn=psn: e.tensor_tensor(out=S[:], in0=psn[:, :], in1=S[:], op=ALU.add), r=[psn, S], w=[S])
                fw.act(lambda e: e.copy(out=S_bf[:], in_=S[:]), r=[S], w=[S_bf])
                if d == 0:
                    fw.dma("sync", yf_d[tsl, :], yb[:], r=[yb], w=[("yf_d", t)])
                else:
                    fw.dma("sync", yfl[:], yf_d[tsl, :], r=[("yf_d", t)], w=[yfl])
                    fw.pool(lambda e: e.tensor_tensor(out=yb[:], in0=yb[:], in1=yfl[:], op=ALU.add), r=[yb, yfl], w=[yb])
                    fw.dve(lambda e, t=t: e.tensor_tensor(out=ytmp[:].rearrange("p (e q) -> p e q", e=8),
                                                         in0=xs_tok[:, t, :].rearrange("p (e q) -> p e q", e=8),
                                                         in1=dskB[:, g * 8:(g + 1) * 8].unsqueeze(2).to_broadcast([128, 8, 64]), op=ALU.mult),
                           r=[(xs_tok, t), dskB], w=[ytmp])
                    fw.pool(lambda e: e.tensor_tensor(out=yb[:], in0=yb[:], in1=ytmp[:], op=ALU.add), r=[yb, ytmp], w=[yb])
                    pz = b.psum()
                    lin_tok(b, b.xnT, t, Wz, 0, 512, pz)
                    fw.act(lambda e, pz=pz: e.activation(out=zs[:], in_=pz[:, :], func=AF.Silu), r=[pz], w=[zs])
                    fw.dve(lambda e: e.tensor_tensor(out=yb[:], in0=yb[:], in1=zs[:], op=ALU.mult), r=[yb, zs], w=[yb])
                    fw.act(lambda e, t=t: e.activation(out=ygb[:], in_=yb[:], func=AF.Square, accum_out=ssqg[:, t, g:g + 1]),
                           r=[yb], w=[ygb, (ssqg, t)])
                    fw.pool(lambda e: e.tensor_copy(out=ygb[:], in_=yb[:]), r=[yb], w=[ygb])
                    fw.dma("sync", yg_d[tsl, g * 512:(g + 1) * 512], ygb[:], r=[ygb], w=[("yg_d", t)])


MIXERS[2] = mamba


def _host_consts(cfg):
    ident = np.eye(128, dtype=np.float32)
    k = np.arange(128)[:, None]
    t = np.arange(128)[None, :]
    tri = np.stack([(k <= t), (k > t), (k >= t), (k < t)]).astype(np.float32)
    out = {"c_ident": ident, "c_tri": tri}
    out.update(host_tables(cfg))
    return out


def kernel(**inputs):
    from concourse.bass_utils import run_bass_kernel_spmd
    inp = {k: np.ascontiguousarray(np.asarray(v)) for k, v in inputs.items()}
    n_cores = 8
    NB = inp["x"].shape[0] // n_cores
    cfg = Cfg(S_LAT=inp["x"].shape[1], S_CTX=inp["ctx"].shape[1], NB=NB, kinds=[0, 1, 2, 3])
    wnames = [k for k in inp if k not in ("x", "c", "ctx", "c_ctx")]
    cfg.wshapes = {k: inp[k].shape for k in wnames}
    nc, b = build_program(cfg)
    consts = _host_consts(cfg)
    maps = []
    for core in range(n_cores):
        m = {k: inp[k] for k in wnames if k in b.dram}
        m.update({k: v for k, v in consts.items() if k in b.dram})
        m["x"] = inp["x"][core * NB:(core + 1) * NB]
        m["ctx"] = inp["ctx"][core * NB:(core + 1) * NB]
        m["c"] = inp["c"][core * NB:(core + 1) * NB]
        m["c_ctx"] = inp["c_ctx"][None, :]
        maps.append(m)
    res = run_bass_kernel_spmd(nc, maps, core_ids=list(range(n_cores)))
    return np.concatenate([r["out"] for r in res.results], axis=0).astype(np.float32)
```

```python
import os
import numpy as np
import concourse.bass as bass
import concourse.mybir as mybir

F32 = mybir.dt.float32
BF16 = mybir.dt.bfloat16
AF = mybir.ActivationFunctionType
ALU = mybir.AluOpType
AX = mybir.AxisListType

ENGS = ("tensor", "vector", "scalar", "gpsimd", "sync")
N_DMA_SEMS = 8


class _Op:
    __slots__ = ("eng", "fn", "deps", "chan", "is_dma", "needs_inc", "val", "idx")


class FW:
    def __init__(self, nc, same_engine_sync=True):
        self.nc = nc
        self.ops = []
        self.state = {}
        self.same_engine_sync = same_engine_sync
        self.dma_rr = {e: 0 for e in ENGS}
        self.chan_last = {}
        self.bar = {}

    def barrier(self):
        self.bar = dict(self.chan_last)

    def _entries(self, res):
        name, idx = res if isinstance(res, tuple) else (res, None)
        name = getattr(name, "name", name)
        ent = self.state.setdefault(name, {})
        return name, idx, ent

    def _collect(self, res, is_write, deps):
        name, idx, ent = self._entries(res)
        if idx is None:
            targets = list(ent.values())
        else:
            targets = [ent.get(idx), ent.get(None)]
        for e in targets:
            if e is None:
                continue
            if e[0] is not None:
                deps.add(e[0])
            if is_write:
                deps.update(e[1].values())

    def _update(self, res, is_write, op):
        name, idx, ent = self._entries(res)
        if is_write:
            if idx is None:
                ent.clear()
                ent[None] = [op.idx, {}]
            else:
                ent[idx] = [op.idx, {}]
        else:
            e = ent.get(idx)
            if e is None:
                e = ent[idx] = [None, {}]
            e[1][op.chan] = op.idx

    def op(self, eng, fn, r=(), w=(), dma=False):
        o = _Op()
        o.idx = len(self.ops)
        o.eng = eng
        o.fn = fn
        o.is_dma = dma
        if dma:
            k = self.dma_rr[eng]
            self.dma_rr[eng] = (k + 1) % N_DMA_SEMS
            o.chan = "dma_%s_%d" % (eng, k)
        else:
            o.chan = eng
        o.needs_inc = dma
        o.val = None
        deps = set()
        for res in r:
            self._collect(res, False, deps)
        for res in w:
            self._collect(res, True, deps)
        best = {}
        for d in deps:
            c = self.ops[d].chan
            if c not in best or best[c] < d:
                best[c] = d
        for c, d in self.bar.items():
            if c not in best or best[c] < d:
                best[c] = d
        if dma and o.chan in self.chan_last:
            d = self.chan_last[o.chan]
            if o.chan not in best or best[o.chan] < d:
                best[o.chan] = d
        o.deps = best
        self.chan_last[o.chan] = o.idx
        for res in r:
            self._update(res, False, o)
        for res in w:
            self._update(res, True, o)
        self.ops.append(o)
        return o

    def pe(self, fn, r=(), w=()):
        return self.op("tensor", fn, r, w)

    def dve(self, fn, r=(), w=()):
        return self.op("vector", fn, r, w)

    def act(self, fn, r=(), w=()):
        return self.op("scalar", fn, r, w)

    def pool(self, fn, r=(), w=()):
        return self.op("gpsimd", fn, r, w)

    def dma(self, eng, out, in_, r=(), w=(), **kw):
        return self.op(eng, lambda e: e.dma_start(out=out, in_=in_, **kw), r, w, dma=True)

    def _init_emit(self):
        import contextlib
        self._st = contextlib.ExitStack()
        chans = list(ENGS[:4]) + ["dma_%s_%d" % (e, k) for e in ("sync", "scalar", "gpsimd") for k in range(N_DMA_SEMS)]
        self.sems = {c: self._st.enter_context(self.nc.semaphore("s_" + c)) for c in chans}
        self.cnt = {c: 0 for c in chans}
        self.inc_idx = {c: [] for c in chans}
        self.inc_val = {c: [] for c in chans}
        self.waited = {e: {} for e in ENGS}
        self.flushed = 0

    def _skip_same(self, o, p):
        return p.chan == o.eng and not p.is_dma and (o.eng == "tensor" or not self.same_engine_sync)

    def flush(self):
        import bisect
        if not hasattr(self, "sems"):
            self._init_emit()
        ops = self.ops
        batch = ops[self.flushed:]
        if not batch:
            return
        start = self.flushed
        last_in_chan = {}
        for o in batch:
            last_in_chan[o.chan] = o
            for c, d in o.deps.items():
                p = ops[d]
                if d >= start and not self._skip_same(o, p):
                    p.needs_inc = True
        for o in last_in_chan.values():
            o.needs_inc = True
        for o in batch:
            if o.needs_inc:
                self.cnt[o.chan] += 16 if o.is_dma else 1
                o.val = self.cnt[o.chan]
                self.inc_idx[o.chan].append(o.idx)
                self.inc_val[o.chan].append(o.val)
        for o in batch:
            eng = getattr(self.nc, o.eng)
            waited = self.waited[o.eng]
            for c, d in o.deps.items():
                p = ops[d]
                if self._skip_same(o, p):
                    continue
                k = bisect.bisect_left(self.inc_idx[p.chan], d)
                val = self.inc_val[p.chan][k]
                if waited.get(p.chan, 0) >= val:
                    continue
                eng.wait_ge(self.sems[p.chan], val)
                waited[p.chan] = val
            ins = o.fn(eng)
            if o.needs_inc:
                ins.then_inc(self.sems[o.chan], 16 if o.is_dma else 1)
            o.fn = None
        self.flushed = len(ops)

    def emit(self, final_wait_ops=()):
        self.flush()
        eng = self.nc.sync
        for p in final_wait_ops:
            eng.wait_ge(self.sems[p.chan], p.val)
        self.sem_max = dict(self.cnt)
        self._st.close()


import contextlib
import math
import numpy as np
import concourse.bass as bass
import concourse.mybir as mybir

D = 1024
KC = 8
EPS = 1e-6


class T:
    def __init__(self, h, name):
        self.h = h
        self.name = name

    def __getitem__(self, k):
        return self.h[k]


class Cfg:
    def __init__(self, **kw):
        self.S_LAT = 2048
        self.S_CTX = 256
        self.NB = 2
        self.GROUPS = 4
        self.PER = 8
        self.kinds = [0, 1, 2, 3]
        self.want_ctx = [True, True, True, False]
        self.layer_ids = [0, 1, 2, 3]
        self.moe = True
        self.__dict__.update(kw)
        self.NTC = self.S_CTX // 128
        self.NTL = self.S_LAT // 128
        self.NT = self.NTC + self.NTL
        self.E = self.GROUPS * self.PER
        self.DEPTH = len(self.kinds)


class B:
    def __init__(self, nc, cfg):
        self.nc = nc
        self.cfg = cfg
        self.fw = FW(nc)
        self.uid = 0
        self.stacks = []
        self.dram = {}
        self.ps_rr = 0

    @contextlib.contextmanager
    def scope(self):
        st = contextlib.ExitStack()
        self.stacks.append(st)
        try:
            with st:
                yield
                self.fw.flush()
        finally:
            self.fw.flush()
            self.stacks.pop()
            self.fw.barrier()

    def sb(self, name, shape, dt):
        self.uid += 1
        nm = "%s_%d" % (name, self.uid)
        h = self.stacks[-1].enter_context(self.nc.sbuf_tensor(nm, list(shape), dt))
        return T(h, nm)

    def din(self, name, shape, dt=F32):
        ap = self.nc.dram_tensor(name, list(shape), dt, kind="ExternalInput").ap()
        self.dram[name] = ap
        return ap

    def psum(self):
        p = self.ps[self.ps_rr]
        self.ps_rr = (self.ps_rr + 1) % len(self.ps)
        return p

    def psumb(self):
        p = self.psb[self.psb_rr]
        self.psb_rr = (self.psb_rr + 1) % len(self.psb)
        return p

    def bcast_rows(self, dst, dst_cols, row_ap_f32, n, evac=None):
        fw = self.fw
        for c0 in range(0, n, 512):
            cw = min(512, n - c0)
            p = self.psum()
            fw.pe(lambda e, p=p, c0=c0, cw=cw: e.matmul(p[:, 0:cw], lhsT=self.ones_row[0:1, 0:128],
                                                      rhs=row_ap_f32[1][0:1, c0:c0 + cw], start=True, stop=True),
                  r=[self.ones_row, row_ap_f32[0]], w=[p])
            d0 = dst_cols + c0
            if evac is None:
                fw.act(lambda e, p=p, d0=d0, cw=cw: e.copy(out=dst[:, d0:d0 + cw], in_=p[:, 0:cw]), r=[p], w=[dst])
            else:
                evac(p, d0, cw)


def rstd_op(b, out, in_, scale, r, w):
    fw = b.fw
    fw.act(lambda e: e.activation(out=out, in_=in_, func=AF.Sqrt, bias=b.eps_col[0:out.shape[0], 0:1], scale=scale),
           r=list(r) + [b.eps_col], w=w)
    fw.dve(lambda e: e.reciprocal(out=out, in_=out), r=w, w=w)


def build_consts(b):
    nc, fw = b.nc, b.fw
    cfg = b.cfg
    ident_d = b.din("c_ident", [128, 128])
    tri_d = b.din("c_tri", [4, 128, 128])
    b.din("c_rope64", [cfg.S_CTX + cfg.S_LAT, 64])
    b.din("c_rope32", [cfg.S_CTX + cfg.S_LAT, 32])
    b.identb = b.sb("identb", [128, 128], BF16)
    b.identf = b.sb("identf", [128, 128], F32)
    b.ones_row = b.sb("ones_row", [1, 512], F32)
    b.ones_f = b.sb("ones_f", [128, 128], F32)
    b.tri = b.sb("tri", [128, 4, 128], F32)
    fw.dma("gpsimd", b.identb[:], ident_d, w=[b.identb])
    fw.dma("sync", b.identf[:], ident_d, w=[b.identf])
    fw.dma("sync", b.tri[:], tri_d.rearrange("a p q -> p a q"), w=[b.tri])
    fw.pool(lambda e: e.memset(b.ones_row[:], 1.0), w=[b.ones_row])
    fw.pool(lambda e: e.memset(b.ones_f[:], 1.0), w=[b.ones_f])
    b.eps_col = b.sb("eps_col", [128, 1], F32)
    fw.pool(lambda e: e.memset(b.eps_col[:], EPS), w=[b.eps_col])
    st = b.stacks[-1]
    b.ps = []
    for j in range(6):
        h = st.enter_context(nc.psum_tensor("psf%d" % j, [128, 512], F32))
        b.ps.append(T(h, "psf%d" % j))
    b.psb = []
    for j in range(2):
        h = st.enter_context(nc.psum_tensor("psb%d" % j, [128, 1024], BF16))
        b.psb.append(T(h, "psb%d" % j))
    b.psb_rr = 0


def load_cond(b, bi):
    with b.scope():
        _load_cond(b, bi)


def _load_cond(b, bi):
    fw = b.fw
    c_d, cctx_d = b.dram["c"], b.dram["c_ctx"]
    crow = b.sb("crow", [1, 2 * D], F32)
    fw.dma("sync", crow[0:1, 0:D], c_d[bi:bi + 1, :], w=[(crow, 0)])
    fw.dma("sync", crow[0:1, D:2 * D], cctx_d[0:1, :], w=[(crow, 1)])
    p = b.psum()
    for k in range(2):
        for kc in range(KC):
            fw.pe(lambda e, k=k, kc=kc: e.matmul(p[:, k * KC + kc:k * KC + kc + 1],
                                                lhsT=crow[0:1, k * D + kc * 128:k * D + (kc + 1) * 128],
                                                rhs=b.ones_row[0:1, 0:1], start=True, stop=True),
                  r=[crow, b.ones_row], w=[p])
    ccol = b.sb("ccol", [128, 2 * KC], F32)
    fw.act(lambda e: e.activation(out=ccol[:], in_=p[:, 0:2 * KC], func=AF.Silu), r=[p], w=[ccol])
    for k in range(2):
        fw.dve(lambda e, k=k: e.tensor_copy(out=b.scB[k][:],
                                           in_=ccol[:, k * KC:(k + 1) * KC].unsqueeze(2).to_broadcast([128, KC, 128])),
               r=[ccol], w=[b.scB[k]])


def mod_bcast(b, li, sec, dsts, add_one=False):
    fw = b.fw
    ada_w, ada_b = b.dram["ada_w"], b.dram["ada_b"]
    with b.scope():
        _mod_bcast(b, li, sec, dsts, add_one)


def _mod_bcast(b, li, sec, dsts, add_one):
    fw = b.fw
    ada_w, ada_b = b.dram["ada_w"], b.dram["ada_b"]
    brow = b.sb("brow", [1, D], F32)
    QW = 256
    wsts = [b.sb("wst%d" % j, [128, KC, QW], F32) for j in range(2)]
    fw.dma("sync", brow[:], ada_b[li:li + 1, sec * D:(sec + 1) * D], w=[brow])
    for q in range(D // QW):
        wst = wsts[q % 2]
        c0 = sec * D + q * QW
        fw.dma("sync", wst[:], ada_w[li, :, c0:c0 + QW].rearrange("(kc p) f -> p kc f", p=128), w=[wst])
        for k in range(2):
            if dsts[k] is None:
                continue
            p = b.psum()
            for kc in range(KC):
                fw.pe(lambda e, p=p, k=k, kc=kc, wst=wst: e.matmul(p[:, 0:QW], lhsT=b.scB[k][:, kc, :], rhs=wst[:, kc, :],
                                                                  start=(kc == 0), stop=False),
                      r=[b.scB[k], wst], w=[p])
            fw.pe(lambda e, p=p, q=q: e.matmul(p[:, 0:QW], lhsT=b.ones_row[0:1, 0:128], rhs=brow[0:1, q * QW:(q + 1) * QW],
                                              start=False, stop=True),
                  r=[b.ones_row, brow], w=[p])
            d = dsts[k]
            if add_one:
                fw.act(lambda e, p=p, d=d, q=q: e.activation(out=d[:, q * QW:(q + 1) * QW], in_=p[:, 0:QW],
                                                            func=AF.Identity, bias=b.one_col[:, 0:1], scale=1.0),
                       r=[p, b.one_col], w=[(d, q)])
            else:
                fw.act(lambda e, p=p, d=d, q=q: e.copy(out=d[:, q * QW:(q + 1) * QW], in_=p[:, 0:QW]),
                       r=[p], w=[(d, q)])


def norm_mod(b, li, which, tiles):
    fw, cfg = b.fw, b.cfg
    gname = "norm1_g" if which == 1 else "norm2_g"
    with b.scope():
        A = [b.sb("A%d" % k, [128, D], F32) for k in range(2)]
        S = [b.sb("S%d" % k, [128, D], F32) for k in range(2)]
        G = b.sb("Gn", [128, D], F32)
        grow = b.sb("grow", [1, D], F32)
        need_ctx = any(t < cfg.NTC for t in tiles)
        sec0 = 0 if which == 1 else 3
        fw.dma("sync", grow[:], b.dram[gname][li:li + 1, :], w=[grow])
        b.bcast_rows(G, 0, (grow, grow), D)
        dA = [A[0], A[1] if need_ctx else None]
        dS = [S[0], S[1] if need_ctx else None]
        mod_bcast(b, li, sec0 + 1, dA, add_one=True)
        mod_bcast(b, li, sec0 + 0, dS)
        for k in range(2):
            if dA[k] is not None:
                fw.dve(lambda e, k=k: e.tensor_mul(out=A[k][:], in0=A[k][:], in1=G[:]), r=[A[k], G], w=[A[k]])
        junk = b.sb("junk", [128, D], BF16)
        ss = b.sb("ss", [128, cfg.NT], F32)
        rstd = b.sb("rstd", [128, cfg.NT], F32)
        fw.pool(lambda e: e.memset(ss[:], 0.0), w=[ss])
        tmp = [b.sb("nt%d" % j, [128, D], F32) for j in range(2)]
        xnb = [b.sb("xnb%d" % j, [128, D], BF16) for j in range(2)]
        for j, t in enumerate(tiles):
            k = 1 if t < cfg.NTC else 0
            fw.act(lambda e, t=t: e.activation(out=junk[:], in_=b.h[:, t, :], func=AF.Square, accum_out=ss[:, t:t + 1]),
                   r=[(b.h, t)], w=[junk, (ss, t)])
            rstd_op(b, rstd[:, t:t + 1], ss[:, t:t + 1], 1.0 / D, [(ss, t)], [(rstd, t)])
            tm, xb = tmp[j % 2], xnb[j % 2]
            fw.dve(lambda e, t=t, tm=tm, k=k: e.scalar_tensor_tensor(out=tm[:], in0=b.h[:, t, :], scalar=rstd[:, t:t + 1],
                                                                    in1=A[k][:], op0=ALU.mult, op1=ALU.mult),
                   r=[(b.h, t), (rstd, t), A[k]], w=[tm])
            fw.pool(lambda e, tm=tm, xb=xb, k=k: e.tensor_tensor(out=xb[:], in0=tm[:], in1=S[k][:], op=ALU.add),
                    r=[tm, S[k]], w=[xb])
            pb = b.psumb()
            for kc in range(KC):
                fw.pe(lambda e, pb=pb, kc=kc, xb=xb: e.transpose(out=pb[:, kc * 128:(kc + 1) * 128],
                                                                in_=xb[:, kc * 128:(kc + 1) * 128], identity=b.identb[:]),
                      r=[xb, b.identb], w=[pb])
            fw.act(lambda e, pb=pb, t=t: e.copy(out=b.xnT[:, :, t * 128:(t + 1) * 128],
                                               in_=pb[:, :].rearrange("p (k q) -> p k q", k=KC)),
                   r=[pb], w=[(b.xnT, t)])


def gate_tiles(b, li, sec, need_ctx):
    Gt = [b.sb("Gt0", [128, D], F32), b.sb("Gt1", [128, D], F32) if need_ctx else None]
    mod_bcast(b, li, sec, Gt)
    return Gt


def resid_add(b, t, p, hf, Gk):
    fw = b.fw
    tm = b.rtmp[b.rtmp_rr]
    b.rtmp_rr = (b.rtmp_rr + 1) % len(b.rtmp)
    sl = slice(hf * 512, (hf + 1) * 512)
    fw.dve(lambda e: e.tensor_tensor(out=tm[:], in0=p[:, :], in1=Gk[:, sl], op=ALU.mult), r=[p, Gk], w=[tm])
    fw.pool(lambda e: e.tensor_tensor(out=b.h[:, t, sl], in0=b.h[:, t, sl], in1=tm[:], op=ALU.add),
            r=[tm, (b.h, t)], w=[(b.h, t)])


def moe(b, li, tiles):
    fw, cfg = b.fw, b.cfg
    E, NT = cfg.E, cfg.NT
    NG, PER = cfg.GROUPS, cfg.PER
    NL = NG + E
    need_ctx = any(t < cfg.NTC for t in tiles)
    with b.scope():
        Gt = gate_tiles(b, li, 5, need_ctx)
        gates = b.sb("gates", [128, NT, E], F32)
        with b.scope():
            _router(b, li, tiles, gates)
        _experts(b, li, tiles, gates, Gt)


def _router(b, li, tiles, gates):
    fw, cfg = b.fw, b.cfg
    E, NT = cfg.E, cfg.NT
    NG, PER = cfg.GROUPS, cfg.PER
    NL = NG + E
    if True:
        wgr = b.sb("wgr", [128, KC, NL], BF16)
        brow = b.sb("brow_r", [1, NL], F32)
        fw.dma("gpsimd", wgr[:, :, 0:NG], b.dram["moe_w_group"][li].rearrange("(kc p) f -> p kc f", p=128), w=[(wgr, 0)])
        fw.dma("gpsimd", wgr[:, :, NG:NL], b.dram["moe_w_router"][li].rearrange("(kc p) f -> p kc f", p=128), w=[(wgr, 1)])
        fw.dma("sync", brow[0:1, 0:NG], b.dram["moe_b_group"][li:li + 1, :], w=[(brow, 0)])
        fw.dma("sync", brow[0:1, NG:NL], b.dram["moe_b_router"][li:li + 1, :], w=[(brow, 1)])
        L = b.sb("L", [128, NT, NL], F32)
        fw.pool(lambda e: e.memset(L[:], 0.0), w=[L])
        for t in tiles:
            p = b.psum()
            for kc in range(KC):
                fw.pe(lambda e, p=p, kc=kc, t=t: e.matmul(p[:, 0:NL], lhsT=b.xnT[:, kc, t * 128:(t + 1) * 128], rhs=wgr[:, kc, :],
                                                         start=(kc == 0), stop=False), r=[(b.xnT, t), wgr], w=[p])
            fw.pe(lambda e, p=p: e.matmul(p[:, 0:NL], lhsT=b.ones_row[0:1, 0:128], rhs=brow[0:1, :], start=False, stop=True),
                  r=[b.ones_row, brow], w=[p])
            fw.act(lambda e, p=p, t=t: e.copy(out=L[:, t, :], in_=p[:, 0:NL]), r=[p], w=[L])
        def v(name, shape):
            return b.sb(name, shape, F32)
        gmax = v("gmax", [128, NT]); eg = v("eg", [128, NT, NG]); gsum = v("gsum", [128, NT]); pg = v("pg", [128, NT])
        goh = v("goh", [128, NT, NG]); Lm = v("Lm", [128, NT, E]); m1 = v("m1", [128, NT]); oh1 = v("oh1", [128, NT, E])
        m2 = v("m2", [128, NT]); oh2 = v("oh2", [128, NT, E]); w1 = v("w1", [128, NT]); w2 = v("w2", [128, NT])
        pen = v("pen", [128, NT, NG])
        Lg = L[:, :, 0:NG]
        Le = L[:, :, NG:NL]
        dv = fw.dve
        dv(lambda e: e.tensor_reduce(out=gmax[:], in_=Lg, axis=AX.X, op=ALU.max), r=[L], w=[gmax])
        dv(lambda e: e.tensor_tensor(out=eg[:], in0=Lg, in1=gmax[:].unsqueeze(2).to_broadcast([128, NT, NG]), op=ALU.subtract),
           r=[L, gmax], w=[eg])
        dv(lambda e: e.tensor_tensor(out=goh[:], in0=Lg, in1=gmax[:].unsqueeze(2).to_broadcast([128, NT, NG]), op=ALU.is_equal),
           r=[L, gmax], w=[goh])
        fw.act(lambda e: e.activation(out=eg[:], in_=eg[:], func=AF.Exp), r=[eg], w=[eg])
        dv(lambda e: e.tensor_reduce(out=gsum[:], in_=eg[:], axis=AX.X, op=ALU.add), r=[eg], w=[gsum])
        dv(lambda e: e.reciprocal(out=pg[:], in_=gsum[:]), r=[gsum], w=[pg])
        dv(lambda e: e.tensor_scalar(out=pen[:], in0=goh[:], scalar1=-1.0, scalar2=1e30, op0=ALU.add, op1=ALU.mult),
           r=[goh], w=[pen])
        dv(lambda e: e.tensor_tensor(out=Lm[:].rearrange("p t (g e) -> p t g e", g=NG),
                                     in0=Le.rearrange("p t (g e) -> p t g e", g=NG),
                                     in1=pen[:].unsqueeze(3).to_broadcast([128, NT, NG, PER]), op=ALU.add),
           r=[L, pen], w=[Lm])
        dv(lambda e: e.tensor_reduce(out=m1[:], in_=Lm[:], axis=AX.X, op=ALU.max), r=[Lm], w=[m1])
        dv(lambda e: e.tensor_tensor(out=oh1[:], in0=Lm[:], in1=m1[:].unsqueeze(2).to_broadcast([128, NT, E]), op=ALU.is_equal),
           r=[Lm, m1], w=[oh1])
        dv(lambda e: e.scalar_tensor_tensor(out=Lm[:], in0=oh1[:], scalar=-1e30, in1=Lm[:], op0=ALU.mult, op1=ALU.add),
           r=[oh1, Lm], w=[Lm])
        dv(lambda e: e.tensor_reduce(out=m2[:], in_=Lm[:], axis=AX.X, op=ALU.max), r=[Lm], w=[m2])
        dv(lambda e: e.tensor_tensor(out=oh2[:], in0=Lm[:], in1=m2[:].unsqueeze(2).to_broadcast([128, NT, E]), op=ALU.is_equal),
           r=[Lm, m2], w=[oh2])
        dv(lambda e: e.tensor_tensor(out=w2[:], in0=m2[:], in1=m1[:], op=ALU.subtract), r=[m1, m2], w=[w2])
        fw.act(lambda e: e.activation(out=w2[:], in_=w2[:], func=AF.Sigmoid), r=[w2], w=[w2])
        dv(lambda e: e.tensor_scalar(out=w1[:], in0=w2[:], scalar1=-1.0, scalar2=1.0, op0=ALU.mult, op1=ALU.add), r=[w2], w=[w1])
        dv(lambda e: e.tensor_mul(out=w1[:], in0=w1[:], in1=pg[:]), r=[w1, pg], w=[w1])
        dv(lambda e: e.tensor_mul(out=w2[:], in0=w2[:], in1=pg[:]), r=[w2, pg], w=[w2])
        dv(lambda e: e.tensor_tensor(out=oh1[:], in0=oh1[:], in1=w1[:].unsqueeze(2).to_broadcast([128, NT, E]), op=ALU.mult),
           r=[oh1, w1], w=[oh1])
        dv(lambda e: e.tensor_tensor(out=oh2[:], in0=oh2[:], in1=w2[:].unsqueeze(2).to_broadcast([128, NT, E]), op=ALU.mult),
           r=[oh2, w2], w=[oh2])
        dv(lambda e: e.tensor_add(out=gates[:], in0=oh1[:], in1=oh2[:]), r=[oh1, oh2], w=[gates])


def _experts(b, li, tiles, gates, Gt):
    fw, cfg = b.fw, b.cfg
    E, NT = cfg.E, cfg.NT
    if True:
        FF = 512
        w1b = [b.sb("w1b%d" % j, [128, KC, FF], BF16) for j in range(2)]
        w3b = [b.sb("w3b%d" % j, [128, KC, FF], BF16) for j in range(2)]
        w2b = [b.sb("w2b%d" % j, [128, 4, D], BF16) for j in range(2)]
        hid = [b.sb("hid%d" % j, [128, 4, 512], BF16) for j in range(2)]
        s1 = [b.sb("s1_%d" % j, [128, 512], BF16) for j in range(2)]
        mtmp = [b.sb("mtmp%d" % j, [128, 512], F32) for j in range(2)]
        blocks = []
        i0 = 0
        while i0 < len(tiles):
            blocks.append(tiles[i0:i0 + 4])
            i0 += 4
        def load_expert(ex):
            j = ex % 2
            fw.dma("gpsimd", w1b[j][:], b.dram["moe_w1"][li, ex].rearrange("(kc p) f -> p kc f", p=128), w=[w1b[j]])
            fw.dma("gpsimd", w3b[j][:], b.dram["moe_w3"][li, ex].rearrange("(kc p) f -> p kc f", p=128), w=[w3b[j]])
            fw.dma("gpsimd", w2b[j][:], b.dram["moe_w2"][li, ex].rearrange("(kc p) f -> p kc f", p=128), w=[w2b[j]])

        def up(ex, blk, hd):
            j = ex % 2
            nt = len(blk) * 128
            c0 = blk[0] * 128
            for fc in range(4):
                p1 = b.psum()
                p3 = b.psum()
                for kc in range(KC):
                    fw.pe(lambda e, p1=p1, kc=kc, fc=fc: e.matmul(
                        p1[:, 0:nt], lhsT=w1b[j][:, kc, fc * 128:(fc + 1) * 128], rhs=b.xnT[:, kc, c0:c0 + nt],
                        start=(kc == 0), stop=(kc == KC - 1)), r=[w1b[j], b.xnT], w=[p1])
                for kc in range(KC):
                    fw.pe(lambda e, p3=p3, kc=kc, fc=fc: e.matmul(
                        p3[:, 0:nt], lhsT=w3b[j][:, kc, fc * 128:(fc + 1) * 128], rhs=b.xnT[:, kc, c0:c0 + nt],
                        start=(kc == 0), stop=(kc == KC - 1)), r=[w3b[j], b.xnT], w=[p3])
                sj = s1[fc % 2]
                fw.act(lambda e, p1=p1, sj=sj: e.activation(out=sj[:, 0:nt], in_=p1[:, 0:nt], func=AF.Silu), r=[p1], w=[sj])
                fw.dve(lambda e, p3=p3, sj=sj, fc=fc: e.tensor_tensor(
                    out=hd[:, fc, 0:nt], in0=p3[:, 0:nt], in1=sj[:, 0:nt], op=ALU.mult), r=[p3, sj], w=[(hd, fc)])

        def down(ex, blk, hd):
            j = ex % 2
            for ti, t in enumerate(blk):
                k = 1 if t < cfg.NTC else 0
                for hf in range(2):
                    py = b.psum()
                    for fc in range(4):
                        fw.pe(lambda e, py=py, fc=fc, ti=ti, hf=hf: e.matmul(
                            py[:, :], lhsT=hd[:, fc, ti * 128:(ti + 1) * 128], rhs=w2b[j][:, fc, hf * 512:(hf + 1) * 512],
                            start=(fc == 0), stop=(fc == 3)), r=[hd, w2b[j]], w=[py])
                    tm = mtmp[(ti * 2 + hf) % 2]
                    sl = slice(hf * 512, (hf + 1) * 512)
                    fw.dve(lambda e, py=py, tm=tm, t=t, k=k, sl=sl: e.scalar_tensor_tensor(
                        out=tm[:], in0=py[:, :], scalar=gates[:, t, ex:ex + 1], in1=Gt[k][:, sl], op0=ALU.mult, op1=ALU.mult),
                        r=[py, gates, Gt[k]], w=[tm])
                    fw.pool(lambda e, tm=tm, t=t, sl=sl: e.tensor_tensor(out=b.h[:, t, sl], in0=b.h[:, t, sl], in1=tm[:], op=ALU.add),
                            r=[tm, (b.h, t)], w=[(b.h, t)])

        items = [(ex, blk) for ex in range(E) for blk in blocks]
        load_expert(0)
        if E > 1:
            load_expert(1)
        up(items[0][0], items[0][1], hid[0])
        for i, (ex, blk) in enumerate(items):
            if i + 1 < len(items):
                up(items[i + 1][0], items[i + 1][1], hid[(i + 1) % 2])
            down(ex, blk, hid[i % 2])
            if blk is blocks[-1] and ex >= 1 and ex + 1 < E:
                pass
            if blk is blocks[-1] and ex + 2 < E:
                load_expert(ex + 2)


MIXERS = {}


def build_program(cfg):
    nc = bass.Bass("TRN2", target_bir_lowering=False)
    b = B(nc, cfg)
    fw = b.fw
    NB, NT, NTC = cfg.NB, cfg.NT, cfg.NTC
    x_d = b.din("x", [NB, cfg.S_LAT, D])
    ctx_d = b.din("ctx", [NB, cfg.S_CTX, D])
    b.din("c", [NB, D])
    b.din("c_ctx", [1, D])
    for name, shape in cfg.wshapes.items():
        b.din(name, shape)
    out_d = nc.dram_tensor("out", [NB, cfg.S_LAT, D], F32, kind="ExternalOutput").ap()
    finals = []
    with b.scope():
        build_consts(b)
        b.one_col = b.ones_f
        b.h = b.sb("h", [128, NT, D], F32)
        b.scB = [b.sb("scB%d" % k, [128, KC, 128], F32) for k in range(2)]
        for bi in range(NB):
            with b.scope():
                fw.dma("sync", b.h[:, 0:NTC, :], ctx_d[bi].rearrange("(t p) d -> p t d", p=128), w=[b.h])
                fw.dma("sync", b.h[:, NTC:NT, :], x_d[bi].rearrange("(t p) d -> p t d", p=128), w=[b.h])
                load_cond(b, bi)
                for li in range(cfg.DEPTH):
                    kind = cfg.kinds[li]
                    wc = cfg.want_ctx[li]
                    with b.scope():
                        b.rtmp = [b.sb("rtmp%d" % j, [128, 512], F32) for j in range(2)]
                        b.rtmp_rr = 0
                        all_tiles = list(range(NT))
                        out_tiles = all_tiles if wc else list(range(NTC, NT))
                        if kind >= 0:
                            MIXERS[kind](b, li, all_tiles, out_tiles)
                        if cfg.moe:
                            with b.scope():
                                b.xnT = b.sb("xnT", [128, KC, NT * 128], BF16)
                                norm_mod(b, li, 2, out_tiles)
                                moe(b, li, out_tiles)
                finals.append(fw.dma("sync", out_d[bi].rearrange("(t p) d -> p t d", p=128), b.h[:, NTC:NT, :], r=[b.h]))
        fw.emit(final_wait_ops=finals)
    return nc, b


def host_tables(cfg):
    out = {}
    for name, rot in (("rope64", 64), ("rope32", 32)):
        n = cfg.S_LAT
        rows = n // 64
        row = np.repeat(np.arange(rows, dtype=np.float32), 64)
        col = np.tile(np.arange(64, dtype=np.float32), rows)
        nf = rot // 4
        inv = (10000.0 ** (-np.arange(nf, dtype=np.float32) / nf)).astype(np.float32)
        ang = np.concatenate([row[:, None] * inv, col[:, None] * inv], axis=-1)
        cs = np.concatenate([np.cos(ang), np.sin(ang)], axis=-1).astype(np.float32)
        ctxp = np.concatenate([np.ones((cfg.S_CTX, rot // 2), np.float32), np.zeros((cfg.S_CTX, rot // 2), np.float32)], axis=-1)
        out["c_" + name] = np.concatenate([ctxp, cs], axis=0)
    return out


def load_w(b, name, dram_ap, kchunks, n, dt=BF16, eng="gpsimd"):
    t = b.sb(name, [128, kchunks, n], dt)
    b.fw.dma(eng, t[:], dram_ap.rearrange("(kc p) f -> p kc f", p=128), w=[t])
    return t


def lin_tok(b, xT, t, W, n0, n, p, kchunks=KC, rx=None):
    fw = b.fw
    for kc in range(kchunks):
        fw.pe(lambda e, kc=kc: e.matmul(p[:, 0:n], lhsT=xT[:, kc, t * 128:(t + 1) * 128], rhs=W[:, kc, n0:n0 + n],
                                        start=(kc == 0), stop=(kc == kchunks - 1)),
              r=[(xT, t) if rx is None else rx, W], w=[p])


def transposes(b, src, nblk, dst_ap_fn, r, w):
    fw = b.fw
    pb = b.psumb()
    for k in range(nblk):
        fw.pe(lambda e, k=k: e.transpose(out=pb[:, k * 128:(k + 1) * 128], in_=src[:, k * 128:(k + 1) * 128], identity=b.identb[:]),
              r=[src, b.identb], w=[pb])
    fw.act(lambda e: e.copy(out=dst_ap_fn(), in_=pb[:, 0:nblk * 128].rearrange("p (k q) -> p k q", k=nblk)), r=[pb], w=w)


def row_bcast_tile(b, name, dram_row_ap, n, dt=F32):
    t = b.sb(name, [128, n], dt)
    with b.scope():
        row = b.sb(name + "_row", [1, n], F32)
        b.fw.dma("sync", row[:], dram_row_ap, w=[row])
        b.bcast_rows(t, 0, (row, row), n)
    return t


def out_proj_resid(b, li, t, srcT, W, kchunks, Gk, rsrc):
    fw = b.fw
    for hf in range(2):
        p = b.psum()
        for kc in range(kchunks):
            fw.pe(lambda e, kc=kc, p=p, hf=hf: e.matmul(p[:, :], lhsT=srcT[:, kc, :], rhs=W[:, kc, hf * 512:(hf + 1) * 512],
                                                       start=(kc == 0), stop=(kc == kchunks - 1)), r=[rsrc, W], w=[p])
        resid_add(b, t, p, hf, Gk)


def gmlp(b, li, all_tiles, out_tiles):
    fw, cfg = b.fw, b.cfg
    dr = b.dram
    with b.scope():
        b.xnT = b.sb("xnT", [128, KC, cfg.NT * 128], BF16)
        norm_mod(b, li, 1, out_tiles)
        need_ctx = any(t < cfg.NTC for t in out_tiles)
        Gt = gate_tiles(b, li, 2, need_ctx)
        w_in = load_w(b, "gm_win", dr["gm_w_in"][0], KC, 2 * D)
        w_out = load_w(b, "gm_wout", dr["gm_w_out"][0], KC, D)
        vg = row_bcast_tile(b, "gm_vg", dr["gm_v_g"][0:1, :], D)
        ws_raw = b.sb("ws_raw", [128, 8, 128], BF16)
        fw.dma("gpsimd", ws_raw[:], dr["gm_w_s"][0].rearrange("g p q -> p g q"), w=[ws_raw])
        wsT = b.sb("wsT", [128, 8, 128], BF16)
        transposes(b, ws_raw[:].rearrange("p g q -> p (g q)"), 8, lambda: wsT[:], [ws_raw], [wsT])
        bs_raw = b.sb("bs_raw", [8, 128], F32)
        fw.dma("sync", bs_raw[:], dr["gm_b_s"][0], w=[bs_raw])
        pbs = b.psum()
        fw.pe(lambda e: e.matmul(pbs[:, 0:8], lhsT=bs_raw[:, :], rhs=b.identf[0:8, 0:8], start=True, stop=True),
              r=[bs_raw, b.identf], w=[pbs])
        bsT = b.sb("bsT", [128, 8], F32)
        fw.act(lambda e: e.copy(out=bsT[:], in_=pbs[:, 0:8]), r=[pbs], w=[bsT])
        u = [b.sb("gm_u%d" % j, [128, D], F32) for j in range(1)]
        v = [b.sb("gm_v%d" % j, [128, D], F32) for j in range(1)]
        vb = [b.sb("gm_vb%d" % j, [128, D], BF16) for j in range(1)]
        us = [b.sb("gm_us%d" % j, [128, D], BF16) for j in range(1)]
        usT = [b.sb("gm_usT%d" % j, [128, KC, 128], BF16) for j in range(1)]
        ss = b.sb("gm_ss", [128, cfg.NT], F32)
        fw.pool(lambda e: e.memset(ss[:], 0.0), w=[ss])
        for j, t in enumerate(out_tiles):
            k = 1 if t < cfg.NTC else 0
            uj, vj, vbj, usj, usTj = u[0], v[0], vb[0], us[0], usT[0]
            for blk in range(4):
                p = b.psum()
                lin_tok(b, b.xnT, t, w_in, blk * 512, 512, p)
                dst = uj if blk < 2 else vj
                c0 = (blk % 2) * 512
                fw.act(lambda e, p=p, dst=dst, c0=c0: e.activation(out=dst[:, c0:c0 + 512], in_=p[:, :], func=AF.Gelu),
                       r=[p], w=[(dst, blk % 2)])
            fw.act(lambda e, vj=vj, vbj=vbj, t=t: e.activation(out=vbj[:], in_=vj[:], func=AF.Square, accum_out=ss[:, t:t + 1]),
                   r=[vj], w=[vbj, (ss, t)])
            rstd_op(b, ss[:, t:t + 1], ss[:, t:t + 1], 1.0 / D, [(ss, t)], [(ss, t)])
            fw.dve(lambda e, vj=vj, vbj=vbj, t=t: e.scalar_tensor_tensor(out=vbj[:], in0=vj[:], scalar=ss[:, t:t + 1], in1=vg[:],
                                                                        op0=ALU.mult, op1=ALU.mult), r=[vj, (ss, t), vg], w=[vbj])
            for hf in range(2):
                p = b.psum()
                for g4 in range(4):
                    g = hf * 4 + g4
                    fw.pe(lambda e, p=p, g=g, g4=g4, vbj=vbj: e.matmul(p[:, g4 * 128:(g4 + 1) * 128], lhsT=wsT[:, g, :],
                                                                      rhs=vbj[:, g * 128:(g + 1) * 128], start=True, stop=True),
                          r=[wsT, vbj], w=[p])
                tmp = b.rtmp[b.rtmp_rr]
                b.rtmp_rr = (b.rtmp_rr + 1) % len(b.rtmp)
                fw.dve(lambda e, p=p, tmp=tmp, hf=hf: e.tensor_tensor(
                    out=tmp[:].rearrange("p (g d) -> p g d", g=4), in0=p[:, :].rearrange("p (g d) -> p g d", g=4),
                    in1=bsT[:, hf * 4:(hf + 1) * 4].unsqueeze(2).to_broadcast([128, 4, 128]), op=ALU.add), r=[p, bsT], w=[tmp])
                fw.pool(lambda e, tmp=tmp, uj=uj, usj=usj, hf=hf: e.tensor_tensor(
                    out=usj[:, hf * 512:(hf + 1) * 512], in0=tmp[:], in1=uj[:, hf * 512:(hf + 1) * 512], op=ALU.mult),
                    r=[tmp, uj], w=[(usj, hf)])
            transposes(b, usj, KC, lambda usTj=usTj: usTj[:], [usj], [usTj])
            out_proj_resid(b, li, t, usTj, w_out, KC, Gt[k], usTj)


MIXERS[1] = gmlp


def attn_core(b, qT, kT, v_aug, vd, qtiles, ktiles, scale, Osb, r_q, r_k, r_v):
    fw = b.fw
    nq = len(qtiles) * 128
    q0 = qtiles[0] * 128
    w1 = vd + 1
    per_bank = 1
    pexp = b.pexp
    obanks = [b.ps[0], b.ps[1], b.ps[2], b.ps[3]]
    assert len(qtiles) <= 4
    for ki, kt in enumerate(ktiles):
        ps_s = b.ps[4 + (b.sc_rr % 2)]
        b.sc_rr += 1
        fw.pe(lambda e, ps_s=ps_s, kt=kt: e.matmul(ps_s[:, 0:nq], lhsT=kT(kt * 128, 128), rhs=qT(q0, nq), start=True, stop=True),
              r=[r_q, r_k], w=[ps_s])
        pe_ = pexp[ki % 2]
        fw.act(lambda e, ps_s=ps_s, pe_=pe_: e.activation(out=pe_[:, 0:nq], in_=ps_s[:, 0:nq], func=AF.Exp, scale=scale),
               r=[ps_s], w=[pe_])
        for qi in range(len(qtiles)):
            ob = obanks[qi // per_bank]
            oc = (qi % per_bank) * w1
            fw.pe(lambda e, ob=ob, oc=oc, qi=qi, pe_=pe_, kt=kt, ki=ki: e.matmul(ob[:, oc:oc + w1], lhsT=pe_[:, qi * 128:(qi + 1) * 128],
                                                                       rhs=v_aug(kt), start=(ki == 0), stop=(ki == len(ktiles) - 1)),
                  r=[pe_, r_v], w=[ob])
    for qi in range(len(qtiles)):
        ob = obanks[qi // per_bank]
        oc = (qi % per_bank) * w1
        fw.act(lambda e, ob=ob, oc=oc, qi=qi: e.copy(out=Osb[:, qi, 0:w1], in_=ob[:, oc:oc + w1]), r=[ob], w=[(Osb, qi)])


def qblocks(cfg, out_tiles):
    blocks = []
    ctx_q = [t for t in out_tiles if t < cfg.NTC]
    if ctx_q:
        blocks.append((ctx_q, list(range(cfg.NTC))))
    lat = [t for t in out_tiles if t >= cfg.NTC]
    return blocks, lat


def rope_apply(b, dst, src, cs_t, ngrp, half, r, w):
    fw = b.fw
    t1, t2 = b.rope_tmp
    cos = cs_t[:, 0:half].unsqueeze(1).to_broadcast([128, ngrp, half])
    sin = cs_t[:, half:2 * half].unsqueeze(1).to_broadcast([128, ngrp, half])
    x1 = src[:, :, 0, :]
    x2 = src[:, :, 1, :]
    v1 = t1[:, 0:ngrp * half].rearrange("p (g h) -> p g h", g=ngrp)
    v2 = t2[:, 0:ngrp * half].rearrange("p (g h) -> p g h", g=ngrp)
    fw.dve(lambda e: e.tensor_tensor(out=v1, in0=x1, in1=cos, op=ALU.mult), r=r, w=[t1])
    fw.pool(lambda e: e.tensor_tensor(out=v2, in0=x2, in1=sin, op=ALU.mult), r=r, w=[t2])
    fw.dve(lambda e: e.tensor_tensor(out=dst[:, :, 0, :], in0=v1, in1=v2, op=ALU.subtract), r=[t1, t2], w=w)
    fw.dve(lambda e: e.tensor_tensor(out=v1, in0=x1, in1=sin, op=ALU.mult), r=r, w=[t1])
    fw.pool(lambda e: e.tensor_tensor(out=v2, in0=x2, in1=cos, op=ALU.mult), r=r, w=[t2])
    fw.dve(lambda e: e.tensor_tensor(out=dst[:, :, 1, :], in0=v1, in1=v2, op=ALU.add), r=[t1, t2], w=w)


def group_rms(b, x, ngrp, gd, ssq, r, w_ssq):
    fw = b.fw
    sq = b.sq_tmp
    fw.pool(lambda e: e.tensor_tensor(out=sq[:, 0:ngrp * gd], in0=x[:, 0:ngrp * gd], in1=x[:, 0:ngrp * gd], op=ALU.mult), r=r, w=[sq])
    fw.dve(lambda e: e.tensor_reduce(out=ssq[:, 0:ngrp], in_=sq[:, 0:ngrp * gd].rearrange("p (g d) -> p g d", g=ngrp),
                                     axis=AX.X, op=ALU.add), r=[sq], w=w_ssq)
    rstd_op(b, ssq[:, 0:ngrp], ssq[:, 0:ngrp], 1.0 / gd, w_ssq, w_ssq)


def diff_attn(b, li, all_tiles, out_tiles):
    fw, cfg = b.fw, b.cfg
    dr = b.dram
    NT = cfg.NT
    lam_init = 0.8 - 0.6 * math.exp(-0.3 * cfg.layer_ids[li])
    with b.scope():
        b.xnT = b.sb("xnT", [128, KC, NT * 128], BF16)
        norm_mod(b, li, 1, all_tiles)
        need_ctx = any(t < cfg.NTC for t in out_tiles)
        Gt = gate_tiles(b, li, 2, need_ctx)
        lr = b.sb("lamrow", [1, 4, 64], F32)
        for j, nm in enumerate(("da_lam_q1", "da_lam_k1", "da_lam_q2", "da_lam_k2")):
            fw.dma("sync", lr[0:1, j, :], dr[nm][0:1, :], w=[(lr, j)])
        lp = b.sb("lamp", [1, 2, 64], F32)
        ls = b.sb("lams", [1, 4], F32)
        fw.dve(lambda e: e.tensor_tensor(out=lp[0:1, :, :], in0=lr[0:1, 0:4:2, :], in1=lr[0:1, 1:4:2, :], op=ALU.mult), r=[lr], w=[lp])
        fw.dve(lambda e: e.tensor_reduce(out=ls[0:1, 0:2], in_=lp[0:1, :, :], axis=AX.X, op=ALU.add), r=[lp], w=[ls])
        fw.act(lambda e: e.activation(out=ls[0:1, 0:2], in_=ls[0:1, 0:2], func=AF.Exp), r=[ls], w=[ls])
        fw.dve(lambda e: e.tensor_tensor(out=ls[0:1, 2:3], in0=ls[0:1, 0:1], in1=ls[0:1, 1:2], op=ALU.subtract), r=[ls], w=[ls])
        fw.dve(lambda e: e.tensor_scalar(out=ls[0:1, 3:4], in0=ls[0:1, 2:3], scalar1=lam_init, scalar2=-1.0, op0=ALU.add, op1=ALU.mult),
               r=[ls], w=[ls])
        nlam = b.sb("nlam", [128, 1], F32)
        b.bcast_rows(nlam, 0, (ls, T(ls[0:1, 3:4], ls.name)), 1)
        grow = b.sb("da_grow", [1, 256], F32)
        for j in range(4):
            fw.dma("sync", grow[0:1, j * 64:(j + 1) * 64], dr["da_q_g" if j < 2 else "da_k_g"][0:1, :], w=[(grow, j)])
        qkg = b.sb("da_qkg", [128, 256], F32)
        b.bcast_rows(qkg, 0, (grow, grow), 256)
        cs = b.sb("da_cs", [128, NT, 64], F32)
        fw.dma("sync", cs[:], dr["c_rope64"].rearrange("(t p) c -> p t c", p=128), w=[cs])
        w_out = load_w(b, "da_wout", dr["da_w_out"][0], 8, D)
        sgrow = b.sb("da_sgrow", [1, 128], F32)
        fw.dma("sync", sgrow[:], dr["da_sub_g"][0:1, :], w=[sgrow])
        pc = b.psum()
        fw.pe(lambda e: e.matmul(pc[:, 0:1], lhsT=sgrow[0:1, :], rhs=b.ones_row[0:1, 0:1], start=True, stop=True),
              r=[sgrow, b.ones_row], w=[pc])
        sgcol = b.sb("da_sgcol", [128, 1], F32)
        fw.act(lambda e: e.activation(out=sgcol[:], in_=pc[:, 0:1], func=AF.Copy, scale=(1.0 - lam_init)), r=[pc], w=[sgcol])
        fw.dve(lambda e: e.tensor_scalar(out=w_out[:], in0=w_out[:], scalar1=sgcol[:, 0:1], scalar2=None, op0=ALU.mult),
               r=[w_out, sgcol], w=[w_out])
        b.pexp = [b.sb("pexp%d" % j, [128, 512], BF16) for j in range(2)]
        b.sc_rr = 0
        b.rope_tmp = [b.sb("ropet%d" % j, [128, 512], F32) for j in range(2)]
        b.sq_tmp = b.sb("sqtmp", [128, 256], F32)
        qkT = b.sb("da_qkT", [128, 2, NT * 128], BF16)
        v_aug = b.sb("da_vaug", [128, NT, 132], BF16)
        fw.pool(lambda e: e.memset(v_aug[:, :, 128:129], 1.0), w=[v_aug])
        W_h = [b.sb("da_Wh%d" % j, [128, KC, 384], BF16) for j in range(1)]
        qk = b.sb("da_qk", [128, 256], F32)
        qkn = b.sb("da_qkn", [128, 256], F32)
        qkr = b.sb("da_qkr", [128, 256], BF16)
        ssq = b.sb("da_ssq", [128, 4], F32)
        Osb = [b.sb("da_O%d" % j, [128, 4, 132], F32) for j in range(2)]
        rr = b.sb("da_rr", [128, 2], F32)
        o = b.sb("da_o", [128, 128], F32)
        ob = b.sb("da_ob", [128, 128], BF16)
        oss = b.sb("da_oss", [128, 1], F32)
        junk = b.sb("da_junk", [128, 128], F32)
        oT = b.sb("da_oT", [128, 1, 128], BF16)
        w_in = dr["da_w_in"][0]
        blocks, lat = qblocks(cfg, out_tiles)
        for i0 in range(0, len(lat), 4):
            blocks.append((lat[i0:i0 + 4], all_tiles))
        for hd in range(8):
            Wh = W_h[0]
            for j in range(3):
                fw.dma("gpsimd", Wh[:, :, j * 128:(j + 1) * 128],
                       w_in[:, j * 1024 + hd * 128:j * 1024 + (hd + 1) * 128].rearrange("(kc p) f -> p kc f", p=128), w=[(Wh, j)])
            for t in all_tiles:
                p = b.psum()
                lin_tok(b, b.xnT, t, Wh, 0, 384, p)
                fw.act(lambda e, p=p: e.copy(out=qk[:], in_=p[:, 0:256]), r=[p], w=[qk])
                fw.act(lambda e, p=p, t=t: e.copy(out=v_aug[:, t, 0:128], in_=p[:, 256:384]), r=[p], w=[(v_aug, t)])
                group_rms(b, qk, 4, 64, ssq, [qk], [ssq])
                fw.dve(lambda e: e.tensor_tensor(out=qkn[:].rearrange("p (g d) -> p g d", g=4), in0=qk[:].rearrange("p (g d) -> p g d", g=4),
                                                 in1=ssq[:, 0:4].unsqueeze(2).to_broadcast([128, 4, 64]), op=ALU.mult), r=[qk, ssq], w=[qkn])
                fw.pool(lambda e: e.tensor_tensor(out=qkn[:], in0=qkn[:], in1=qkg[:], op=ALU.mult), r=[qkn, qkg], w=[qkn])
                rope_apply(b, qkr[:].rearrange("p (g two h) -> p g two h", g=4, two=2), qkn[:].rearrange("p (g two h) -> p g two h", g=4, two=2),
                           cs[:, t, :], 4, 32, [qkn, cs], [qkr])
                transposes(b, qkr, 2, lambda t=t: qkT[:, :, t * 128:(t + 1) * 128], [qkr], [(qkT, t)])
            for (qts, kts) in blocks:
                for comp in range(2):
                    sl = slice(comp * 64, (comp + 1) * 64)
                    attn_core(b, lambda c0, n, sl=sl: qkT[sl, 0, c0:c0 + n], lambda c0, n, sl=sl: qkT[sl, 1, c0:c0 + n],
                              lambda kt: v_aug[:, kt, 0:129], 128, qts, kts, 0.125, Osb[comp], qkT, qkT, v_aug)
                for qi, t in enumerate(qts):
                    k = 1 if t < cfg.NTC else 0
                    fw.dve(lambda e, qi=qi: e.reciprocal(out=rr[:, 0:1], in_=Osb[0][:, qi, 128:129]), r=[(Osb[0], qi)], w=[rr])
                    fw.dve(lambda e, qi=qi: e.reciprocal(out=rr[:, 1:2], in_=Osb[1][:, qi, 128:129]), r=[(Osb[1], qi)], w=[rr])
                    fw.dve(lambda e: e.tensor_tensor(out=rr[:, 1:2], in0=rr[:, 1:2], in1=nlam[:, 0:1], op=ALU.mult), r=[rr, nlam], w=[rr])
                    fw.dve(lambda e, qi=qi: e.tensor_scalar(out=o[:], in0=Osb[0][:, qi, 0:128], scalar1=rr[:, 0:1], scalar2=None, op0=ALU.mult),
                           r=[(Osb[0], qi), rr], w=[o])
                    fw.dve(lambda e, qi=qi: e.scalar_tensor_tensor(out=o[:], in0=Osb[1][:, qi, 0:128], scalar=rr[:, 1:2], in1=o[:],
                                                                  op0=ALU.mult, op1=ALU.add), r=[(Osb[1], qi), rr, o], w=[o])
                    fw.pool(lambda e: e.memset(oss[:], 0.0), w=[oss])
                    fw.act(lambda e: e.activation(out=junk[:], in_=o[:], func=AF.Square, accum_out=oss[:, 0:1]), r=[o, oss], w=[junk, oss])
                    rstd_op(b, oss[:, 0:1], oss[:, 0:1], 1.0 / 128, [oss], [oss])
                    fw.dve(lambda e: e.tensor_scalar(out=ob[:], in0=o[:], scalar1=oss[:, 0:1], scalar2=None, op0=ALU.mult), r=[o, oss], w=[ob])
                    transposes(b, ob, 1, lambda: oT[:], [ob], [oT])
                    out_proj_resid(b, li, t, oT, T(w_out[:, hd:hd + 1, :], w_out.name), 1, Gt[k], oT)


MIXERS[0] = diff_attn


def mla(b, li, all_tiles, out_tiles):
    fw, cfg = b.fw, b.cfg
    dr = b.dram
    NT = cfg.NT
    with b.scope():
        need_ctx = any(t < cfg.NTC for t in out_tiles)
        Gt = gate_tiles(b, li, 2, need_ctx)
        cqnT = b.sb("cqnT", [128, 3, NT * 128], BF16)
        ckvnT = b.sb("ckvnT", [128, 2, NT * 128], BF16)
        kpe = b.sb("kpe", [128, NT, 32], F32)
        b.sq_tmp = b.sb("sqtmp", [128, 512], F32)
        ssq = b.sb("ml_ssq", [128, 8], F32)
        with b.scope():
            b.xnT = b.sb("xnT", [128, KC, NT * 128], BF16)
            norm_mod(b, li, 1, all_tiles)
            w_in = load_w(b, "ml_win", dr["mla_w_in"][0], KC, 672)
            qng = row_bcast_tile(b, "ml_qng", dr["mla_q_norm_g"][0:1, :], 384)
            kvng = row_bcast_tile(b, "ml_kvng", dr["mla_kv_norm_g"][0:1, :], 256)
            cq = b.sb("ml_cq", [128, 384], F32)
            ckv = b.sb("ml_ckv", [128, 288], F32)
            cqb = b.sb("ml_cqb", [128, 384], BF16)
            ckvb = b.sb("ml_ckvb", [128, 256], BF16)
            for t in all_tiles:
                p0 = b.psum()
                lin_tok(b, b.xnT, t, w_in, 0, 384, p0)
                p1 = b.psum()
                lin_tok(b, b.xnT, t, w_in, 384, 288, p1)
                fw.act(lambda e, p0=p0: e.copy(out=cq[:], in_=p0[:, 0:384]), r=[p0], w=[cq])
                fw.act(lambda e, p1=p1: e.copy(out=ckv[:], in_=p1[:, 0:288]), r=[p1], w=[ckv])
                fw.pool(lambda e, t=t: e.tensor_copy(out=kpe[:, t, :], in_=ckv[:, 256:288]), r=[ckv], w=[(kpe, t)])
                group_rms(b, cq, 1, 384, ssq, [cq], [ssq])
                fw.dve(lambda e: e.scalar_tensor_tensor(out=cqb[:], in0=cq[:], scalar=ssq[:, 0:1], in1=qng[:], op0=ALU.mult, op1=ALU.mult),
                       r=[cq, ssq, qng], w=[cqb])
                transposes(b, cqb, 3, lambda t=t: cqnT[:, :, t * 128:(t + 1) * 128], [cqb], [(cqnT, t)])
                group_rms(b, ckv, 1, 256, ssq, [ckv], [ssq])
                fw.dve(lambda e: e.scalar_tensor_tensor(out=ckvb[:], in0=ckv[:, 0:256], scalar=ssq[:, 0:1], in1=kvng[:], op0=ALU.mult, op1=ALU.mult),
                       r=[ckv, ssq, kvng], w=[ckvb])
                transposes(b, ckvb, 2, lambda t=t: ckvnT[:, :, t * 128:(t + 1) * 128], [ckvb], [(ckvnT, t)])
        with b.scope():
            qg = row_bcast_tile(b, "ml_qg", dr["mla_q_g"][0:1, :], 96)
            kg = row_bcast_tile(b, "ml_kg", dr["mla_k_g"][0:1, :], 96)
            cs = b.sb("ml_cs", [128, NT, 32], F32)
            fw.dma("sync", cs[:], dr["c_rope32"].rearrange("(t p) c -> p t c", p=128), w=[cs])
            b.pexp = [b.sb("pexp%d" % j, [128, 512], BF16) for j in range(2)]
            b.sc_rr = 0
            b.rope_tmp = [b.sb("ropet%d" % j, [128, 512], F32) for j in range(2)]
            qT_g = b.sb("ml_qT", [128, 4, NT * 128], BF16)
            kT_g = b.sb("ml_kT", [128, 4, NT * 128], BF16)
            v_aug = b.sb("ml_vaug", [128, NT, 4, 66], BF16)
            fw.pool(lambda e: e.memset(v_aug[:, :, :, 64:65], 1.0), w=[v_aug])
            wuq = [b.sb("ml_wuq%d" % j, [128, 3, 384], BF16) for j in range(2)]
            wukv = [b.sb("ml_wukv%d" % j, [128, 2, 512], BF16) for j in range(2)]
            wout = [b.sb("ml_wout%d" % j, [128, 2, D], BF16) for j in range(2)]
            qs = b.sb("ml_qs", [128, 384], F32)
            ks = b.sb("ml_ks", [128, 384], F32)
            qr = b.sb("ml_qr", [128, 384], BF16)
            kr = b.sb("ml_kr", [128, 384], BF16)
            Osb = [b.sb("ml_O%d" % j, [128, 4, 68], F32) for j in range(2)]
            rr = b.sb("ml_rr", [128, 1], F32)
            opair = b.sb("ml_opair", [128, 128], BF16)
            oT = b.sb("ml_oT", [128, 1, 128], BF16)
            blocks, lat = qblocks(cfg, out_tiles)
            for i0 in range(0, len(lat), 4):
                blocks.append((lat[i0:i0 + 4], all_tiles))
            for hg in range(4):
                j = hg % 2
                fw.dma("gpsimd", wuq[j][:], dr["mla_w_uq"][0][:, hg * 384:(hg + 1) * 384].rearrange("(kc p) f -> p kc f", p=128), w=[wuq[j]])
                fw.dma("gpsimd", wukv[j][:], dr["mla_w_ukv"][0][:, hg * 512:(hg + 1) * 512].rearrange("(kc p) f -> p kc f", p=128), w=[wukv[j]])
                fw.dma("gpsimd", wout[j][:], dr["mla_w_out"][0][hg * 256:(hg + 1) * 256, :].rearrange("(kc p) f -> p kc f", p=128), w=[wout[j]])
                for t in all_tiles:
                    pq = b.psum()
                    lin_tok(b, cqnT, t, wuq[j], 0, 384, pq, kchunks=3)
                    pkv = b.psum()
                    lin_tok(b, ckvnT, t, wukv[j], 0, 512, pkv, kchunks=2)
                    fw.act(lambda e, pq=pq: e.copy(out=qs[:], in_=pq[:, 0:384]), r=[pq], w=[qs])
                    pkv4 = pkv[:, :].rearrange("p (h d) -> p h d", h=4)
                    fw.act(lambda e, pkv4=pkv4: e.copy(out=ks[:].rearrange("p (h d) -> p h d", h=4)[:, :, 0:64], in_=pkv4[:, :, 0:64]),
                           r=[pkv], w=[(ks, 0)])
                    fw.pool(lambda e, t=t: e.tensor_copy(out=ks[:].rearrange("p (h d) -> p h d", h=4)[:, :, 64:96],
                                                        in_=kpe[:, t, :].unsqueeze(1).to_broadcast([128, 4, 32])), r=[(kpe, t)], w=[(ks, 1)])
                    fw.act(lambda e, pkv4=pkv4, t=t: e.copy(out=v_aug[:, t, :, 0:64], in_=pkv4[:, :, 64:128]), r=[pkv], w=[(v_aug, t)])
                    for (src, gain, dst, dstT) in ((qs, qg, qr, qT_g), (ks, kg, kr, kT_g)):
                        group_rms(b, src, 4, 96, ssq, [src], [ssq])
                        s4 = src[:].rearrange("p (h d) -> p h d", h=4)
                        fw.dve(lambda e, s4=s4: e.tensor_tensor(out=s4, in0=s4, in1=ssq[:, 0:4].unsqueeze(2).to_broadcast([128, 4, 96]), op=ALU.mult),
                               r=[src, ssq], w=[src])
                        fw.pool(lambda e, s4=s4, gain=gain: e.tensor_tensor(out=s4, in0=s4, in1=gain[:].unsqueeze(1).to_broadcast([128, 4, 96]), op=ALU.mult),
                                r=[src, gain], w=[src])
                        d4 = dst[:].rearrange("p (h d) -> p h d", h=4)
                        fw.act(lambda e, s4=s4, d4=d4: e.copy(out=d4[:, :, 0:64], in_=s4[:, :, 0:64]), r=[src], w=[(dst, 0)])
                        rope_apply(b, d4[:, :, 64:96].rearrange("p h (two x) -> p h two x", two=2),
                                   s4[:, :, 64:96].rearrange("p h (two x) -> p h two x", two=2), cs[:, t, :], 4, 16, [src, cs], [(dst, 1)])
                        pb = b.psumb()
                        for h in range(4):
                            fw.pe(lambda e, pb=pb, h=h, dst=dst: e.transpose(out=pb[0:96, h * 128:(h + 1) * 128], in_=dst[:, h * 96:(h + 1) * 96],
                                                                            identity=b.identb[:]), r=[dst, b.identb], w=[pb])
                        fw.act(lambda e, pb=pb, t=t, dstT=dstT: e.copy(out=dstT[0:96, :, t * 128:(t + 1) * 128],
                                                                      in_=pb[0:96, 0:512].rearrange("p (k q) -> p k q", k=4)), r=[pb], w=[(dstT, t)])
                for pair in range(2):
                    for (qts, kts) in blocks:
                        for hh in range(2):
                            h = pair * 2 + hh
                            attn_core(b, lambda c0, n, h=h: qT_g[0:96, h, c0:c0 + n], lambda c0, n, h=h: kT_g[0:96, h, c0:c0 + n],
                                      lambda kt, h=h: v_aug[:, kt, h, 0:65], 64, qts, kts, 96 ** -0.5, Osb[hh], qT_g, kT_g, v_aug)
                        for qi, t in enumerate(qts):
                            k = 1 if t < cfg.NTC else 0
                            for hh in range(2):
                                fw.dve(lambda e, qi=qi, hh=hh: e.reciprocal(out=rr[:, 0:1], in_=Osb[hh][:, qi, 64:65]), r=[(Osb[hh], qi)], w=[rr])
                                fw.dve(lambda e, qi=qi, hh=hh: e.tensor_scalar(out=opair[:, hh * 64:(hh + 1) * 64], in0=Osb[hh][:, qi, 0:64],
                                                                              scalar1=rr[:, 0:1], scalar2=None, op0=ALU.mult),
                                       r=[(Osb[hh], qi), rr], w=[(opair, hh)])
                            transposes(b, opair, 1, lambda: oT[:], [opair], [oT])
                            out_proj_resid(b, li, t, oT, T(wout[j][:, pair:pair + 1, :], wout[j].name), 1, Gt[k], oT)


MIXERS[3] = mla


def mamba(b, li, all_tiles, out_tiles):
    fw, cfg, nc = b.fw, b.cfg, b.nc
    dr = b.dram
    NT, NTC = cfg.NT, cfg.NTC
    S_CTX, S_LAT = cfg.S_CTX, cfg.S_LAT
    w_in = dr["ssm_w_in"][0]
    if not hasattr(b, "yf_d"):
        b.yf_d = nc.dram_tensor("ssm_yf_scratch", [NT * 128, 512], F32, kind="Internal").ap()
        b.yg_d = nc.dram_tensor("ssm_yg_scratch", [NT * 128, 2048], BF16, kind="Internal").ap()
    yf_d, yg_d = b.yf_d, b.yg_d
    with b.scope():
        need_ctx = any(t < NTC for t in out_tiles)
        ssqg = b.sb("sm_ssqg", [128, NT, 4], F32)
        fw.pool(lambda e: e.memset(ssqg[:], 0.0), w=[ssqg])
        with b.scope():
            b.xnT = b.sb("xnT", [128, KC, NT * 128], BF16)
            norm_mod(b, li, 1, all_tiles)
            dt_all = b.sb("sm_dt", [128, NT, 64], F32)
            a_all = b.sb("sm_a", [128, NT, 64], F32)
            dskB = b.sb("sm_dsk", [128, 32], F32)
            with b.scope():
                Wdt = load_w(b, "sm_wdt", w_in[:, 5120:5184], KC, 64)
                rows = b.sb("sm_rows", [1, 160], F32)
                fw.dma("sync", rows[0:1, 0:64], dr["ssm_dt_bias"][0:1].rearrange("a d h -> a (d h)"), w=[(rows, 0)])
                fw.dma("sync", rows[0:1, 64:128], dr["ssm_a_log"][0:1].rearrange("a d h -> a (d h)"), w=[(rows, 1)])
                fw.dma("sync", rows[0:1, 128:160], dr["ssm_d"][0:1, :], w=[(rows, 2)])
                fw.act(lambda e: e.activation(out=rows[0:1, 64:128], in_=rows[0:1, 64:128], func=AF.Exp), r=[(rows, 1)], w=[(rows, 1)])
                fw.dve(lambda e: e.tensor_scalar(out=rows[0:1, 64:128], in0=rows[0:1, 64:128], scalar1=-1.0, scalar2=None, op0=ALU.mult),
                       r=[(rows, 1)], w=[(rows, 1)])
                cB = b.sb("sm_cB", [128, 160], F32)
                b.bcast_rows(cB, 0, (rows, rows), 160)
                fw.dve(lambda e: e.tensor_copy(out=dskB[:], in_=cB[:, 128:160]), r=[cB], w=[dskB])
                xr = b.sb("sm_xr", [128, 64], F32)
                ax = b.sb("sm_ax", [128, 64], F32)
                for t in all_tiles:
                    p = b.psum()
                    lin_tok(b, b.xnT, t, Wdt, 0, 64, p)
                    fw.dve(lambda e, p=p: e.tensor_tensor(out=xr[:], in0=p[:, 0:64], in1=cB[:, 0:64], op=ALU.add), r=[p, cB], w=[xr])
                    fw.act(lambda e: e.activation(out=ax[:], in_=xr[:], func=AF.Abs), r=[xr], w=[ax])
                    fw.act(lambda e: e.activation(out=ax[:], in_=ax[:], func=AF.Exp, scale=-1.0), r=[ax], w=[ax])
                    fw.act(lambda e: e.activation(out=ax[:], in_=ax[:], func=AF.Ln, bias=b.ones_f[:, 0:1], scale=1.0), r=[ax, b.ones_f], w=[ax])
                    fw.dve(lambda e, t=t: e.scalar_tensor_tensor(out=dt_all[:, t, :], in0=xr[:], scalar=0.0, in1=ax[:], op0=ALU.max, op1=ALU.add),
                           r=[xr, ax], w=[(dt_all, t)])
                    fw.pool(lambda e, t=t: e.tensor_tensor(out=a_all[:, t, :], in0=dt_all[:, t, :], in1=cB[:, 64:128], op=ALU.mult),
                            r=[(dt_all, t), cB], w=[(a_all, t)])
            for g in range(4):
                with b.scope():
                    _mamba_group(b, li, g, all_tiles, dt_all, a_all, dskB, ssqg, w_in, yf_d, yg_d)
        with b.scope():
            Gt = gate_tiles(b, li, 2, need_ctx)
            w_out = load_w(b, "sm_wout", dr["ssm_w_out"][0], 16, D)
            OG = row_bcast_tile(b, "sm_og", dr["ssm_out_g"][0:1, :], 2048)
            rstd = b.sb("sm_rstd", [128, NT], F32)
            fw.dve(lambda e: e.tensor_reduce(out=rstd[:], in_=ssqg[:], axis=AX.X, op=ALU.add), r=[ssqg], w=[rstd])
            rstd_op(b, rstd[:], rstd[:], 1.0 / 2048, [rstd], [rstd])
            ygt = [b.sb("sm_ygt%d" % j, [128, 2048], BF16) for j in range(2)]
            ygn = b.sb("sm_ygn", [128, 2048], BF16)
            ygT = [b.sb("sm_ygT%d" % j, [128, 16, 128], BF16) for j in range(2)]
            for j, t in enumerate(out_tiles):
                k = 1 if t < NTC else 0
                yt, yT = ygt[j % 2], ygT[j % 2]
                fw.dma("sync", yt[:], yg_d[t * 128:(t + 1) * 128, :], r=[("yg_d", t)], w=[yt])
                fw.dve(lambda e, yt=yt, t=t: e.scalar_tensor_tensor(out=ygn[:], in0=yt[:], scalar=rstd[:, t:t + 1], in1=OG[:],
                                                                  op0=ALU.mult, op1=ALU.mult), r=[yt, rstd, OG], w=[ygn])
                for hf in range(2):
                    transposes(b, T(ygn[:, hf * 1024:(hf + 1) * 1024], ygn.name), 8,
                               lambda yT=yT, hf=hf: yT[:, hf * 8:(hf + 1) * 8, :], [ygn], [(yT, hf)])
                out_proj_resid(b, li, t, yT, w_out, 16, Gt[k], yT)


def _mamba_group(b, li, g, all_tiles, dt_all, a_all, dskB, ssqg, w_in, yf_d, yg_d):
    fw, cfg = b.fw, b.cfg
    dr = b.dram
    NT, NTC = cfg.NT, cfg.NTC
    S_CTX, S_LAT = cfg.S_CTX, cfg.S_LAT
    NTOK = NT * 128
    xs_tok = b.sb("sm_xs", [128, NT, 512], BF16)
    BT = b.sb("sm_BT", [128, NTOK], BF16)
    CT = b.sb("sm_CT", [128, NTOK], BF16)
    B_tok = b.sb("sm_Btok", [128, NT, 128], BF16)
    with b.scope():
        XR = S_CTX + S_LAT + 8
        xrow = b.sb("sm_xrow", [128, XR], F32)
        acc = b.sb("sm_acc", [128, NTOK], F32)
        conv_o = b.sb("sm_convo", [128, NTOK], BF16)
        fw.pool(lambda e: e.memset(xrow[:], 0.0), w=[xrow])
        segs = [(0, S_CTX, 2), (S_CTX, S_LAT, S_CTX + 6)]
        Wcs = [b.sb("sm_Wc%d" % j, [128, KC, 128], BF16) for j in range(2)]
        cwrow = b.sb("sm_cwrow", [6, 128], F32)
        cw = b.sb("sm_cw", [128, 6], F32)
        chunks = [("xs", g * 512 + cc * 128, cc) for cc in range(4)] + [("B", 2048 + g * 128, 0), ("C", 2560 + g * 128, 0)]
        for ci, (kind, cch, cc) in enumerate(chunks):
            Wc = Wcs[ci % 2]
            fw.dma("gpsimd", Wc[:], w_in[:, 2048 + cch:2048 + cch + 128].rearrange("(kc p) f -> p kc f", p=128), w=[Wc])
            fw.dma("sync", cwrow[0:5, :], dr["ssm_conv_w"][0][:, cch:cch + 128], w=[(cwrow, 0)])
            fw.dma("sync", cwrow[5:6, :], dr["ssm_conv_b"][0:1, cch:cch + 128], w=[(cwrow, 1)])
            pc = b.psum()
            fw.pe(lambda e, pc=pc: e.matmul(pc[:, 0:6], lhsT=cwrow[:, :], rhs=b.identf[0:6, 0:6], start=True, stop=True),
                  r=[cwrow, b.identf], w=[pc])
            fw.act(lambda e, pc=pc: e.copy(out=cw[:], in_=pc[:, 0:6]), r=[pc], w=[cw])
            dst = conv_o if kind == "xs" else (BT if kind == "B" else CT)
            for (tok0, ln, xoff) in segs:
                for c0 in range(0, ln, 512):
                    n = min(512, ln - c0)
                    p = b.psum()
                    for kc in range(KC):
                        fw.pe(lambda e, p=p, kc=kc, Wc=Wc, a0=tok0 + c0, n=n: e.matmul(p[:, 0:n], lhsT=Wc[:, kc, :], rhs=b.xnT[:, kc, a0:a0 + n],
                                                                                     start=(kc == 0), stop=(kc == KC - 1)), r=[Wc, b.xnT], w=[p])
                    fw.act(lambda e, p=p, x0=xoff + c0, n=n: e.copy(out=xrow[:, x0:x0 + n], in_=p[:, 0:n]), r=[p], w=[xrow])
                fw.dve(lambda e, tok0=tok0, ln=ln, xoff=xoff: e.tensor_scalar(out=acc[:, tok0:tok0 + ln], in0=xrow[:, xoff - 2:xoff - 2 + ln],
                                                                             scalar1=cw[:, 0:1], scalar2=None, op0=ALU.mult),
                       r=[xrow, cw], w=[acc])
                for j in range(1, 5):
                    fw.dve(lambda e, j=j, tok0=tok0, ln=ln, xoff=xoff: e.scalar_tensor_tensor(
                        out=acc[:, tok0:tok0 + ln], in0=xrow[:, xoff - 2 + j:xoff - 2 + j + ln], scalar=cw[:, j:j + 1],
                        in1=acc[:, tok0:tok0 + ln], op0=ALU.mult, op1=ALU.add), r=[xrow, cw, acc], w=[acc])
                fw.act(lambda e, tok0=tok0, ln=ln, dst=dst: e.activation(out=dst[:, tok0:tok0 + ln], in_=acc[:, tok0:tok0 + ln], func=AF.Silu,
                                                                        bias=cw[:, 5:6], scale=1.0), r=[acc, cw], w=[dst])
            if kind == "xs":
                for t in all_tiles:
                    transposes(b, T(conv_o[:, t * 128:(t + 1) * 128], conv_o.name), 1,
                               lambda t=t, cc=cc: xs_tok[:, t:t + 1, cc * 128:(cc + 1) * 128], [conv_o], [(xs_tok, t)])
            elif kind == "B":
                for t in all_tiles:
                    transposes(b, T(BT[:, t * 128:(t + 1) * 128], BT.name), 1, lambda t=t: B_tok[:, t:t + 1, :], [BT], [(B_tok, t)])
    with b.scope():
        Wz = load_w(b, "sm_wz", w_in[:, g * 512:(g + 1) * 512], KC, 512)
        S = b.sb("sm_S", [128, 512], F32)
        S_bf = b.sb("sm_Sbf", [128, 512], BF16)
        CBm = b.sb("sm_CBm", [128, 128], F32)
        aU = b.sb("sm_aU", [128, 8, 128], F32)
        Ex = b.sb("sm_Ex", [128, 8, 128], F32)
        MT = b.sb("sm_MT", [128, 8, 128], BF16)
        xdt = b.sb("sm_xdt", [128, 512], BF16)
        ytmp = b.sb("sm_ytmp", [128, 512], F32)
        ygb = b.sb("sm_ygb", [128, 512], BF16)
        ct2 = [b.sb("sm_ct%d" % j, [128, 24], F32) for j in range(2)]
        ex2 = [b.sb("sm_ex%d" % j, [128, 24], F32) for j in range(2)]
        xdtd2 = [b.sb("sm_xdtd%d" % j, [128, 512], BF16) for j in range(2)]
        ydg2 = [b.sb("sm_ydg%d" % j, [128, 512], F32) for j in range(2)]
        zs2 = [b.sb("sm_zs%d" % j, [128, 512], F32) for j in range(2)]
        yfl2 = [b.sb("sm_yfl%d" % j, [128, 512], F32) for j in range(2)]
        ctx_t = list(range(NTC))
        lat_t = list(range(NTC, NT))
        v8 = lambda ap: ap.rearrange("p (e q) -> p e q", e=8)

        def stageA(d, t, k):
            iU_r, iU_l = (0, 1) if d == 0 else (2, 3)
            c0 = d * 32 + g * 8
            tsl = slice(t * 128, (t + 1) * 128)
            a8 = a_all[:, t, c0:c0 + 8]
            dt8 = dt_all[:, t, c0:c0 + 8]
            ct, ex, xdtd, ydg, zs, yfl = ct2[k], ex2[k], xdtd2[k], ydg2[k], zs2[k], yfl2[k]
            pct = b.psum()
            fw.pe(lambda e: e.matmul(pct[:, 0:8], lhsT=b.tri[:, iU_r, :], rhs=a8, start=True, stop=True), r=[b.tri, (a_all, t)], w=[pct])
            fw.pe(lambda e: e.matmul(pct[:, 8:16], lhsT=b.ones_f[:, :], rhs=a8, start=True, stop=True), r=[b.ones_f, (a_all, t)], w=[pct])
            fw.act(lambda e: e.copy(out=ct[:, 0:16], in_=pct[:, 0:16]), r=[pct], w=[ct])
            fw.dve(lambda e: e.tensor_tensor(out=ct[:, 16:24], in0=ct[:, 8:16], in1=ct[:, 0:8], op=ALU.subtract), r=[ct], w=[ct])
            fw.act(lambda e: e.activation(out=ex[:], in_=ct[:], func=AF.Exp), r=[ct], w=[ex])
            pcb = b.psum()
            fw.pe(lambda e: e.matmul(pcb[:, 0:128], lhsT=BT[:, tsl], rhs=CT[:, tsl], start=True, stop=True), r=[BT, CT], w=[pcb])
            fw.dve(lambda e: e.tensor_tensor(out=CBm[:], in0=pcb[:, 0:128], in1=b.tri[:, iU_r, :], op=ALU.mult), r=[pcb, b.tri], w=[CBm])
            fw.pool(lambda e: e.tensor_tensor(out=aU[:], in0=a8.unsqueeze(2).to_broadcast([128, 8, 128]),
                                              in1=b.tri[:, iU_r, :].unsqueeze(1).to_broadcast([128, 8, 128]), op=ALU.mult),
                    r=[(a_all, t), b.tri], w=[aU])
            for hf in range(2):
                pd = b.psum()
                fw.pe(lambda e, pd=pd, hf=hf: e.matmul(pd[:, :], lhsT=b.tri[:, iU_l, :],
                                                      rhs=aU[:, hf * 4:(hf + 1) * 4, :].rearrange("p e t -> p (e t)"), start=True, stop=True),
                      r=[b.tri, aU], w=[pd])
                fw.act(lambda e, pd=pd, hf=hf: e.activation(out=Ex[:, hf * 4:(hf + 1) * 4, :].rearrange("p e t -> p (e t)"), in_=pd[:, :], func=AF.Exp),
                       r=[pd], w=[(Ex, hf)])
            fw.dve(lambda e: e.tensor_tensor(out=MT[:], in0=Ex[:], in1=CBm[:].unsqueeze(1).to_broadcast([128, 8, 128]), op=ALU.mult),
                   r=[Ex, CBm], w=[MT])
            fw.pool(lambda e: e.tensor_tensor(out=v8(xdt[:]), in0=v8(xs_tok[:, t, :]), in1=dt8.unsqueeze(2).to_broadcast([128, 8, 64]), op=ALU.mult),
                    r=[(xs_tok, t), (dt_all, t)], w=[xdt])
            fw.pool(lambda e: e.tensor_tensor(out=v8(xdtd[:]), in0=v8(xdt[:]), in1=ex[:, 16:24].unsqueeze(2).to_broadcast([128, 8, 64]), op=ALU.mult),
                    r=[xdt, ex], w=[xdtd])
            pyd = b.psum()
            for e8 in range(8):
                fw.pe(lambda e, e8=e8: e.matmul(pyd[:, e8 * 64:(e8 + 1) * 64], lhsT=MT[:, e8, :], rhs=xdt[:, e8 * 64:(e8 + 1) * 64],
                                                start=True, stop=True), r=[MT, xdt], w=[pyd])
            if d == 0:
                fw.act(lambda e: e.copy(out=ydg[:], in_=pyd[:, :]), r=[pyd], w=[ydg])
            else:
                fw.dma("sync", yfl[:], yf_d[tsl, :], r=[("yf_d", t)], w=[yfl])
                fw.dve(lambda e: e.tensor_tensor(out=ydg[:], in0=pyd[:, :], in1=yfl[:], op=ALU.add), r=[pyd, yfl], w=[ydg])
                fw.pool(lambda e: e.tensor_tensor(out=v8(yfl[:]), in0=v8(xs_tok[:, t, :]),
                                                  in1=dskB[:, g * 8:(g + 1) * 8].unsqueeze(2).to_broadcast([128, 8, 64]), op=ALU.mult),
                        r=[(xs_tok, t), dskB, yfl, ydg], w=[yfl])
                fw.pool(lambda e: e.tensor_tensor(out=ydg[:], in0=ydg[:], in1=yfl[:], op=ALU.add), r=[ydg, yfl], w=[ydg])
                pz = b.psum()
                lin_tok(b, b.xnT, t, Wz, 0, 512, pz)
                fw.act(lambda e: e.activation(out=zs[:], in_=pz[:, :], func=AF.Silu), r=[pz], w=[zs])

        def stageB(d, t, k):
            tsl = slice(t * 128, (t + 1) * 128)
            ex, xdtd, ydg, zs = ex2[k], xdtd2[k], ydg2[k], zs2[k]
            pyo = b.psum()
            fw.pe(lambda e: e.matmul(pyo[:, :], lhsT=CT[:, tsl], rhs=S_bf[:, :], start=True, stop=True), r=[CT, S_bf], w=[pyo])
            psn = b.psum()
            fw.pe(lambda e: e.matmul(psn[:, :], lhsT=B_tok[:, t, :], rhs=xdtd[:, :], start=True, stop=True), r=[(B_tok, t), xdtd], w=[psn])
            fw.dve(lambda e: e.tensor_tensor(out=v8(ytmp[:]), in0=v8(pyo[:, :]), in1=ex[:, 0:8].unsqueeze(2).to_broadcast([128, 8, 64]), op=ALU.mult),
                   r=[pyo, ex], w=[ytmp])
            fw.pool(lambda e: e.tensor_tensor(out=v8(S[:]), in0=v8(S[:]), in1=ex[:, 8:16].unsqueeze(2).to_broadcast([128, 8, 64]), op=ALU.mult),
                    r=[S, ex], w=[S])
            fw.dve(lambda e: e.tensor_tensor(out=S[:], in0=psn[:, :], in1=S[:], op=ALU.add), r=[psn, S], w=[S])
            fw.act(lambda e: e.copy(out=S_bf[:], in_=S[:]), r=[S], w=[S_bf])
            fw.dve(lambda e: e.tensor_tensor(out=ydg[:], in0=ydg[:], in1=ytmp[:], op=ALU.add), r=[ydg, ytmp], w=[ydg])
            if d == 0:
                fw.dma("sync", yf_d[tsl, :], ydg[:], r=[ydg], w=[("yf_d", t)])
            else:
                fw.dve(lambda e: e.tensor_tensor(out=ydg[:], in0=ydg[:], in1=zs[:], op=ALU.mult), r=[ydg, zs], w=[ydg])
                fw.act(lambda e: e.activation(out=ygb[:], in_=ydg[:], func=AF.Square, accum_out=ssqg[:, t, g:g + 1]),
                       r=[ydg], w=[ygb, (ssqg, t)])
                fw.pool(lambda e: e.tensor_copy(out=ygb[:], in_=ydg[:]), r=[ydg], w=[ygb])
                fw.dma("sync", yg_d[tsl, g * 512:(g + 1) * 512], ygb[:], r=[ygb], w=[("yg_d", t)])

        for d in range(2):
            order = (ctx_t + lat_t) if d == 0 else (ctx_t[::-1] + lat_t[::-1])
            fw.pool(lambda e: e.memset(S[:], 0.0), w=[S])
            fw.pool(lambda e: e.memset(S_bf[:], 0.0), w=[S_bf])
            stageA(d, order[0], 0)
            for i, t in enumerate(order):
                if i + 1 < len(order):
                    stageA(d, order[i + 1], (i + 1) % 2)
                stageB(d, t, i % 2)


MIXERS[2] = mamba


def _host_consts(cfg):
    ident = np.eye(128, dtype=np.float32)
    k = np.arange(128)[:, None]
    t = np.arange(128)[None, :]
    tri = np.stack([(k <= t), (k > t), (k >= t), (k < t)]).astype(np.float32)
    out = {"c_ident": ident, "c_tri": tri}
    out.update(host_tables(cfg))
    return out


def kernel(**inputs):
    from concourse.bass_utils import run_bass_kernel_spmd
    inp = {k: np.ascontiguousarray(np.asarray(v)) for k, v in inputs.items()}
    n_cores = 8
    NB = inp["x"].shape[0] // n_cores
    cfg = Cfg(S_LAT=inp["x"].shape[1], S_CTX=inp["ctx"].shape[1], NB=NB, kinds=[0, 1, 2, 3])
    wnames = [k for k in inp if k not in ("x", "c", "ctx", "c_ctx")]
    cfg.wshapes = {k: inp[k].shape for k in wnames}
    nc, b = build_program(cfg)
    consts = _host_consts(cfg)
    maps = []
    for core in range(n_cores):
        m = {k: inp[k] for k in wnames if k in b.dram}
        m.update({k: v for k, v in consts.items() if k in b.dram})
        m["x"] = inp["x"][core * NB:(core + 1) * NB]
        m["ctx"] = inp["ctx"][core * NB:(core + 1) * NB]
        m["c"] = inp["c"][core * NB:(core + 1) * NB]
        m["c_ctx"] = inp["c_ctx"][None, :]
        maps.append(m)
    res = run_bass_kernel_spmd(nc, maps, core_ids=list(range(n_cores)))
    return np.concatenate([r["out"] for r in res.results], axis=0).astype(np.float32)
```

```python
import os
import numpy as np
import concourse.bass as bass
import concourse.mybir as mybir

F32 = mybir.dt.float32
BF16 = mybir.dt.bfloat16
AF = mybir.ActivationFunctionType
ALU = mybir.AluOpType
AX = mybir.AxisListType

ENGS = ("tensor", "vector", "scalar", "gpsimd", "sync")
N_DMA_SEMS = 8


class _Op:
    __slots__ = ("eng", "fn", "deps", "chan", "is_dma", "needs_inc", "val", "idx")


class FW:
    def __init__(self, nc, same_engine_sync=True):
        self.nc = nc
        self.ops = []
        self.state = {}
        self.same_engine_sync = same_engine_sync
        self.dma_rr = {e: 0 for e in ENGS}
        self.chan_last = {}
        self.bar = {}

    def barrier(self):
        self.bar = dict(self.chan_last)

    def _entries(self, res):
        name, idx = res if isinstance(res, tuple) else (res, None)
        name = getattr(name, "name", name)
        ent = self.state.setdefault(name, {})
        return name, idx, ent

    def _collect(self, res, is_write, deps):
        name, idx, ent = self._entries(res)
        if idx is None:
            targets = list(ent.values())
        else:
            targets = [ent.get(idx), ent.get(None)]
        for e in targets:
            if e is None:
                continue
            if e[0] is not None:
                deps.add(e[0])
            if is_write:
                deps.update(e[1].values())

    def _update(self, res, is_write, op):
        name, idx, ent = self._entries(res)
        if is_write:
            if idx is None:
                ent.clear()
                ent[None] = [op.idx, {}]
            else:
                ent[idx] = [op.idx, {}]
        else:
            e = ent.get(idx)
            if e is None:
                e = ent[idx] = [None, {}]
            e[1][op.chan] = op.idx

    capture = None

    def op(self, eng, fn, r=(), w=(), dma=False):
        if self.capture is not None:
            self.capture.append((eng, fn, tuple(r), tuple(w), dma))
            return None
        o = _Op()
        o.idx = len(self.ops)
        o.eng = eng
        o.fn = fn
        o.is_dma = dma
        if dma:
            k = self.dma_rr[eng]
            self.dma_rr[eng] = (k + 1) % N_DMA_SEMS
            o.chan = "dma_%s_%d" % (eng, k)
        else:
            o.chan = eng
        o.needs_inc = dma
        o.val = None
        deps = set()
        for res in r:
            self._collect(res, False, deps)
        for res in w:
            self._collect(res, True, deps)
        best = {}
        for d in deps:
            c = self.ops[d].chan
            if c not in best or best[c] < d:
                best[c] = d
        for c, d in self.bar.items():
            if c not in best or best[c] < d:
                best[c] = d
        if dma and o.chan in self.chan_last:
            d = self.chan_last[o.chan]
            if o.chan not in best or best[o.chan] < d:
                best[o.chan] = d
        o.deps = best
        self.chan_last[o.chan] = o.idx
        for res in r:
            self._update(res, False, o)
        for res in w:
            self._update(res, True, o)
        self.ops.append(o)
        return o

    def lockstep(self, items, nslot, body):
        for i0 in range(0, len(items), nslot):
            lists = []
            for slot, it in enumerate(items[i0:i0 + nslot]):
                self.capture = []
                body(it, slot)
                lists.append(self.capture)
                self.capture = None
            idx = [0] * len(lists)
            more = True
            while more:
                more = False
                for L, lst in enumerate(lists):
                    if idx[L] < len(lst):
                        self.op(*lst[idx[L]])
                        idx[L] += 1
                        more = True

    def pe(self, fn, r=(), w=()):
        return self.op("tensor", fn, r, w)

    def dve(self, fn, r=(), w=()):
        return self.op("vector", fn, r, w)

    def act(self, fn, r=(), w=()):
        return self.op("scalar", fn, r, w)

    def pool(self, fn, r=(), w=()):
        return self.op("gpsimd", fn, r, w)

    def dma(self, eng, out, in_, r=(), w=(), **kw):
        return self.op(eng, lambda e: e.dma_start(out=out, in_=in_, **kw), r, w, dma=True)

    def _init_emit(self):
        import contextlib
        self._st = contextlib.ExitStack()
        chans = list(ENGS[:4]) + ["dma_%s_%d" % (e, k) for e in ("sync", "scalar", "gpsimd") for k in range(N_DMA_SEMS)]
        self.sems = {c: self._st.enter_context(self.nc.semaphore("s_" + c)) for c in chans}
        self.cnt = {c: 0 for c in chans}
        self.inc_idx = {c: [] for c in chans}
        self.inc_val = {c: [] for c in chans}
        self.waited = {e: {} for e in ENGS}
        self.flushed = 0

    def _skip_same(self, o, p):
        return p.chan == o.eng and not p.is_dma and (o.eng == "tensor" or not self.same_engine_sync)

    def flush(self):
        import bisect
        if not hasattr(self, "sems"):
            self._init_emit()
        ops = self.ops
        batch = ops[self.flushed:]
        if not batch:
            return
        start = self.flushed
        last_in_chan = {}
        for o in batch:
            last_in_chan[o.chan] = o
            for c, d in o.deps.items():
                p = ops[d]
                if d >= start and not self._skip_same(o, p):
                    p.needs_inc = True
        for o in last_in_chan.values():
            o.needs_inc = True
        for o in batch:
            if o.needs_inc:
                self.cnt[o.chan] += 16 if o.is_dma else 1
                o.val = self.cnt[o.chan]
                self.inc_idx[o.chan].append(o.idx)
                self.inc_val[o.chan].append(o.val)
        for o in batch:
            eng = getattr(self.nc, o.eng)
            waited = self.waited[o.eng]
            for c, d in o.deps.items():
                p = ops[d]
                if self._skip_same(o, p):
                    continue
                k = bisect.bisect_left(self.inc_idx[p.chan], d)
                val = self.inc_val[p.chan][k]
                if waited.get(p.chan, 0) >= val:
                    continue
                eng.wait_ge(self.sems[p.chan], val)
                waited[p.chan] = val
            ins = o.fn(eng)
            if o.needs_inc:
                ins.then_inc(self.sems[o.chan], 16 if o.is_dma else 1)
            o.fn = None
        self.flushed = len(ops)

    def emit(self, final_wait_ops=()):
        self.flush()
        eng = self.nc.sync
        for p in final_wait_ops:
            eng.wait_ge(self.sems[p.chan], p.val)
        self.sem_max = dict(self.cnt)
        self._st.close()


import contextlib
import math
import numpy as np
import concourse.bass as bass
import concourse.mybir as mybir

D = 1024
KC = 8
EPS = 1e-6


class T:
    def __init__(self, h, name):
        self.h = h
        self.name = name

    def __getitem__(self, k):
        return self.h[k]


class Cfg:
    def __init__(self, **kw):
        self.S_LAT = 2048
        self.S_CTX = 256
        self.NB = 2
        self.GROUPS = 4
        self.PER = 8
        self.kinds = [0, 1, 2, 3]
        self.want_ctx = [True, True, True, False]
        self.layer_ids = [0, 1, 2, 3]
        self.moe = True
        self.__dict__.update(kw)
        self.NTC = self.S_CTX // 128
        self.NTL = self.S_LAT // 128
        self.NT = self.NTC + self.NTL
        self.E = self.GROUPS * self.PER
        self.DEPTH = len(self.kinds)


class B:
    def __init__(self, nc, cfg):
        self.nc = nc
        self.cfg = cfg
        self.fw = FW(nc)
        self.uid = 0
        self.stacks = []
        self.dram = {}
        self.ps_rr = 0

    @contextlib.contextmanager
    def scope(self):
        st = contextlib.ExitStack()
        self.stacks.append(st)
        try:
            with st:
                yield
                self.fw.flush()
        finally:
            self.fw.flush()
            self.stacks.pop()
            self.fw.barrier()

    def sb(self, name, shape, dt):
        self.uid += 1
        nm = "%s_%d" % (name, self.uid)
        h = self.stacks[-1].enter_context(self.nc.sbuf_tensor(nm, list(shape), dt))
        return T(h, nm)

    def din(self, name, shape, dt=F32):
        ap = self.nc.dram_tensor(name, list(shape), dt, kind="ExternalInput").ap()
        self.dram[name] = ap
        return ap

    def psum(self):
        p = self.ps[self.ps_rr]
        self.ps_rr = (self.ps_rr + 1) % len(self.ps)
        return p

    def psumb(self):
        p = self.psb[self.psb_rr]
        self.psb_rr = (self.psb_rr + 1) % len(self.psb)
        return p

    def bcast_rows(self, dst, dst_cols, row_ap_f32, n, evac=None):
        fw = self.fw
        for c0 in range(0, n, 512):
            cw = min(512, n - c0)
            p = self.psum()
            fw.pe(lambda e, p=p, c0=c0, cw=cw: e.matmul(p[:, 0:cw], lhsT=self.ones_row[0:1, 0:128],
                                                      rhs=row_ap_f32[1][0:1, c0:c0 + cw], start=True, stop=True),
                  r=[self.ones_row, row_ap_f32[0]], w=[p])
            d0 = dst_cols + c0
            if evac is None:
                fw.act(lambda e, p=p, d0=d0, cw=cw: e.copy(out=dst[:, d0:d0 + cw], in_=p[:, 0:cw]), r=[p], w=[dst])
            else:
                evac(p, d0, cw)


def rstd_op(b, out, in_, scale, r, w):
    fw = b.fw
    fw.act(lambda e: e.activation(out=out, in_=in_, func=AF.Sqrt, bias=b.eps_col[0:out.shape[0], 0:1], scale=scale),
           r=list(r) + [b.eps_col], w=w)
    fw.dve(lambda e: e.reciprocal(out=out, in_=out), r=w, w=w)


def build_consts(b):
    nc, fw = b.nc, b.fw
    cfg = b.cfg
    ident_d = b.din("c_ident", [128, 128])
    tri_d = b.din("c_tri", [4, 128, 128])
    b.din("c_rope64", [cfg.S_CTX + cfg.S_LAT, 64])
    b.din("c_rope32", [cfg.S_CTX + cfg.S_LAT, 32])
    b.identb = b.sb("identb", [128, 128], BF16)
    b.identf = b.sb("identf", [128, 128], F32)
    b.ones_row = b.sb("ones_row", [1, 512], F32)
    b.ones_f = b.sb("ones_f", [128, 128], F32)
    b.tri = b.sb("tri", [128, 4, 128], F32)
    fw.dma("gpsimd", b.identb[:], ident_d, w=[b.identb])
    fw.dma("sync", b.identf[:], ident_d, w=[b.identf])
    fw.dma("sync", b.tri[:], tri_d.rearrange("a p q -> p a q"), w=[b.tri])
    fw.pool(lambda e: e.memset(b.ones_row[:], 1.0), w=[b.ones_row])
    fw.pool(lambda e: e.memset(b.ones_f[:], 1.0), w=[b.ones_f])
    b.eps_col = b.sb("eps_col", [128, 1], F32)
    fw.pool(lambda e: e.memset(b.eps_col[:], EPS), w=[b.eps_col])
    st = b.stacks[-1]
    b.ps = []
    for j in range(6):
        h = st.enter_context(nc.psum_tensor("psf%d" % j, [128, 512], F32))
        b.ps.append(T(h, "psf%d" % j))
    b.psb = []
    for j in range(2):
        h = st.enter_context(nc.psum_tensor("psb%d" % j, [128, 1024], BF16))
        b.psb.append(T(h, "psb%d" % j))
    b.psb_rr = 0


def load_cond(b, bi):
    with b.scope():
        _load_cond(b, bi)


def _load_cond(b, bi):
    fw = b.fw
    c_d, cctx_d = b.dram["c"], b.dram["c_ctx"]
    crow = b.sb("crow", [1, 2 * D], F32)
    fw.dma("sync", crow[0:1, 0:D], c_d[bi:bi + 1, :], w=[(crow, 0)])
    fw.dma("sync", crow[0:1, D:2 * D], cctx_d[0:1, :], w=[(crow, 1)])
    p = b.psum()
    for k in range(2):
        for kc in range(KC):
            fw.pe(lambda e, k=k, kc=kc: e.matmul(p[:, k * KC + kc:k * KC + kc + 1],
                                                lhsT=crow[0:1, k * D + kc * 128:k * D + (kc + 1) * 128],
                                                rhs=b.ones_row[0:1, 0:1], start=True, stop=True),
                  r=[crow, b.ones_row], w=[p])
    ccol = b.sb("ccol", [128, 2 * KC], F32)
    fw.act(lambda e: e.activation(out=ccol[:], in_=p[:, 0:2 * KC], func=AF.Silu), r=[p], w=[ccol])
    for k in range(2):
        fw.dve(lambda e, k=k: e.tensor_copy(out=b.scB[k][:],
                                           in_=ccol[:, k * KC:(k + 1) * KC].unsqueeze(2).to_broadcast([128, KC, 128])),
               r=[ccol], w=[b.scB[k]])


def mod_bcast(b, li, sec, dsts, add_one=False):
    fw = b.fw
    ada_w, ada_b = b.dram["ada_w"], b.dram["ada_b"]
    with b.scope():
        _mod_bcast(b, li, sec, dsts, add_one)


def _mod_bcast(b, li, sec, dsts, add_one):
    fw = b.fw
    ada_w, ada_b = b.dram["ada_w"], b.dram["ada_b"]
    brow = b.sb("brow", [1, D], F32)
    QW = 256
    wsts = [b.sb("wst%d" % j, [128, KC, QW], F32) for j in range(2)]
    fw.dma("sync", brow[:], ada_b[li:li + 1, sec * D:(sec + 1) * D], w=[brow])
    for q in range(D // QW):
        wst = wsts[q % 2]
        c0 = sec * D + q * QW
        fw.dma("sync", wst[:], ada_w[li, :, c0:c0 + QW].rearrange("(kc p) f -> p kc f", p=128), w=[wst])
        for k in range(2):
            if dsts[k] is None:
                continue
            p = b.psum()
            for kc in range(KC):
                fw.pe(lambda e, p=p, k=k, kc=kc, wst=wst: e.matmul(p[:, 0:QW], lhsT=b.scB[k][:, kc, :], rhs=wst[:, kc, :],
                                                                  start=(kc == 0), stop=False),
                      r=[b.scB[k], wst], w=[p])
            fw.pe(lambda e, p=p, q=q: e.matmul(p[:, 0:QW], lhsT=b.ones_row[0:1, 0:128], rhs=brow[0:1, q * QW:(q + 1) * QW],
                                              start=False, stop=True),
                  r=[b.ones_row, brow], w=[p])
            d = dsts[k]
            if add_one:
                fw.act(lambda e, p=p, d=d, q=q: e.activation(out=d[:, q * QW:(q + 1) * QW], in_=p[:, 0:QW],
                                                            func=AF.Identity, bias=b.one_col[:, 0:1], scale=1.0),
                       r=[p, b.one_col], w=[(d, q)])
            else:
                fw.act(lambda e, p=p, d=d, q=q: e.copy(out=d[:, q * QW:(q + 1) * QW], in_=p[:, 0:QW]),
                       r=[p], w=[(d, q)])


def norm_mod(b, li, which, tiles):
    fw, cfg = b.fw, b.cfg
    gname = "norm1_g" if which == 1 else "norm2_g"
    with b.scope():
        A = [b.sb("A%d" % k, [128, D], F32) for k in range(2)]
        S = [b.sb("S%d" % k, [128, D], F32) for k in range(2)]
        G = b.sb("Gn", [128, D], F32)
        grow = b.sb("grow", [1, D], F32)
        need_ctx = any(t < cfg.NTC for t in tiles)
        sec0 = 0 if which == 1 else 3
        fw.dma("sync", grow[:], b.dram[gname][li:li + 1, :], w=[grow])
        b.bcast_rows(G, 0, (grow, grow), D)
        dA = [A[0], A[1] if need_ctx else None]
        dS = [S[0], S[1] if need_ctx else None]
        mod_bcast(b, li, sec0 + 1, dA, add_one=True)
        mod_bcast(b, li, sec0 + 0, dS)
        for k in range(2):
            if dA[k] is not None:
                fw.dve(lambda e, k=k: e.tensor_mul(out=A[k][:], in0=A[k][:], in1=G[:]), r=[A[k], G], w=[A[k]])
        junk = b.sb("junk", [128, D], BF16)
        ss = b.sb("ss", [128, cfg.NT], F32)
        rstd = b.sb("rstd", [128, cfg.NT], F32)
        fw.pool(lambda e: e.memset(ss[:], 0.0), w=[ss])
        tmp = [b.sb("nt%d" % j, [128, D], F32) for j in range(2)]
        xnb = [b.sb("xnb%d" % j, [128, D], BF16) for j in range(2)]
        for j, t in enumerate(tiles):
            k = 1 if t < cfg.NTC else 0
            fw.act(lambda e, t=t: e.activation(out=junk[:], in_=b.h[:, t, :], func=AF.Square, accum_out=ss[:, t:t + 1]),
                   r=[(b.h, t)], w=[junk, (ss, t)])
            rstd_op(b, rstd[:, t:t + 1], ss[:, t:t + 1], 1.0 / D, [(ss, t)], [(rstd, t)])
            tm, xb = tmp[j % 2], xnb[j % 2]
            fw.dve(lambda e, t=t, tm=tm, k=k: e.scalar_tensor_tensor(out=tm[:], in0=b.h[:, t, :], scalar=rstd[:, t:t + 1],
                                                                    in1=A[k][:], op0=ALU.mult, op1=ALU.mult),
                   r=[(b.h, t), (rstd, t), A[k]], w=[tm])
            fw.pool(lambda e, tm=tm, xb=xb, k=k: e.tensor_tensor(out=xb[:], in0=tm[:], in1=S[k][:], op=ALU.add),
                    r=[tm, S[k]], w=[xb])
            pb = b.psumb()
            for kc in range(KC):
                fw.pe(lambda e, pb=pb, kc=kc, xb=xb: e.transpose(out=pb[:, kc * 128:(kc + 1) * 128],
                                                                in_=xb[:, kc * 128:(kc + 1) * 128], identity=b.identb[:]),
                      r=[xb, b.identb], w=[pb])
            fw.act(lambda e, pb=pb, t=t: e.copy(out=b.xnT[:, :, t * 128:(t + 1) * 128],
                                               in_=pb[:, :].rearrange("p (k q) -> p k q", k=KC)),
                   r=[pb], w=[(b.xnT, t)])


def gate_tiles(b, li, sec, need_ctx):
    Gt = [b.sb("Gt0", [128, D], F32), b.sb("Gt1", [128, D], F32) if need_ctx else None]
    mod_bcast(b, li, sec, Gt)
    return Gt


def resid_add(b, t, p, hf, Gk):
    fw = b.fw
    tm = b.rtmp[b.rtmp_rr]
    b.rtmp_rr = (b.rtmp_rr + 1) % len(b.rtmp)
    sl = slice(hf * 512, (hf + 1) * 512)
    fw.dve(lambda e: e.tensor_tensor(out=tm[:], in0=p[:, :], in1=Gk[:, sl], op=ALU.mult), r=[p, Gk], w=[tm])
    fw.pool(lambda e: e.tensor_tensor(out=b.h[:, t, sl], in0=b.h[:, t, sl], in1=tm[:], op=ALU.add),
            r=[tm, (b.h, t)], w=[(b.h, t)])


def moe(b, li, tiles):
    fw, cfg = b.fw, b.cfg
    E, NT = cfg.E, cfg.NT
    NG, PER = cfg.GROUPS, cfg.PER
    NL = NG + E
    need_ctx = any(t < cfg.NTC for t in tiles)
    with b.scope():
        Gt = gate_tiles(b, li, 5, need_ctx)
        gates = b.sb("gates", [128, NT, E], F32)
        with b.scope():
            _router(b, li, tiles, gates)
        _experts(b, li, tiles, gates, Gt)


def _router(b, li, tiles, gates):
    fw, cfg = b.fw, b.cfg
    E, NT = cfg.E, cfg.NT
    NG, PER = cfg.GROUPS, cfg.PER
    NL = NG + E
    if True:
        wgr = b.sb("wgr", [128, KC, NL], BF16)
        brow = b.sb("brow_r", [1, NL], F32)
        fw.dma("gpsimd", wgr[:, :, 0:NG], b.dram["moe_w_group"][li].rearrange("(kc p) f -> p kc f", p=128), w=[(wgr, 0)])
        fw.dma("gpsimd", wgr[:, :, NG:NL], b.dram["moe_w_router"][li].rearrange("(kc p) f -> p kc f", p=128), w=[(wgr, 1)])
        fw.dma("sync", brow[0:1, 0:NG], b.dram["moe_b_group"][li:li + 1, :], w=[(brow, 0)])
        fw.dma("sync", brow[0:1, NG:NL], b.dram["moe_b_router"][li:li + 1, :], w=[(brow, 1)])
        L = b.sb("L", [128, NT, NL], F32)
        fw.pool(lambda e: e.memset(L[:], 0.0), w=[L])
        for t in tiles:
            p = b.psum()
            for kc in range(KC):
                fw.pe(lambda e, p=p, kc=kc, t=t: e.matmul(p[:, 0:NL], lhsT=b.xnT[:, kc, t * 128:(t + 1) * 128], rhs=wgr[:, kc, :],
                                                         start=(kc == 0), stop=False), r=[(b.xnT, t), wgr], w=[p])
            fw.pe(lambda e, p=p: e.matmul(p[:, 0:NL], lhsT=b.ones_row[0:1, 0:128], rhs=brow[0:1, :], start=False, stop=True),
                  r=[b.ones_row, brow], w=[p])
            fw.act(lambda e, p=p, t=t: e.copy(out=L[:, t, :], in_=p[:, 0:NL]), r=[p], w=[L])
        def v(name, shape):
            return b.sb(name, shape, F32)
        gmax = v("gmax", [128, NT]); eg = v("eg", [128, NT, NG]); gsum = v("gsum", [128, NT]); pg = v("pg", [128, NT])
        goh = v("goh", [128, NT, NG]); Lm = v("Lm", [128, NT, E]); m1 = v("m1", [128, NT]); oh1 = v("oh1", [128, NT, E])
        m2 = v("m2", [128, NT]); oh2 = v("oh2", [128, NT, E]); w1 = v("w1", [128, NT]); w2 = v("w2", [128, NT])
        pen = v("pen", [128, NT, NG])
        Lg = L[:, :, 0:NG]
        Le = L[:, :, NG:NL]
        dv = fw.dve
        dv(lambda e: e.tensor_reduce(out=gmax[:], in_=Lg, axis=AX.X, op=ALU.max), r=[L], w=[gmax])
        dv(lambda e: e.tensor_tensor(out=eg[:], in0=Lg, in1=gmax[:].unsqueeze(2).to_broadcast([128, NT, NG]), op=ALU.subtract),
           r=[L, gmax], w=[eg])
        dv(lambda e: e.tensor_tensor(out=goh[:], in0=Lg, in1=gmax[:].unsqueeze(2).to_broadcast([128, NT, NG]), op=ALU.is_equal),
           r=[L, gmax], w=[goh])
        fw.act(lambda e: e.activation(out=eg[:], in_=eg[:], func=AF.Exp), r=[eg], w=[eg])
        dv(lambda e: e.tensor_reduce(out=gsum[:], in_=eg[:], axis=AX.X, op=ALU.add), r=[eg], w=[gsum])
        dv(lambda e: e.reciprocal(out=pg[:], in_=gsum[:]), r=[gsum], w=[pg])
        dv(lambda e: e.tensor_scalar(out=pen[:], in0=goh[:], scalar1=-1.0, scalar2=1e30, op0=ALU.add, op1=ALU.mult),
           r=[goh], w=[pen])
        dv(lambda e: e.tensor_tensor(out=Lm[:].rearrange("p t (g e) -> p t g e", g=NG),
                                     in0=Le.rearrange("p t (g e) -> p t g e", g=NG),
                                     in1=pen[:].unsqueeze(3).to_broadcast([128, NT, NG, PER]), op=ALU.add),
           r=[L, pen], w=[Lm])
        dv(lambda e: e.tensor_reduce(out=m1[:], in_=Lm[:], axis=AX.X, op=ALU.max), r=[Lm], w=[m1])
        dv(lambda e: e.tensor_tensor(out=oh1[:], in0=Lm[:], in1=m1[:].unsqueeze(2).to_broadcast([128, NT, E]), op=ALU.is_equal),
           r=[Lm, m1], w=[oh1])
        dv(lambda e: e.scalar_tensor_tensor(out=Lm[:], in0=oh1[:], scalar=-1e30, in1=Lm[:], op0=ALU.mult, op1=ALU.add),
           r=[oh1, Lm], w=[Lm])
        dv(lambda e: e.tensor_reduce(out=m2[:], in_=Lm[:], axis=AX.X, op=ALU.max), r=[Lm], w=[m2])
        dv(lambda e: e.tensor_tensor(out=oh2[:], in0=Lm[:], in1=m2[:].unsqueeze(2).to_broadcast([128, NT, E]), op=ALU.is_equal),
           r=[Lm, m2], w=[oh2])
        dv(lambda e: e.tensor_tensor(out=w2[:], in0=m2[:], in1=m1[:], op=ALU.subtract), r=[m1, m2], w=[w2])
        fw.act(lambda e: e.activation(out=w2[:], in_=w2[:], func=AF.Sigmoid), r=[w2], w=[w2])
        dv(lambda e: e.tensor_scalar(out=w1[:], in0=w2[:], scalar1=-1.0, scalar2=1.0, op0=ALU.mult, op1=ALU.add), r=[w2], w=[w1])
        dv(lambda e: e.tensor_mul(out=w1[:], in0=w1[:], in1=pg[:]), r=[w1, pg], w=[w1])
        dv(lambda e: e.tensor_mul(out=w2[:], in0=w2[:], in1=pg[:]), r=[w2, pg], w=[w2])
        dv(lambda e: e.tensor_tensor(out=oh1[:], in0=oh1[:], in1=w1[:].unsqueeze(2).to_broadcast([128, NT, E]), op=ALU.mult),
           r=[oh1, w1], w=[oh1])
        dv(lambda e: e.tensor_tensor(out=oh2[:], in0=oh2[:], in1=w2[:].unsqueeze(2).to_broadcast([128, NT, E]), op=ALU.mult),
           r=[oh2, w2], w=[oh2])
        dv(lambda e: e.tensor_add(out=gates[:], in0=oh1[:], in1=oh2[:]), r=[oh1, oh2], w=[gates])


def _experts(b, li, tiles, gates, Gt):
    fw, cfg = b.fw, b.cfg
    E, NT = cfg.E, cfg.NT
    if True:
        FF = 512
        w1b = [b.sb("w1b%d" % j, [128, KC, FF], BF16) for j in range(2)]
        w3b = [b.sb("w3b%d" % j, [128, KC, FF], BF16) for j in range(2)]
        w2b = [b.sb("w2b%d" % j, [128, 4, D], BF16) for j in range(2)]
        hid = [b.sb("hid%d" % j, [128, 4, 512], BF16) for j in range(2)]
        s1 = [b.sb("s1_%d" % j, [128, 512], BF16) for j in range(2)]
        mtmp = [b.sb("mtmp%d" % j, [128, 512], F32) for j in range(2)]
        blocks = []
        i0 = 0
        while i0 < len(tiles):
            blocks.append(tiles[i0:i0 + 4])
            i0 += 4
        def load_expert(ex):
            j = ex % 2
            fw.dma("gpsimd", w1b[j][:], b.dram["moe_w1"][li, ex].rearrange("(kc p) f -> p kc f", p=128), w=[w1b[j]])
            fw.dma("gpsimd", w3b[j][:], b.dram["moe_w3"][li, ex].rearrange("(kc p) f -> p kc f", p=128), w=[w3b[j]])
            fw.dma("gpsimd", w2b[j][:], b.dram["moe_w2"][li, ex].rearrange("(kc p) f -> p kc f", p=128), w=[w2b[j]])

        def up(ex, blk, hd):
            j = ex % 2
            nt = len(blk) * 128
            c0 = blk[0] * 128
            for fc in range(4):
                p1 = b.psum()
                p3 = b.psum()
                for kc in range(KC):
                    fw.pe(lambda e, p1=p1, kc=kc, fc=fc: e.matmul(
                        p1[:, 0:nt], lhsT=w1b[j][:, kc, fc * 128:(fc + 1) * 128], rhs=b.xnT[:, kc, c0:c0 + nt],
                        start=(kc == 0), stop=(kc == KC - 1)), r=[w1b[j], b.xnT], w=[p1])
                for kc in range(KC):
                    fw.pe(lambda e, p3=p3, kc=kc, fc=fc: e.matmul(
                        p3[:, 0:nt], lhsT=w3b[j][:, kc, fc * 128:(fc + 1) * 128], rhs=b.xnT[:, kc, c0:c0 + nt],
                        start=(kc == 0), stop=(kc == KC - 1)), r=[w3b[j], b.xnT], w=[p3])
                sj = s1[fc % 2]
                fw.act(lambda e, p1=p1, sj=sj: e.activation(out=sj[:, 0:nt], in_=p1[:, 0:nt], func=AF.Silu), r=[p1], w=[sj])
                fw.dve(lambda e, p3=p3, sj=sj, fc=fc: e.tensor_tensor(
                    out=hd[:, fc, 0:nt], in0=p3[:, 0:nt], in1=sj[:, 0:nt], op=ALU.mult), r=[p3, sj], w=[(hd, fc)])

        def down(ex, blk, hd):
            j = ex % 2
            for ti, t in enumerate(blk):
                k = 1 if t < cfg.NTC else 0
                for hf in range(2):
                    py = b.psum()
                    for fc in range(4):
                        fw.pe(lambda e, py=py, fc=fc, ti=ti, hf=hf: e.matmul(
                            py[:, :], lhsT=hd[:, fc, ti * 128:(ti + 1) * 128], rhs=w2b[j][:, fc, hf * 512:(hf + 1) * 512],
                            start=(fc == 0), stop=(fc == 3)), r=[hd, w2b[j]], w=[py])
                    tm = mtmp[(ti * 2 + hf) % 2]
                    sl = slice(hf * 512, (hf + 1) * 512)
                    fw.dve(lambda e, py=py, tm=tm, t=t, k=k, sl=sl: e.scalar_tensor_tensor(
                        out=tm[:], in0=py[:, :], scalar=gates[:, t, ex:ex + 1], in1=Gt[k][:, sl], op0=ALU.mult, op1=ALU.mult),
                        r=[py, gates, Gt[k]], w=[tm])
                    fw.pool(lambda e, tm=tm, t=t, sl=sl: e.tensor_tensor(out=b.h[:, t, sl], in0=b.h[:, t, sl], in1=tm[:], op=ALU.add),
                            r=[tm, (b.h, t)], w=[(b.h, t)])

        items = [(ex, blk) for ex in range(E) for blk in blocks]
        load_expert(0)
        if E > 1:
            load_expert(1)
        up(items[0][0], items[0][1], hid[0])
        for i, (ex, blk) in enumerate(items):
            if i + 1 < len(items):
                up(items[i + 1][0], items[i + 1][1], hid[(i + 1) % 2])
            down(ex, blk, hid[i % 2])
            if blk is blocks[-1] and ex >= 1 and ex + 1 < E:
                pass
            if blk is blocks[-1] and ex + 2 < E:
                load_expert(ex + 2)


MIXERS = {}


def build_program(cfg):
    nc = bass.Bass("TRN2", target_bir_lowering=False)
    b = B(nc, cfg)
    fw = b.fw
    NB, NT, NTC = cfg.NB, cfg.NT, cfg.NTC
    x_d = b.din("x", [NB, cfg.S_LAT, D])
    ctx_d = b.din("ctx", [NB, cfg.S_CTX, D])
    b.din("c", [NB, D])
    b.din("c_ctx", [1, D])
    for name, shape in cfg.wshapes.items():
        b.din(name, shape)
    out_d = nc.dram_tensor("out", [NB, cfg.S_LAT, D], F32, kind="ExternalOutput").ap()
    finals = []
    with b.scope():
        build_consts(b)
        b.one_col = b.ones_f
        b.h = b.sb("h", [128, NT, D], F32)
        b.scB = [b.sb("scB%d" % k, [128, KC, 128], F32) for k in range(2)]
        for bi in range(NB):
            with b.scope():
                fw.dma("sync", b.h[:, 0:NTC, :], ctx_d[bi].rearrange("(t p) d -> p t d", p=128), w=[b.h])
                fw.dma("sync", b.h[:, NTC:NT, :], x_d[bi].rearrange("(t p) d -> p t d", p=128), w=[b.h])
                load_cond(b, bi)
                for li in range(cfg.DEPTH):
                    kind = cfg.kinds[li]
                    wc = cfg.want_ctx[li]
                    with b.scope():
                        b.rtmp = [b.sb("rtmp%d" % j, [128, 512], F32) for j in range(4)]
                        b.rtmp_rr = 0
                        all_tiles = list(range(NT))
                        out_tiles = all_tiles if wc else list(range(NTC, NT))
                        if kind >= 0:
                            MIXERS[kind](b, li, all_tiles, out_tiles)
                        if cfg.moe:
                            with b.scope():
                                b.xnT = b.sb("xnT", [128, KC, NT * 128], BF16)
                                norm_mod(b, li, 2, out_tiles)
                                moe(b, li, out_tiles)
                finals.append(fw.dma("sync", out_d[bi].rearrange("(t p) d -> p t d", p=128), b.h[:, NTC:NT, :], r=[b.h]))
        fw.emit(final_wait_ops=finals)
    return nc, b


def host_tables(cfg):
    out = {}
    for name, rot in (("rope64", 64), ("rope32", 32)):
        n = cfg.S_LAT
        rows = n // 64
        row = np.repeat(np.arange(rows, dtype=np.float32), 64)
        col = np.tile(np.arange(64, dtype=np.float32), rows)
        nf = rot // 4
        inv = (10000.0 ** (-np.arange(nf, dtype=np.float32) / nf)).astype(np.float32)
        ang = np.concatenate([row[:, None] * inv, col[:, None] * inv], axis=-1)
        cs = np.concatenate([np.cos(ang), np.sin(ang)], axis=-1).astype(np.float32)
        ctxp = np.concatenate([np.ones((cfg.S_CTX, rot // 2), np.float32), np.zeros((cfg.S_CTX, rot // 2), np.float32)], axis=-1)
        out["c_" + name] = np.concatenate([ctxp, cs], axis=0)
    return out


def load_w(b, name, dram_ap, kchunks, n, dt=BF16, eng="gpsimd"):
    t = b.sb(name, [128, kchunks, n], dt)
    b.fw.dma(eng, t[:], dram_ap.rearrange("(kc p) f -> p kc f", p=128), w=[t])
    return t


def lin_tok(b, xT, t, W, n0, n, p, kchunks=KC, rx=None):
    fw = b.fw
    for kc in range(kchunks):
        fw.pe(lambda e, kc=kc: e.matmul(p[:, 0:n], lhsT=xT[:, kc, t * 128:(t + 1) * 128], rhs=W[:, kc, n0:n0 + n],
                                        start=(kc == 0), stop=(kc == kchunks - 1)),
              r=[(xT, t) if rx is None else rx, W], w=[p])


def transposes(b, src, nblk, dst_ap_fn, r, w):
    fw = b.fw
    pb = b.psumb()
    for k in range(nblk):
        fw.pe(lambda e, k=k: e.transpose(out=pb[:, k * 128:(k + 1) * 128], in_=src[:, k * 128:(k + 1) * 128], identity=b.identb[:]),
              r=[src, b.identb], w=[pb])
    fw.act(lambda e: e.copy(out=dst_ap_fn(), in_=pb[:, 0:nblk * 128].rearrange("p (k q) -> p k q", k=nblk)), r=[pb], w=w)


def row_bcast_tile(b, name, dram_row_ap, n, dt=F32):
    t = b.sb(name, [128, n], dt)
    with b.scope():
        row = b.sb(name + "_row", [1, n], F32)
        b.fw.dma("sync", row[:], dram_row_ap, w=[row])
        b.bcast_rows(t, 0, (row, row), n)
    return t


def out_proj_resid(b, li, t, srcT, W, kchunks, Gk, rsrc):
    fw = b.fw
    for hf in range(2):
        p = b.psum()
        for kc in range(kchunks):
            fw.pe(lambda e, kc=kc, p=p, hf=hf: e.matmul(p[:, :], lhsT=srcT[:, kc, :], rhs=W[:, kc, hf * 512:(hf + 1) * 512],
                                                       start=(kc == 0), stop=(kc == kchunks - 1)), r=[rsrc, W], w=[p])
        resid_add(b, t, p, hf, Gk)


def gmlp(b, li, all_tiles, out_tiles):
    fw, cfg = b.fw, b.cfg
    dr = b.dram
    with b.scope():
        b.xnT = b.sb("xnT", [128, KC, cfg.NT * 128], BF16)
        norm_mod(b, li, 1, out_tiles)
        need_ctx = any(t < cfg.NTC for t in out_tiles)
        Gt = gate_tiles(b, li, 2, need_ctx)
        w_in = load_w(b, "gm_win", dr["gm_w_in"][0], KC, 2 * D)
        w_out = load_w(b, "gm_wout", dr["gm_w_out"][0], KC, D)
        vg = row_bcast_tile(b, "gm_vg", dr["gm_v_g"][0:1, :], D)
        wsT = b.sb("wsT", [128, 8, 128], BF16)
        bsT = b.sb("bsT", [128, 8], F32)
        with b.scope():
            ws_raw = b.sb("ws_raw", [128, 8, 128], BF16)
            fw.dma("gpsimd", ws_raw[:], dr["gm_w_s"][0].rearrange("g p q -> p g q"), w=[ws_raw])
            transposes(b, ws_raw[:].rearrange("p g q -> p (g q)"), 8, lambda: wsT[:], [ws_raw], [wsT])
            bs_raw = b.sb("bs_raw", [8, 128], F32)
            fw.dma("sync", bs_raw[:], dr["gm_b_s"][0], w=[bs_raw])
            pbs = b.psum()
            fw.pe(lambda e: e.matmul(pbs[:, 0:8], lhsT=bs_raw[:, :], rhs=b.identf[0:8, 0:8], start=True, stop=True),
                  r=[bs_raw, b.identf], w=[pbs])
            fw.act(lambda e: e.copy(out=bsT[:], in_=pbs[:, 0:8]), r=[pbs], w=[bsT])
        u = [b.sb("gm_u%d" % j, [128, D], F32) for j in range(1)]
        v = [b.sb("gm_v%d" % j, [128, D], F32) for j in range(1)]
        vb = [b.sb("gm_vb%d" % j, [128, D], BF16) for j in range(1)]
        us = [b.sb("gm_us%d" % j, [128, D], BF16) for j in range(1)]
        usT = [b.sb("gm_usT%d" % j, [128, KC, 128], BF16) for j in range(1)]
        ss = b.sb("gm_ss", [128, cfg.NT], F32)
        fw.pool(lambda e: e.memset(ss[:], 0.0), w=[ss])
        for j, t in enumerate(out_tiles):
            k = 1 if t < cfg.NTC else 0
            uj, vj, vbj, usj, usTj = u[0], v[0], vb[0], us[0], usT[0]
            for blk in range(4):
                p = b.psum()
                lin_tok(b, b.xnT, t, w_in, blk * 512, 512, p)
                dst = uj if blk < 2 else vj
                c0 = (blk % 2) * 512
                fw.act(lambda e, p=p, dst=dst, c0=c0: e.activation(out=dst[:, c0:c0 + 512], in_=p[:, :], func=AF.Gelu),
                       r=[p], w=[(dst, blk % 2)])
            fw.act(lambda e, vj=vj, vbj=vbj, t=t: e.activation(out=vbj[:], in_=vj[:], func=AF.Square, accum_out=ss[:, t:t + 1]),
                   r=[vj], w=[vbj, (ss, t)])
            rstd_op(b, ss[:, t:t + 1], ss[:, t:t + 1], 1.0 / D, [(ss, t)], [(ss, t)])
            fw.dve(lambda e, vj=vj, vbj=vbj, t=t: e.scalar_tensor_tensor(out=vbj[:], in0=vj[:], scalar=ss[:, t:t + 1], in1=vg[:],
                                                                        op0=ALU.mult, op1=ALU.mult), r=[vj, (ss, t), vg], w=[vbj])
            for hf in range(2):
                p = b.psum()
                for g4 in range(4):
                    g = hf * 4 + g4
                    fw.pe(lambda e, p=p, g=g, g4=g4, vbj=vbj: e.matmul(p[:, g4 * 128:(g4 + 1) * 128], lhsT=wsT[:, g, :],
                                                                      rhs=vbj[:, g * 128:(g + 1) * 128], start=True, stop=True),
                          r=[wsT, vbj], w=[p])
                tmp = b.rtmp[b.rtmp_rr]
                b.rtmp_rr = (b.rtmp_rr + 1) % len(b.rtmp)
                fw.dve(lambda e, p=p, tmp=tmp, hf=hf: e.tensor_tensor(
                    out=tmp[:].rearrange("p (g d) -> p g d", g=4), in0=p[:, :].rearrange("p (g d) -> p g d", g=4),
                    in1=bsT[:, hf * 4:(hf + 1) * 4].unsqueeze(2).to_broadcast([128, 4, 128]), op=ALU.add), r=[p, bsT], w=[tmp])
                fw.pool(lambda e, tmp=tmp, uj=uj, usj=usj, hf=hf: e.tensor_tensor(
                    out=usj[:, hf * 512:(hf + 1) * 512], in0=tmp[:], in1=uj[:, hf * 512:(hf + 1) * 512], op=ALU.mult),
                    r=[tmp, uj], w=[(usj, hf)])
            transposes(b, usj, KC, lambda usTj=usTj: usTj[:], [usj], [usTj])
            out_proj_resid(b, li, t, usTj, w_out, KC, Gt[k], usTj)


MIXERS[1] = gmlp


def attn_core(b, qT, kT, v_aug, vd, qtiles, ktiles, scale, Osb, r_q, r_k, r_v):
    fw = b.fw
    nq = len(qtiles) * 128
    q0 = qtiles[0] * 128
    w1 = vd + 1
    per_bank = 1
    pexp = b.pexp
    obanks = [b.ps[0], b.ps[1], b.ps[2], b.ps[3]]
    assert len(qtiles) <= 4
    for ki, kt in enumerate(ktiles):
        ps_s = b.ps[4 + (b.sc_rr % 2)]
        b.sc_rr += 1
        fw.pe(lambda e, ps_s=ps_s, kt=kt: e.matmul(ps_s[:, 0:nq], lhsT=kT(kt * 128, 128), rhs=qT(q0, nq), start=True, stop=True),
              r=[r_q, r_k], w=[ps_s])
        pe_ = pexp[ki % 2]
        fw.act(lambda e, ps_s=ps_s, pe_=pe_: e.activation(out=pe_[:, 0:nq], in_=ps_s[:, 0:nq], func=AF.Exp, scale=scale),
               r=[ps_s], w=[pe_])
        for qi in range(len(qtiles)):
            ob = obanks[qi // per_bank]
            oc = (qi % per_bank) * w1
            fw.pe(lambda e, ob=ob, oc=oc, qi=qi, pe_=pe_, kt=kt, ki=ki: e.matmul(ob[:, oc:oc + w1], lhsT=pe_[:, qi * 128:(qi + 1) * 128],
                                                                       rhs=v_aug(kt), start=(ki == 0), stop=(ki == len(ktiles) - 1)),
                  r=[pe_, r_v], w=[ob])
    for qi in range(len(qtiles)):
        ob = obanks[qi // per_bank]
        oc = (qi % per_bank) * w1
        fw.act(lambda e, ob=ob, oc=oc, qi=qi: e.copy(out=Osb[:, qi, 0:w1], in_=ob[:, oc:oc + w1]), r=[ob], w=[(Osb, qi)])


def qblocks(cfg, out_tiles):
    blocks = []
    ctx_q = [t for t in out_tiles if t < cfg.NTC]
    if ctx_q:
        blocks.append((ctx_q, list(range(cfg.NTC))))
    lat = [t for t in out_tiles if t >= cfg.NTC]
    return blocks, lat


def rope_apply(b, dst, src, cs_t, ngrp, half, r, w):
    fw = b.fw
    t1, t2 = b.rope_tmp
    cos = cs_t[:, 0:half].unsqueeze(1).to_broadcast([128, ngrp, half])
    sin = cs_t[:, half:2 * half].unsqueeze(1).to_broadcast([128, ngrp, half])
    x1 = src[:, :, 0, :]
    x2 = src[:, :, 1, :]
    v1 = t1[:, 0:ngrp * half].rearrange("p (g h) -> p g h", g=ngrp)
    v2 = t2[:, 0:ngrp * half].rearrange("p (g h) -> p g h", g=ngrp)
    fw.dve(lambda e: e.tensor_tensor(out=v1, in0=x1, in1=cos, op=ALU.mult), r=r, w=[t1])
    fw.pool(lambda e: e.tensor_tensor(out=v2, in0=x2, in1=sin, op=ALU.mult), r=r, w=[t2])
    fw.dve(lambda e: e.tensor_tensor(out=dst[:, :, 0, :], in0=v1, in1=v2, op=ALU.subtract), r=[t1, t2], w=w)
    fw.dve(lambda e: e.tensor_tensor(out=v1, in0=x1, in1=sin, op=ALU.mult), r=r, w=[t1])
    fw.pool(lambda e: e.tensor_tensor(out=v2, in0=x2, in1=cos, op=ALU.mult), r=r, w=[t2])
    fw.dve(lambda e: e.tensor_tensor(out=dst[:, :, 1, :], in0=v1, in1=v2, op=ALU.add), r=[t1, t2], w=w)


def group_rms(b, x, ngrp, gd, ssq, r, w_ssq):
    fw = b.fw
    sq = b.sq_tmp
    fw.pool(lambda e: e.tensor_tensor(out=sq[:, 0:ngrp * gd], in0=x[:, 0:ngrp * gd], in1=x[:, 0:ngrp * gd], op=ALU.mult), r=r, w=[sq])
    fw.dve(lambda e: e.tensor_reduce(out=ssq[:, 0:ngrp], in_=sq[:, 0:ngrp * gd].rearrange("p (g d) -> p g d", g=ngrp),
                                     axis=AX.X, op=ALU.add), r=[sq], w=w_ssq)
    rstd_op(b, ssq[:, 0:ngrp], ssq[:, 0:ngrp], 1.0 / gd, w_ssq, w_ssq)


def diff_attn(b, li, all_tiles, out_tiles):
    fw, cfg = b.fw, b.cfg
    dr = b.dram
    NT = cfg.NT
    lam_init = 0.8 - 0.6 * math.exp(-0.3 * cfg.layer_ids[li])
    with b.scope():
        b.xnT = b.sb("xnT", [128, KC, NT * 128], BF16)
        norm_mod(b, li, 1, all_tiles)
        need_ctx = any(t < cfg.NTC for t in out_tiles)
        Gt = gate_tiles(b, li, 2, need_ctx)
        lr = b.sb("lamrow", [1, 4, 64], F32)
        for j, nm in enumerate(("da_lam_q1", "da_lam_k1", "da_lam_q2", "da_lam_k2")):
            fw.dma("sync", lr[0:1, j, :], dr[nm][0:1, :], w=[(lr, j)])
        lp = b.sb("lamp", [1, 2, 64], F32)
        ls = b.sb("lams", [1, 4], F32)
        fw.dve(lambda e: e.tensor_tensor(out=lp[0:1, :, :], in0=lr[0:1, 0:4:2, :], in1=lr[0:1, 1:4:2, :], op=ALU.mult), r=[lr], w=[lp])
        fw.dve(lambda e: e.tensor_reduce(out=ls[0:1, 0:2], in_=lp[0:1, :, :], axis=AX.X, op=ALU.add), r=[lp], w=[ls])
        fw.act(lambda e: e.activation(out=ls[0:1, 0:2], in_=ls[0:1, 0:2], func=AF.Exp), r=[ls], w=[ls])
        fw.dve(lambda e: e.tensor_tensor(out=ls[0:1, 2:3], in0=ls[0:1, 0:1], in1=ls[0:1, 1:2], op=ALU.subtract), r=[ls], w=[ls])
        fw.dve(lambda e: e.tensor_scalar(out=ls[0:1, 3:4], in0=ls[0:1, 2:3], scalar1=lam_init, scalar2=-1.0, op0=ALU.add, op1=ALU.mult),
               r=[ls], w=[ls])
        nlam = b.sb("nlam", [128, 1], F32)
        b.bcast_rows(nlam, 0, (ls, T(ls[0:1, 3:4], ls.name)), 1)
        grow = b.sb("da_grow", [1, 256], F32)
        for j in range(4):
            fw.dma("sync", grow[0:1, j * 64:(j + 1) * 64], dr["da_q_g" if j < 2 else "da_k_g"][0:1, :], w=[(grow, j)])
        qkg = b.sb("da_qkg", [128, 256], F32)
        b.bcast_rows(qkg, 0, (grow, grow), 256)
        cs = b.sb("da_cs", [128, NT, 64], F32)
        fw.dma("sync", cs[:], dr["c_rope64"].rearrange("(t p) c -> p t c", p=128), w=[cs])
        w_out2 = [b.sb("da_wout%d" % j, [128, 1, D], BF16) for j in range(2)]
        sgrow = b.sb("da_sgrow", [1, 128], F32)
        fw.dma("sync", sgrow[:], dr["da_sub_g"][0:1, :], w=[sgrow])
        pc = b.psum()
        fw.pe(lambda e: e.matmul(pc[:, 0:1], lhsT=sgrow[0:1, :], rhs=b.ones_row[0:1, 0:1], start=True, stop=True),
              r=[sgrow, b.ones_row], w=[pc])
        sgcol = b.sb("da_sgcol", [128, 1], F32)
        fw.act(lambda e: e.activation(out=sgcol[:], in_=pc[:, 0:1], func=AF.Copy, scale=(1.0 - lam_init)), r=[pc], w=[sgcol])
        b.pexp = [b.sb("pexp%d" % j, [128, 512], BF16) for j in range(2)]
        b.sc_rr = 0
        NS = 2
        rope_tmps = [[b.sb("ropet%d_%d" % (sl, j), [128, 128], F32) for j in range(2)] for sl in range(NS)]
        sq_tmps = [b.sb("sqtmp%d" % sl, [128, 256], F32) for sl in range(NS)]
        qkT = b.sb("da_qkT", [128, 2, NT * 128], BF16)
        v_aug = b.sb("da_vaug", [128, NT, 132], BF16)
        fw.pool(lambda e: e.memset(v_aug[:, :, 128:129], 1.0), w=[v_aug])
        W_h = [b.sb("da_Wh%d" % j, [128, KC, 384], BF16) for j in range(1)]
        qk_s = [b.sb("da_qk%d" % sl, [128, 256], F32) for sl in range(NS)]
        qkn_s = [b.sb("da_qkn%d" % sl, [128, 256], F32) for sl in range(NS)]
        qkr_s = [b.sb("da_qkr%d" % sl, [128, 256], BF16) for sl in range(NS)]
        ssq_s = [b.sb("da_ssq%d" % sl, [128, 4], F32) for sl in range(NS)]
        Osb = [b.sb("da_O%d" % j, [128, 4, 132], F32) for j in range(2)]
        rr_s = [b.sb("da_rr%d" % sl, [128, 2], F32) for sl in range(2)]
        o_s = [b.sb("da_o%d" % sl, [128, 128], F32) for sl in range(4)]
        ob_s = [b.sb("da_ob%d" % sl, [128, 128], BF16) for sl in range(4)]
        oss_s = [b.sb("da_oss%d" % sl, [128, 1], F32) for sl in range(4)]
        junk_s = [b.sb("da_junk%d" % sl, [128, 128], BF16) for sl in range(4)]
        oT_s = [b.sb("da_oT%d" % sl, [128, 1, 128], BF16) for sl in range(4)]
        w_in = dr["da_w_in"][0]
        blocks, lat = qblocks(cfg, out_tiles)
        for i0 in range(0, len(lat), 4):
            blocks.append((lat[i0:i0 + 4], all_tiles))
        for hd in range(8):
            Wh = W_h[0]
            w_out = w_out2[hd % 2]
            for j in range(3):
                fw.dma("gpsimd", Wh[:, :, j * 128:(j + 1) * 128],
                       w_in[:, j * 1024 + hd * 128:j * 1024 + (hd + 1) * 128].rearrange("(kc p) f -> p kc f", p=128), w=[(Wh, j)])
            fw.dma("gpsimd", w_out[:], dr["da_w_out"][0][hd * 128:(hd + 1) * 128, :].rearrange("(kc p) f -> p kc f", p=128), w=[w_out])
            fw.dve(lambda e, w_out=w_out: e.tensor_scalar(out=w_out[:], in0=w_out[:], scalar1=sgcol[:, 0:1], scalar2=None, op0=ALU.mult),
                   r=[w_out, sgcol], w=[w_out])

            def pre(t, sl):
                qk, qkn, qkr, ssq = qk_s[sl], qkn_s[sl], qkr_s[sl], ssq_s[sl]
                b.sq_tmp = sq_tmps[sl]
                b.rope_tmp = rope_tmps[sl]
                p = b.psum()
                lin_tok(b, b.xnT, t, Wh, 0, 384, p)
                fw.act(lambda e: e.copy(out=qk[:], in_=p[:, 0:256]), r=[p], w=[qk])
                fw.act(lambda e: e.copy(out=v_aug[:, t, 0:128], in_=p[:, 256:384]), r=[p], w=[(v_aug, t)])
                group_rms(b, qk, 4, 64, ssq, [qk], [ssq])
                fw.dve(lambda e: e.tensor_tensor(out=qkn[:].rearrange("p (g d) -> p g d", g=4), in0=qk[:].rearrange("p (g d) -> p g d", g=4),
                                                 in1=ssq[:, 0:4].unsqueeze(2).to_broadcast([128, 4, 64]), op=ALU.mult), r=[qk, ssq], w=[qkn])
                fw.pool(lambda e: e.tensor_tensor(out=qkn[:], in0=qkn[:], in1=qkg[:], op=ALU.mult), r=[qkn, qkg], w=[qkn])
                rope_apply(b, qkr[:].rearrange("p (g two h) -> p g two h", g=4, two=2), qkn[:].rearrange("p (g two h) -> p g two h", g=4, two=2),
                           cs[:, t, :], 4, 32, [qkn, cs], [qkr])
                transposes(b, qkr, 2, lambda: qkT[:, :, t * 128:(t + 1) * 128], [qkr], [(qkT, t)])

            fw.lockstep(all_tiles, NS, pre)
            for (qts, kts) in blocks:
                for comp in range(2):
                    sl = slice(comp * 64, (comp + 1) * 64)
                    attn_core(b, lambda c0, n, sl=sl: qkT[sl, 0, c0:c0 + n], lambda c0, n, sl=sl: qkT[sl, 1, c0:c0 + n],
                              lambda kt: v_aug[:, kt, 0:129], 128, qts, kts, 0.125, Osb[comp], qkT, qkT, v_aug)

                def post(item, sl):
                    qi, t = item
                    rr, o, ob, oss, junk, oT = rr_s[sl], o_s[sl], ob_s[sl], oss_s[sl], junk_s[sl], oT_s[sl]
                    k = 1 if t < cfg.NTC else 0
                    b.rtmp_rr = sl * 2
                    fw.dve(lambda e: e.reciprocal(out=rr[:, 0:1], in_=Osb[0][:, qi, 128:129]), r=[(Osb[0], qi)], w=[rr])
                    fw.dve(lambda e: e.reciprocal(out=rr[:, 1:2], in_=Osb[1][:, qi, 128:129]), r=[(Osb[1], qi)], w=[rr])
                    fw.dve(lambda e: e.tensor_tensor(out=rr[:, 1:2], in0=rr[:, 1:2], in1=nlam[:, 0:1], op=ALU.mult), r=[rr, nlam], w=[rr])
                    fw.dve(lambda e: e.tensor_scalar(out=o[:], in0=Osb[0][:, qi, 0:128], scalar1=rr[:, 0:1], scalar2=None, op0=ALU.mult),
                           r=[(Osb[0], qi), rr], w=[o])
                    fw.dve(lambda e: e.scalar_tensor_tensor(out=o[:], in0=Osb[1][:, qi, 0:128], scalar=rr[:, 1:2], in1=o[:],
                                                            op0=ALU.mult, op1=ALU.add), r=[(Osb[1], qi), rr, o], w=[o])
                    fw.pool(lambda e: e.memset(oss[:], 0.0), w=[oss])
                    fw.act(lambda e: e.activation(out=junk[:], in_=o[:], func=AF.Square, accum_out=oss[:, 0:1]), r=[o, oss], w=[junk, oss])
                    rstd_op(b, oss[:, 0:1], oss[:, 0:1], 1.0 / 128, [oss], [oss])
                    fw.dve(lambda e: e.tensor_scalar(out=ob[:], in0=o[:], scalar1=oss[:, 0:1], scalar2=None, op0=ALU.mult), r=[o, oss], w=[ob])
                    transposes(b, ob, 1, lambda: oT[:], [ob], [oT])
                    out_proj_resid(b, li, t, oT, w_out, 1, Gt[k], oT)

                fw.lockstep(list(enumerate(qts)), 2, post)


MIXERS[0] = diff_attn


def mla(b, li, all_tiles, out_tiles):
    fw, cfg = b.fw, b.cfg
    dr = b.dram
    NT = cfg.NT
    with b.scope():
        need_ctx = any(t < cfg.NTC for t in out_tiles)
        Gt = gate_tiles(b, li, 2, need_ctx)
        cqnT = b.sb("cqnT", [128, 3, NT * 128], BF16)
        ckvnT = b.sb("ckvnT", [128, 2, NT * 128], BF16)
        kpe = b.sb("kpe", [128, NT, 32], F32)
        b.sq_tmp = b.sb("sqtmp", [128, 512], F32)
        ssq = b.sb("ml_ssq", [128, 8], F32)
        with b.scope():
            b.xnT = b.sb("xnT", [128, KC, NT * 128], BF16)
            norm_mod(b, li, 1, all_tiles)
            w_in = load_w(b, "ml_win", dr["mla_w_in"][0], KC, 672)
            qng = row_bcast_tile(b, "ml_qng", dr["mla_q_norm_g"][0:1, :], 384)
            kvng = row_bcast_tile(b, "ml_kvng", dr["mla_kv_norm_g"][0:1, :], 256)
            cq = b.sb("ml_cq", [128, 384], F32)
            ckv = b.sb("ml_ckv", [128, 288], F32)
            cqb = b.sb("ml_cqb", [128, 384], BF16)
            ckvb = b.sb("ml_ckvb", [128, 256], BF16)
            for t in all_tiles:
                p0 = b.psum()
                lin_tok(b, b.xnT, t, w_in, 0, 384, p0)
                p1 = b.psum()
                lin_tok(b, b.xnT, t, w_in, 384, 288, p1)
                fw.act(lambda e, p0=p0: e.copy(out=cq[:], in_=p0[:, 0:384]), r=[p0], w=[cq])
                fw.act(lambda e, p1=p1: e.copy(out=ckv[:], in_=p1[:, 0:288]), r=[p1], w=[ckv])
                fw.pool(lambda e, t=t: e.tensor_copy(out=kpe[:, t, :], in_=ckv[:, 256:288]), r=[ckv], w=[(kpe, t)])
                group_rms(b, cq, 1, 384, ssq, [cq], [ssq])
                fw.dve(lambda e: e.scalar_tensor_tensor(out=cqb[:], in0=cq[:], scalar=ssq[:, 0:1], in1=qng[:], op0=ALU.mult, op1=ALU.mult),
                       r=[cq, ssq, qng], w=[cqb])
                transposes(b, cqb, 3, lambda t=t: cqnT[:, :, t * 128:(t + 1) * 128], [cqb], [(cqnT, t)])
                group_rms(b, ckv, 1, 256, ssq, [ckv], [ssq])
                fw.dve(lambda e: e.scalar_tensor_tensor(out=ckvb[:], in0=ckv[:, 0:256], scalar=ssq[:, 0:1], in1=kvng[:], op0=ALU.mult, op1=ALU.mult),
                       r=[ckv, ssq, kvng], w=[ckvb])
                transposes(b, ckvb, 2, lambda t=t: ckvnT[:, :, t * 128:(t + 1) * 128], [ckvb], [(ckvnT, t)])
        with b.scope():
            qg = row_bcast_tile(b, "ml_qg", dr["mla_q_g"][0:1, :], 96)
            kg = row_bcast_tile(b, "ml_kg", dr["mla_k_g"][0:1, :], 96)
            cs = b.sb("ml_cs", [128, NT, 32], F32)
            fw.dma("sync", cs[:], dr["c_rope32"].rearrange("(t p) c -> p t c", p=128), w=[cs])
            b.pexp = [b.sb("pexp%d" % j, [128, 512], BF16) for j in range(2)]
            b.sc_rr = 0
            qT_g = b.sb("ml_qT", [128, 4, NT * 128], BF16)
            kT_g = b.sb("ml_kT", [128, 4, NT * 128], BF16)
            v_aug = b.sb("ml_vaug", [128, NT, 4, 66], BF16)
            fw.pool(lambda e: e.memset(v_aug[:, :, :, 64:65], 1.0), w=[v_aug])
            wuq = [b.sb("ml_wuq%d" % j, [128, 3, 384], BF16) for j in range(2)]
            wukv = [b.sb("ml_wukv%d" % j, [128, 2, 512], BF16) for j in range(2)]
            wout = [b.sb("ml_wout%d" % j, [128, 2, D], BF16) for j in range(2)]
            qs_s = [b.sb("ml_qs%d" % sl, [128, 384], F32) for sl in range(2)]
            ks_s = [b.sb("ml_ks%d" % sl, [128, 384], F32) for sl in range(2)]
            qr_s = [b.sb("ml_qr%d" % sl, [128, 384], BF16) for sl in range(2)]
            kr_s = [b.sb("ml_kr%d" % sl, [128, 384], BF16) for sl in range(2)]
            ssq_s = [b.sb("ml_ssq%d" % sl, [128, 8], F32) for sl in range(2)]
            sq_s = [b.sb("ml_sq%d" % sl, [128, 384], F32) for sl in range(2)]
            ropet_s = [[b.sb("ml_ropet%d_%d" % (sl, jj), [128, 64], F32) for jj in range(2)] for sl in range(2)]
            Osb = [b.sb("ml_O%d" % j, [128, 4, 68], F32) for j in range(2)]
            rr_s = [b.sb("ml_rr%d" % sl, [128, 2], F32) for sl in range(2)]
            opair_s = [b.sb("ml_opair%d" % sl, [128, 128], BF16) for sl in range(2)]
            oT_s = [b.sb("ml_oT%d" % sl, [128, 1, 128], BF16) for sl in range(2)]
            blocks, lat = qblocks(cfg, out_tiles)
            for i0 in range(0, len(lat), 4):
                blocks.append((lat[i0:i0 + 4], all_tiles))
            for hg in range(4):
                j = hg % 2
                fw.dma("gpsimd", wuq[j][:], dr["mla_w_uq"][0][:, hg * 384:(hg + 1) * 384].rearrange("(kc p) f -> p kc f", p=128), w=[wuq[j]])
                fw.dma("gpsimd", wukv[j][:], dr["mla_w_ukv"][0][:, hg * 512:(hg + 1) * 512].rearrange("(kc p) f -> p kc f", p=128), w=[wukv[j]])
                fw.dma("gpsimd", wout[j][:], dr["mla_w_out"][0][hg * 256:(hg + 1) * 256, :].rearrange("(kc p) f -> p kc f", p=128), w=[wout[j]])
                def pre2(t, sl):
                    qs, ks, qr, kr, ssq = qs_s[sl], ks_s[sl], qr_s[sl], kr_s[sl], ssq_s[sl]
                    b.sq_tmp = sq_s[sl]
                    b.rope_tmp = ropet_s[sl]
                    pb = b.psb[sl]
                    pq = b.psum()
                    lin_tok(b, cqnT, t, wuq[j], 0, 384, pq, kchunks=3)
                    pkv = b.psum()
                    lin_tok(b, ckvnT, t, wukv[j], 0, 512, pkv, kchunks=2)
                    fw.act(lambda e: e.copy(out=qs[:], in_=pq[:, 0:384]), r=[pq], w=[qs])
                    pkv4 = pkv[:, :].rearrange("p (h d) -> p h d", h=4)
                    fw.act(lambda e: e.copy(out=ks[:].rearrange("p (h d) -> p h d", h=4)[:, :, 0:64], in_=pkv4[:, :, 0:64]),
                           r=[pkv], w=[(ks, 0)])
                    fw.pool(lambda e: e.tensor_copy(out=ks[:].rearrange("p (h d) -> p h d", h=4)[:, :, 64:96],
                                                    in_=kpe[:, t, :].unsqueeze(1).to_broadcast([128, 4, 32])), r=[(kpe, t)], w=[(ks, 1)])
                    fw.act(lambda e: e.copy(out=v_aug[:, t, :, 0:64], in_=pkv4[:, :, 64:128]), r=[pkv], w=[(v_aug, t)])
                    for (src, gain, dst, dstT) in ((qs, qg, qr, qT_g), (ks, kg, kr, kT_g)):
                        group_rms(b, src, 4, 96, ssq, [src], [ssq])
                        s4 = src[:].rearrange("p (h d) -> p h d", h=4)
                        fw.dve(lambda e, s4=s4: e.tensor_tensor(out=s4, in0=s4, in1=ssq[:, 0:4].unsqueeze(2).to_broadcast([128, 4, 96]), op=ALU.mult),
                               r=[src, ssq], w=[src])
                        fw.pool(lambda e, s4=s4, gain=gain: e.tensor_tensor(out=s4, in0=s4, in1=gain[:].unsqueeze(1).to_broadcast([128, 4, 96]), op=ALU.mult),
                                r=[src, gain], w=[src])
                        d4 = dst[:].rearrange("p (h d) -> p h d", h=4)
                        fw.act(lambda e, s4=s4, d4=d4: e.copy(out=d4[:, :, 0:64], in_=s4[:, :, 0:64]), r=[src], w=[(dst, 0)])
                        rope_apply(b, d4[:, :, 64:96].rearrange("p h (two x) -> p h two x", two=2),
                                   s4[:, :, 64:96].rearrange("p h (two x) -> p h two x", two=2), cs[:, t, :], 4, 16, [src, cs], [(dst, 1)])
                        for h in range(4):
                            fw.pe(lambda e, h=h, dst=dst: e.transpose(out=pb[0:96, h * 128:(h + 1) * 128], in_=dst[:, h * 96:(h + 1) * 96],
                                                                     identity=b.identb[:]), r=[dst, b.identb], w=[pb])
                        fw.act(lambda e, dstT=dstT: e.copy(out=dstT[0:96, :, t * 128:(t + 1) * 128],
                                                           in_=pb[0:96, 0:512].rearrange("p (k q) -> p k q", k=4)), r=[pb], w=[(dstT, t)])

                fw.lockstep(all_tiles, 2, pre2)
                for pair in range(2):
                    for (qts, kts) in blocks:
                        for hh in range(2):
                            h = pair * 2 + hh
                            attn_core(b, lambda c0, n, h=h: qT_g[0:96, h, c0:c0 + n], lambda c0, n, h=h: kT_g[0:96, h, c0:c0 + n],
                                      lambda kt, h=h: v_aug[:, kt, h, 0:65], 64, qts, kts, 96 ** -0.5, Osb[hh], qT_g, kT_g, v_aug)

                        def post2(item, sl):
                            qi, t = item
                            rr, opair, oT = rr_s[sl], opair_s[sl], oT_s[sl]
                            k = 1 if t < cfg.NTC else 0
                            b.rtmp_rr = sl * 2
                            for hh in range(2):
                                fw.dve(lambda e, hh=hh: e.reciprocal(out=rr[:, hh:hh + 1], in_=Osb[hh][:, qi, 64:65]), r=[(Osb[hh], qi)], w=[(rr, hh)])
                                fw.dve(lambda e, hh=hh: e.tensor_scalar(out=opair[:, hh * 64:(hh + 1) * 64], in0=Osb[hh][:, qi, 0:64],
                                                                       scalar1=rr[:, hh:hh + 1], scalar2=None, op0=ALU.mult),
                                       r=[(Osb[hh], qi), (rr, hh)], w=[(opair, hh)])
                            pbs = b.psb[sl]
                            fw.pe(lambda e: e.transpose(out=pbs[:, 0:128], in_=opair[:, 0:128], identity=b.identb[:]), r=[opair, b.identb], w=[pbs])
                            fw.act(lambda e: e.copy(out=oT[:], in_=pbs[:, 0:128].rearrange("p (k q) -> p k q", k=1)), r=[pbs], w=[oT])
                            out_proj_resid(b, li, t, oT, T(wout[j][:, pair:pair + 1, :], wout[j].name), 1, Gt[k], oT)

                        fw.lockstep(list(enumerate(qts)), 2, post2)


MIXERS[3] = mla


def mamba(b, li, all_tiles, out_tiles):
    fw, cfg, nc = b.fw, b.cfg, b.nc
    dr = b.dram
    NT, NTC = cfg.NT, cfg.NTC
    S_CTX, S_LAT = cfg.S_CTX, cfg.S_LAT
    w_in = dr["ssm_w_in"][0]
    if not hasattr(b, "yf_d"):
        b.yf_d = nc.dram_tensor("ssm_yf_scratch", [NT * 128, 512], F32, kind="Internal").ap()
        b.yg_d = nc.dram_tensor("ssm_yg_scratch", [NT * 128, 2048], BF16, kind="Internal").ap()
    yf_d, yg_d = b.yf_d, b.yg_d
    with b.scope():
        need_ctx = any(t < NTC for t in out_tiles)
        ssqg = b.sb("sm_ssqg", [128, NT, 4], F32)
        fw.pool(lambda e: e.memset(ssqg[:], 0.0), w=[ssqg])
        with b.scope():
            b.xnT = b.sb("xnT", [128, KC, NT * 128], BF16)
            norm_mod(b, li, 1, all_tiles)
            dt_all = b.sb("sm_dt", [128, NT, 64], F32)
            a_all = b.sb("sm_a", [128, NT, 64], F32)
            dskB = b.sb("sm_dsk", [128, 32], F32)
            with b.scope():
                Wdt = load_w(b, "sm_wdt", w_in[:, 5120:5184], KC, 64)
                rows = b.sb("sm_rows", [1, 160], F32)
                fw.dma("sync", rows[0:1, 0:64], dr["ssm_dt_bias"][0:1].rearrange("a d h -> a (d h)"), w=[(rows, 0)])
                fw.dma("sync", rows[0:1, 64:128], dr["ssm_a_log"][0:1].rearrange("a d h -> a (d h)"), w=[(rows, 1)])
                fw.dma("sync", rows[0:1, 128:160], dr["ssm_d"][0:1, :], w=[(rows, 2)])
                fw.act(lambda e: e.activation(out=rows[0:1, 64:128], in_=rows[0:1, 64:128], func=AF.Exp), r=[(rows, 1)], w=[(rows, 1)])
                fw.dve(lambda e: e.tensor_scalar(out=rows[0:1, 64:128], in0=rows[0:1, 64:128], scalar1=-1.0, scalar2=None, op0=ALU.mult),
                       r=[(rows, 1)], w=[(rows, 1)])
                cB = b.sb("sm_cB", [128, 160], F32)
                b.bcast_rows(cB, 0, (rows, rows), 160)
                fw.dve(lambda e: e.tensor_copy(out=dskB[:], in_=cB[:, 128:160]), r=[cB], w=[dskB])
                xr = b.sb("sm_xr", [128, 64], F32)
                ax = b.sb("sm_ax", [128, 64], F32)
                for t in all_tiles:
                    p = b.psum()
                    lin_tok(b, b.xnT, t, Wdt, 0, 64, p)
                    fw.dve(lambda e, p=p: e.tensor_tensor(out=xr[:], in0=p[:, 0:64], in1=cB[:, 0:64], op=ALU.add), r=[p, cB], w=[xr])
                    fw.act(lambda e: e.activation(out=ax[:], in_=xr[:], func=AF.Abs), r=[xr], w=[ax])
                    fw.act(lambda e: e.activation(out=ax[:], in_=ax[:], func=AF.Exp, scale=-1.0), r=[ax], w=[ax])
                    fw.act(lambda e: e.activation(out=ax[:], in_=ax[:], func=AF.Ln, bias=b.ones_f[:, 0:1], scale=1.0), r=[ax, b.ones_f], w=[ax])
                    fw.dve(lambda e, t=t: e.scalar_tensor_tensor(out=dt_all[:, t, :], in0=xr[:], scalar=0.0, in1=ax[:], op0=ALU.max, op1=ALU.add),
                           r=[xr, ax], w=[(dt_all, t)])
                    fw.pool(lambda e, t=t: e.tensor_tensor(out=a_all[:, t, :], in0=dt_all[:, t, :], in1=cB[:, 64:128], op=ALU.mult),
                            r=[(dt_all, t), cB], w=[(a_all, t)])
            for g in range(4):
                with b.scope():
                    _mamba_group(b, li, g, all_tiles, dt_all, a_all, dskB, ssqg, w_in, yf_d, yg_d)
        with b.scope():
            Gt = gate_tiles(b, li, 2, need_ctx)
            w_out = load_w(b, "sm_wout", dr["ssm_w_out"][0], 16, D)
            OG = row_bcast_tile(b, "sm_og", dr["ssm_out_g"][0:1, :], 2048)
            rstd = b.sb("sm_rstd", [128, NT], F32)
            fw.dve(lambda e: e.tensor_reduce(out=rstd[:], in_=ssqg[:], axis=AX.X, op=ALU.add), r=[ssqg], w=[rstd])
            rstd_op(b, rstd[:], rstd[:], 1.0 / 2048, [rstd], [rstd])
            ygt = [b.sb("sm_ygt%d" % j, [128, 2048], BF16) for j in range(2)]
            ygn = b.sb("sm_ygn", [128, 2048], BF16)
            ygT = [b.sb("sm_ygT%d" % j, [128, 16, 128], BF16) for j in range(2)]
            for j, t in enumerate(out_tiles):
                k = 1 if t < NTC else 0
                yt, yT = ygt[j % 2], ygT[j % 2]
                fw.dma("sync", yt[:], yg_d[t * 128:(t + 1) * 128, :], r=[("yg_d", t)], w=[yt])
                fw.dve(lambda e, yt=yt, t=t: e.scalar_tensor_tensor(out=ygn[:], in0=yt[:], scalar=rstd[:, t:t + 1], in1=OG[:],
                                                                  op0=ALU.mult, op1=ALU.mult), r=[yt, rstd, OG], w=[ygn])
                for hf in range(2):
                    transposes(b, T(ygn[:, hf * 1024:(hf + 1) * 1024], ygn.name), 8,
                               lambda yT=yT, hf=hf: yT[:, hf * 8:(hf + 1) * 8, :], [ygn], [(yT, hf)])
                out_proj_resid(b, li, t, yT, w_out, 16, Gt[k], yT)


def _mamba_group(b, li, g, all_tiles, dt_all, a_all, dskB, ssqg, w_in, yf_d, yg_d):
    fw, cfg = b.fw, b.cfg
    dr = b.dram
    NT, NTC = cfg.NT, cfg.NTC
    S_CTX, S_LAT = cfg.S_CTX, cfg.S_LAT
    NTOK = NT * 128
    xs_tok = b.sb("sm_xs", [128, NT, 512], BF16)
    BT = b.sb("sm_BT", [128, NTOK], BF16)
    CT = b.sb("sm_CT", [128, NTOK], BF16)
    B_tok = b.sb("sm_Btok", [128, NT, 128], BF16)
    with b.scope():
        XR = S_CTX + S_LAT + 8
        xrow = b.sb("sm_xrow", [128, XR], F32)
        acc = b.sb("sm_acc", [128, NTOK], F32)
        conv_o = b.sb("sm_convo", [128, NTOK], BF16)
        fw.pool(lambda e: e.memset(xrow[:], 0.0), w=[xrow])
        segs = [(0, S_CTX, 2), (S_CTX, S_LAT, S_CTX + 6)]
        Wcs = [b.sb("sm_Wc%d" % j, [128, KC, 128], BF16) for j in range(2)]
        cwrow = b.sb("sm_cwrow", [6, 128], F32)
        cw = b.sb("sm_cw", [128, 6], F32)
        chunks = [("xs", g * 512 + cc * 128, cc) for cc in range(4)] + [("B", 2048 + g * 128, 0), ("C", 2560 + g * 128, 0)]
        for ci, (kind, cch, cc) in enumerate(chunks):
            Wc = Wcs[ci % 2]
            fw.dma("gpsimd", Wc[:], w_in[:, 2048 + cch:2048 + cch + 128].rearrange("(kc p) f -> p kc f", p=128), w=[Wc])
            fw.dma("sync", cwrow[0:5, :], dr["ssm_conv_w"][0][:, cch:cch + 128], w=[(cwrow, 0)])
            fw.dma("sync", cwrow[5:6, :], dr["ssm_conv_b"][0:1, cch:cch + 128], w=[(cwrow, 1)])
            pc = b.psum()
            fw.pe(lambda e, pc=pc: e.matmul(pc[:, 0:6], lhsT=cwrow[:, :], rhs=b.identf[0:6, 0:6], start=True, stop=True),
                  r=[cwrow, b.identf], w=[pc])
            fw.act(lambda e, pc=pc: e.copy(out=cw[:], in_=pc[:, 0:6]), r=[pc], w=[cw])
            dst = conv_o if kind == "xs" else (BT if kind == "B" else CT)
            for (tok0, ln, xoff) in segs:
                for c0 in range(0, ln, 512):
                    n = min(512, ln - c0)
                    p = b.psum()
                    for kc in range(KC):
                        fw.pe(lambda e, p=p, kc=kc, Wc=Wc, a0=tok0 + c0, n=n: e.matmul(p[:, 0:n], lhsT=Wc[:, kc, :], rhs=b.xnT[:, kc, a0:a0 + n],
                                                                                     start=(kc == 0), stop=(kc == KC - 1)), r=[Wc, b.xnT], w=[p])
                    fw.act(lambda e, p=p, x0=xoff + c0, n=n: e.copy(out=xrow[:, x0:x0 + n], in_=p[:, 0:n]), r=[p], w=[xrow])
                fw.dve(lambda e, tok0=tok0, ln=ln, xoff=xoff: e.tensor_scalar(out=acc[:, tok0:tok0 + ln], in0=xrow[:, xoff - 2:xoff - 2 + ln],
                                                                             scalar1=cw[:, 0:1], scalar2=None, op0=ALU.mult),
                       r=[xrow, cw], w=[acc])
                for j in range(1, 5):
                    fw.dve(lambda e, j=j, tok0=tok0, ln=ln, xoff=xoff: e.scalar_tensor_tensor(
                        out=acc[:, tok0:tok0 + ln], in0=xrow[:, xoff - 2 + j:xoff - 2 + j + ln], scalar=cw[:, j:j + 1],
                        in1=acc[:, tok0:tok0 + ln], op0=ALU.mult, op1=ALU.add), r=[xrow, cw, acc], w=[acc])
                fw.act(lambda e, tok0=tok0, ln=ln, dst=dst: e.activation(out=dst[:, tok0:tok0 + ln], in_=acc[:, tok0:tok0 + ln], func=AF.Silu,
                                                                        bias=cw[:, 5:6], scale=1.0), r=[acc, cw], w=[dst])
            if kind == "xs":
                for t in all_tiles:
                    transposes(b, T(conv_o[:, t * 128:(t + 1) * 128], conv_o.name), 1,
                               lambda t=t, cc=cc: xs_tok[:, t:t + 1, cc * 128:(cc + 1) * 128], [conv_o], [(xs_tok, t)])
            elif kind == "B":
                for t in all_tiles:
                    transposes(b, T(BT[:, t * 128:(t + 1) * 128], BT.name), 1, lambda t=t: B_tok[:, t:t + 1, :], [BT], [(B_tok, t)])
    with b.scope():
        Wz = load_w(b, "sm_wz", w_in[:, g * 512:(g + 1) * 512], KC, 512)
        S = b.sb("sm_S", [128, 512], F32)
        S_bf = b.sb("sm_Sbf", [128, 512], BF16)
        CBm = b.sb("sm_CBm", [128, 128], F32)
        aU = b.sb("sm_aU", [128, 8, 128], F32)
        Ex = b.sb("sm_Ex", [128, 8, 128], BF16)
        MT = b.sb("sm_MT", [128, 8, 128], BF16)
        xdt = b.sb("sm_xdt", [128, 512], BF16)
        ytmp = b.sb("sm_ytmp", [128, 512], F32)
        ygb = b.sb("sm_ygb", [128, 512], BF16)
        ct2 = [b.sb("sm_ct%d" % j, [128, 24], F32) for j in range(2)]
        ex2 = [b.sb("sm_ex%d" % j, [128, 24], F32) for j in range(2)]
        xdtd2 = [b.sb("sm_xdtd%d" % j, [128, 512], BF16) for j in range(2)]
        ydg2 = [b.sb("sm_ydg%d" % j, [128, 512], F32) for j in range(2)]
        zs2 = [b.sb("sm_zs%d" % j, [128, 512], BF16) for j in range(2)]
        yfl2 = [b.sb("sm_yfl%d" % j, [128, 512], F32) for j in range(2)]
        ctx_t = list(range(NTC))
        lat_t = list(range(NTC, NT))
        v8 = lambda ap: ap.rearrange("p (e q) -> p e q", e=8)

        def stageA(d, t, k):
            iU_r, iU_l = (0, 1) if d == 0 else (2, 3)
            c0 = d * 32 + g * 8
            tsl = slice(t * 128, (t + 1) * 128)
            a8 = a_all[:, t, c0:c0 + 8]
            dt8 = dt_all[:, t, c0:c0 + 8]
            ct, ex, xdtd, ydg, zs, yfl = ct2[k], ex2[k], xdtd2[k], ydg2[k], zs2[k], yfl2[k]
            pct = b.psum()
            fw.pe(lambda e: e.matmul(pct[:, 0:8], lhsT=b.tri[:, iU_r, :], rhs=a8, start=True, stop=True), r=[b.tri, (a_all, t)], w=[pct])
            fw.pe(lambda e: e.matmul(pct[:, 8:16], lhsT=b.ones_f[:, :], rhs=a8, start=True, stop=True), r=[b.ones_f, (a_all, t)], w=[pct])
            fw.act(lambda e: e.copy(out=ct[:, 0:16], in_=pct[:, 0:16]), r=[pct], w=[ct])
            fw.dve(lambda e: e.tensor_tensor(out=ct[:, 16:24], in0=ct[:, 8:16], in1=ct[:, 0:8], op=ALU.subtract), r=[ct], w=[ct])
            fw.act(lambda e: e.activation(out=ex[:], in_=ct[:], func=AF.Exp), r=[ct], w=[ex])
            pcb = b.psum()
            fw.pe(lambda e: e.matmul(pcb[:, 0:128], lhsT=BT[:, tsl], rhs=CT[:, tsl], start=True, stop=True), r=[BT, CT], w=[pcb])
            fw.dve(lambda e: e.tensor_tensor(out=CBm[:], in0=pcb[:, 0:128], in1=b.tri[:, iU_r, :], op=ALU.mult), r=[pcb, b.tri], w=[CBm])
            fw.pool(lambda e: e.tensor_tensor(out=aU[:], in0=a8.unsqueeze(2).to_broadcast([128, 8, 128]),
                                              in1=b.tri[:, iU_r, :].unsqueeze(1).to_broadcast([128, 8, 128]), op=ALU.mult),
                    r=[(a_all, t), b.tri], w=[aU])
            for hf in range(2):
                pd = b.psum()
                fw.pe(lambda e, pd=pd, hf=hf: e.matmul(pd[:, :], lhsT=b.tri[:, iU_l, :],
                                                      rhs=aU[:, hf * 4:(hf + 1) * 4, :].rearrange("p e t -> p (e t)"), start=True, stop=True),
                      r=[b.tri, aU], w=[pd])
                fw.act(lambda e, pd=pd, hf=hf: e.activation(out=Ex[:, hf * 4:(hf + 1) * 4, :].rearrange("p e t -> p (e t)"), in_=pd[:, :], func=AF.Exp),
                       r=[pd], w=[(Ex, hf)])
            fw.dve(lambda e: e.tensor_tensor(out=MT[:], in0=Ex[:], in1=CBm[:].unsqueeze(1).to_broadcast([128, 8, 128]), op=ALU.mult),
                   r=[Ex, CBm], w=[MT])
            fw.pool(lambda e: e.tensor_tensor(out=v8(xdt[:]), in0=v8(xs_tok[:, t, :]), in1=dt8.unsqueeze(2).to_broadcast([128, 8, 64]), op=ALU.mult),
                    r=[(xs_tok, t), (dt_all, t)], w=[xdt])
            fw.pool(lambda e: e.tensor_tensor(out=v8(xdtd[:]), in0=v8(xdt[:]), in1=ex[:, 16:24].unsqueeze(2).to_broadcast([128, 8, 64]), op=ALU.mult),
                    r=[xdt, ex], w=[xdtd])
            pyd = b.psum()
            for e8 in range(8):
                fw.pe(lambda e, e8=e8: e.matmul(pyd[:, e8 * 64:(e8 + 1) * 64], lhsT=MT[:, e8, :], rhs=xdt[:, e8 * 64:(e8 + 1) * 64],
                                                start=True, stop=True), r=[MT, xdt], w=[pyd])
            if d == 0:
                fw.act(lambda e: e.copy(out=ydg[:], in_=pyd[:, :]), r=[pyd], w=[ydg])
            else:
                fw.dma("sync", yfl[:], yf_d[tsl, :], r=[("yf_d", t)], w=[yfl])
                fw.dve(lambda e: e.tensor_tensor(out=ydg[:], in0=pyd[:, :], in1=yfl[:], op=ALU.add), r=[pyd, yfl], w=[ydg])
                fw.pool(lambda e: e.tensor_tensor(out=v8(yfl[:]), in0=v8(xs_tok[:, t, :]),
                                                  in1=dskB[:, g * 8:(g + 1) * 8].unsqueeze(2).to_broadcast([128, 8, 64]), op=ALU.mult),
                        r=[(xs_tok, t), dskB, yfl, ydg], w=[yfl])
                fw.pool(lambda e: e.tensor_tensor(out=ydg[:], in0=ydg[:], in1=yfl[:], op=ALU.add), r=[ydg, yfl], w=[ydg])
                pz = b.psum()
                lin_tok(b, b.xnT, t, Wz, 0, 512, pz)
                fw.act(lambda e: e.activation(out=zs[:], in_=pz[:, :], func=AF.Silu), r=[pz], w=[zs])

        def stageB(d, t, k):
            tsl = slice(t * 128, (t + 1) * 128)
            ex, xdtd, ydg, zs = ex2[k], xdtd2[k], ydg2[k], zs2[k]
            pyo = b.psum()
            fw.pe(lambda e: e.matmul(pyo[:, :], lhsT=CT[:, tsl], rhs=S_bf[:, :], start=True, stop=True), r=[CT, S_bf], w=[pyo])
            psn = b.psum()
            fw.pe(lambda e: e.matmul(psn[:, :], lhsT=B_tok[:, t, :], rhs=xdtd[:, :], start=True, stop=True), r=[(B_tok, t), xdtd], w=[psn])
            fw.dve(lambda e: e.tensor_tensor(out=v8(ytmp[:]), in0=v8(pyo[:, :]), in1=ex[:, 0:8].unsqueeze(2).to_broadcast([128, 8, 64]), op=ALU.mult),
                   r=[pyo, ex], w=[ytmp])
            fw.pool(lambda e: e.tensor_tensor(out=v8(S[:]), in0=v8(S[:]), in1=ex[:, 8:16].unsqueeze(2).to_broadcast([128, 8, 64]), op=ALU.mult),
                    r=[S, ex], w=[S])
            fw.dve(lambda e: e.tensor_tensor(out=S[:], in0=psn[:, :], in1=S[:], op=ALU.add), r=[psn, S], w=[S])
            fw.act(lambda e: e.copy(out=S_bf[:], in_=S[:]), r=[S], w=[S_bf])
            fw.dve(lambda e: e.tensor_tensor(out=ydg[:], in0=ydg[:], in1=ytmp[:], op=ALU.add), r=[ydg, ytmp], w=[ydg])
            if d == 0:
                fw.dma("sync", yf_d[tsl, :], ydg[:], r=[ydg], w=[("yf_d", t)])
            else:
                fw.dve(lambda e: e.tensor_tensor(out=ydg[:], in0=ydg[:], in1=zs[:], op=ALU.mult), r=[ydg, zs], w=[ydg])
                fw.act(lambda e: e.activation(out=ygb[:], in_=ydg[:], func=AF.Square, accum_out=ssqg[:, t, g:g + 1]),
                       r=[ydg], w=[ygb, (ssqg, t)])
                fw.pool(lambda e: e.tensor_copy(out=ygb[:], in_=ydg[:]), r=[ydg], w=[ygb])
                fw.dma("sync", yg_d[tsl, g * 512:(g + 1) * 512], ygb[:], r=[ygb], w=[("yg_d", t)])

        for d in range(2):
            order = (ctx_t + lat_t) if d == 0 else (ctx_t[::-1] + lat_t[::-1])
            fw.pool(lambda e: e.memset(S[:], 0.0), w=[S])
            fw.pool(lambda e: e.memset(S_bf[:], 0.0), w=[S_bf])
            stageA(d, order[0], 0)
            for i, t in enumerate(order):
                if i + 1 < len(order):
                    stageA(d, order[i + 1], (i + 1) % 2)
                stageB(d, t, i % 2)


MIXERS[2] = mamba


def _host_consts(cfg):
    ident = np.eye(128, dtype=np.float32)
    k = np.arange(128)[:, None]
    t = np.arange(128)[None, :]
    tri = np.stack([(k <= t), (k > t), (k >= t), (k < t)]).astype(np.float32)
    out = {"c_ident": ident, "c_tri": tri}
    out.update(host_tables(cfg))
    return out


def kernel(**inputs):
    from concourse.bass_utils import run_bass_kernel_spmd
    inp = {k: np.ascontiguousarray(np.asarray(v)) for k, v in inputs.items()}
    n_cores = 8
    NB = inp["x"].shape[0] // n_cores
    cfg = Cfg(S_LAT=inp["x"].shape[1], S_CTX=inp["ctx"].shape[1], NB=NB, kinds=[0, 1, 2, 3])
    wnames = [k for k in inp if k not in ("x", "c", "ctx", "c_ctx")]
    cfg.wshapes = {k: inp[k].shape for k in wnames}
    nc, b = build_program(cfg)
    consts = _host_consts(cfg)
    maps = []
    for core in range(n_cores):
        m = {k: inp[k] for k in wnames if k in b.dram}
        m.update({k: v for k, v in consts.items() if k in b.dram})
        m["x"] = inp["x"][core * NB:(core + 1) * NB]
        m["ctx"] = inp["ctx"][core * NB:(core + 1) * NB]
        m["c"] = inp["c"][core * NB:(core + 1) * NB]
        m["c_ctx"] = inp["c_ctx"][None, :]
        maps.append(m)
    res = run_bass_kernel_spmd(nc, maps, core_ids=list(range(n_cores)))
    return np.concatenate([r["out"] for r in res.results], axis=0).astype(np.float32)
```

```python
import os
import numpy as np
import concourse.bass as bass
import concourse.mybir as mybir

F32 = mybir.dt.float32
BF16 = mybir.dt.bfloat16
AF = mybir.ActivationFunctionType
ALU = mybir.AluOpType
AX = mybir.AxisListType

ENGS = ("tensor", "vector", "scalar", "gpsimd", "sync")
N_DMA_SEMS = 8


class _Op:
    __slots__ = ("eng", "fn", "deps", "chan", "is_dma", "needs_inc", "val", "idx")


class FW:
    def __init__(self, nc, same_engine_sync=True):
        self.nc = nc
        self.ops = []
        self.state = {}
        self.same_engine_sync = same_engine_sync
        self.dma_rr = {e: 0 for e in ENGS}
        self.chan_last = {}
        self.bar = {}

    def barrier(self):
        self.bar = dict(self.chan_last)

    def _entries(self, res):
        name, idx = res if isinstance(res, tuple) else (res, None)
        name = getattr(name, "name", name)
        ent = self.state.setdefault(name, {})
        return name, idx, ent

    def _collect(self, res, is_write, deps):
        name, idx, ent = self._entries(res)
        if idx is None:
            targets = list(ent.values())
        else:
            targets = [ent.get(idx), ent.get(None)]
        for e in targets:
            if e is None:
                continue
            if e[0] is not None:
                deps.add(e[0])
            if is_write:
                deps.update(e[1].values())

    def _update(self, res, is_write, op):
        name, idx, ent = self._entries(res)
        if is_write:
            if idx is None:
                ent.clear()
                ent[None] = [op.idx, {}]
            else:
                ent[idx] = [op.idx, {}]
        else:
            e = ent.get(idx)
            if e is None:
                e = ent[idx] = [None, {}]
            e[1][op.chan] = op.idx

    capture = None

    def op(self, eng, fn, r=(), w=(), dma=False):
        if self.capture is not None:
            self.capture.append((eng, fn, tuple(r), tuple(w), dma))
            return None
        o = _Op()
        o.idx = len(self.ops)
        o.eng = eng
        o.fn = fn
        o.is_dma = dma
        if dma:
            k = self.dma_rr[eng]
            self.dma_rr[eng] = (k + 1) % N_DMA_SEMS
            o.chan = "dma_%s_%d" % (eng, k)
        else:
            o.chan = eng
        o.needs_inc = dma
        o.val = None
        deps = set()
        for res in r:
            self._collect(res, False, deps)
        for res in w:
            self._collect(res, True, deps)
        best = {}
        for d in deps:
            c = self.ops[d].chan
            if c not in best or best[c] < d:
                best[c] = d
        for c, d in self.bar.items():
            if c not in best or best[c] < d:
                best[c] = d
        if dma and o.chan in self.chan_last:
            d = self.chan_last[o.chan]
            if o.chan not in best or best[o.chan] < d:
                best[o.chan] = d
        o.deps = best
        self.chan_last[o.chan] = o.idx
        for res in r:
            self._update(res, False, o)
        for res in w:
            self._update(res, True, o)
        self.ops.append(o)
        return o

    def lockstep(self, items, nslot, body):
        for i0 in range(0, len(items), nslot):
            lists = []
            for slot, it in enumerate(items[i0:i0 + nslot]):
                self.capture = []
                body(it, slot)
                lists.append(self.capture)
                self.capture = None
            idx = [0] * len(lists)
            more = True
            while more:
                more = False
                for L, lst in enumerate(lists):
                    if idx[L] < len(lst):
                        self.op(*lst[idx[L]])
                        idx[L] += 1
                        more = True

    def pe(self, fn, r=(), w=()):
        return self.op("tensor", fn, r, w)

    def dve(self, fn, r=(), w=()):
        return self.op("vector", fn, r, w)

    def act(self, fn, r=(), w=()):
        return self.op("scalar", fn, r, w)

    def pool(self, fn, r=(), w=()):
        return self.op("gpsimd", fn, r, w)

    def dma(self, eng, out, in_, r=(), w=(), **kw):
        return self.op(eng, lambda e: e.dma_start(out=out, in_=in_, **kw), r, w, dma=True)

    def _init_emit(self):
        import contextlib
        self._st = contextlib.ExitStack()
        chans = list(ENGS[:4]) + ["dma_%s_%d" % (e, k) for e in ("sync", "scalar", "gpsimd") for k in range(N_DMA_SEMS)]
        self.sems = {c: self._st.enter_context(self.nc.semaphore("s_" + c)) for c in chans}
        self.cnt = {c: 0 for c in chans}
        self.inc_idx = {c: [] for c in chans}
        self.inc_val = {c: [] for c in chans}
        self.waited = {e: {} for e in ENGS}
        self.flushed = 0

    def _skip_same(self, o, p):
        return p.chan == o.eng and not p.is_dma and (o.eng == "tensor" or not self.same_engine_sync)

    def flush(self):
        import bisect
        if not hasattr(self, "sems"):
            self._init_emit()
        ops = self.ops
        batch = ops[self.flushed:]
        if not batch:
            return
        start = self.flushed
        last_in_chan = {}
        for o in batch:
            last_in_chan[o.chan] = o
            for c, d in o.deps.items():
                p = ops[d]
                if d >= start and not self._skip_same(o, p):
                    p.needs_inc = True
        for o in last_in_chan.values():
            o.needs_inc = True
        for o in batch:
            if o.needs_inc:
                self.cnt[o.chan] += 16 if o.is_dma else 1
                o.val = self.cnt[o.chan]
                self.inc_idx[o.chan].append(o.idx)
                self.inc_val[o.chan].append(o.val)
        for o in batch:
            eng = getattr(self.nc, o.eng)
            waited = self.waited[o.eng]
            for c, d in o.deps.items():
                p = ops[d]
                if self._skip_same(o, p):
                    continue
                k = bisect.bisect_left(self.inc_idx[p.chan], d)
                val = self.inc_val[p.chan][k]
                if waited.get(p.chan, 0) >= val:
                    continue
                eng.wait_ge(self.sems[p.chan], val)
                waited[p.chan] = val
            ins = o.fn(eng)
            if o.needs_inc:
                ins.then_inc(self.sems[o.chan], 16 if o.is_dma else 1)
            o.fn = None
        self.flushed = len(ops)

    def emit(self, final_wait_ops=()):
        self.flush()
        eng = self.nc.sync
        for p in final_wait_ops:
            eng.wait_ge(self.sems[p.chan], p.val)
        self.sem_max = dict(self.cnt)
        self._st.close()


import contextlib
import math
import numpy as np
import concourse.bass as bass
import concourse.mybir as mybir

D = 1024
KC = 8
EPS = 1e-6


class T:
    def __init__(self, h, name):
        self.h = h
        self.name = name

    def __getitem__(self, k):
        return self.h[k]


class Cfg:
    def __init__(self, **kw):
        self.S_LAT = 2048
        self.S_CTX = 256
        self.NB = 2
        self.GROUPS = 4
        self.PER = 8
        self.kinds = [0, 1, 2, 3]
        self.want_ctx = [True, True, True, False]
        self.layer_ids = [0, 1, 2, 3]
        self.moe = True
        self.__dict__.update(kw)
        self.NTC = self.S_CTX // 128
        self.NTL = self.S_LAT // 128
        self.NT = self.NTC + self.NTL
        self.E = self.GROUPS * self.PER
        self.DEPTH = len(self.kinds)


class B:
    def __init__(self, nc, cfg):
        self.nc = nc
        self.cfg = cfg
        self.fw = FW(nc)
        self.uid = 0
        self.stacks = []
        self.dram = {}
        self.ps_rr = 0

    @contextlib.contextmanager
    def scope(self):
        st = contextlib.ExitStack()
        self.stacks.append(st)
        try:
            with st:
                yield
                self.fw.flush()
        finally:
            self.fw.flush()
            self.stacks.pop()
            self.fw.barrier()

    def sb(self, name, shape, dt):
        self.uid += 1
        nm = "%s_%d" % (name, self.uid)
        h = self.stacks[-1].enter_context(self.nc.sbuf_tensor(nm, list(shape), dt))
        return T(h, nm)

    def din(self, name, shape, dt=F32):
        ap = self.nc.dram_tensor(name, list(shape), dt, kind="ExternalInput").ap()
        self.dram[name] = ap
        return ap

    def psum(self):
        p = self.ps[self.ps_rr]
        self.ps_rr = (self.ps_rr + 1) % len(self.ps)
        return p

    def psumb(self):
        p = self.psb[self.psb_rr]
        self.psb_rr = (self.psb_rr + 1) % len(self.psb)
        return p

    def bcast_rows(self, dst, dst_cols, row_ap_f32, n, evac=None):
        fw = self.fw
        for c0 in range(0, n, 512):
            cw = min(512, n - c0)
            p = self.psum()
            fw.pe(lambda e, p=p, c0=c0, cw=cw: e.matmul(p[:, 0:cw], lhsT=self.ones_row[0:1, 0:128],
                                                      rhs=row_ap_f32[1][0:1, c0:c0 + cw], start=True, stop=True),
                  r=[self.ones_row, row_ap_f32[0]], w=[p])
            d0 = dst_cols + c0
            if evac is None:
                fw.act(lambda e, p=p, d0=d0, cw=cw: e.copy(out=dst[:, d0:d0 + cw], in_=p[:, 0:cw]), r=[p], w=[dst])
            else:
                evac(p, d0, cw)


def rstd_op(b, out, in_, scale, r, w):
    fw = b.fw
    fw.act(lambda e: e.activation(out=out, in_=in_, func=AF.Sqrt, bias=b.eps_col[0:out.shape[0], 0:1], scale=scale),
           r=list(r) + [b.eps_col], w=w)
    fw.dve(lambda e: e.reciprocal(out=out, in_=out), r=w, w=w)


def build_consts(b):
    nc, fw = b.nc, b.fw
    cfg = b.cfg
    ident_d = b.din("c_ident", [128, 128])
    tri_d = b.din("c_tri", [4, 128, 128])
    b.din("c_rope64", [cfg.S_CTX + cfg.S_LAT, 64])
    b.din("c_rope32", [cfg.S_CTX + cfg.S_LAT, 32])
    b.identb = b.sb("identb", [128, 128], BF16)
    b.identf = b.sb("identf", [128, 128], F32)
    b.ones_row = b.sb("ones_row", [1, 512], F32)
    b.ones_f = b.sb("ones_f", [128, 128], F32)
    b.tri = b.sb("tri", [128, 4, 128], F32)
    fw.dma("gpsimd", b.identb[:], ident_d, w=[b.identb])
    fw.dma("sync", b.identf[:], ident_d, w=[b.identf])
    fw.dma("sync", b.tri[:], tri_d.rearrange("a p q -> p a q"), w=[b.tri])
    fw.pool(lambda e: e.memset(b.ones_row[:], 1.0), w=[b.ones_row])
    fw.pool(lambda e: e.memset(b.ones_f[:], 1.0), w=[b.ones_f])
    b.eps_col = b.sb("eps_col", [128, 1], F32)
    fw.pool(lambda e: e.memset(b.eps_col[:], EPS), w=[b.eps_col])
    st = b.stacks[-1]
    b.ps = []
    for j in range(6):
        h = st.enter_context(nc.psum_tensor("psf%d" % j, [128, 512], F32))
        b.ps.append(T(h, "psf%d" % j))
    b.psb = []
    for j in range(2):
        h = st.enter_context(nc.psum_tensor("psb%d" % j, [128, 1024], BF16))
        b.psb.append(T(h, "psb%d" % j))
    b.psb_rr = 0


def load_cond(b, bi):
    with b.scope():
        _load_cond(b, bi)


def _load_cond(b, bi):
    fw = b.fw
    c_d, cctx_d = b.dram["c"], b.dram["c_ctx"]
    crow = b.sb("crow", [1, 2 * D], F32)
    fw.dma("sync", crow[0:1, 0:D], c_d[bi:bi + 1, :], w=[(crow, 0)])
    fw.dma("sync", crow[0:1, D:2 * D], cctx_d[0:1, :], w=[(crow, 1)])
    p = b.psum()
    for k in range(2):
        for kc in range(KC):
            fw.pe(lambda e, k=k, kc=kc: e.matmul(p[:, k * KC + kc:k * KC + kc + 1],
                                                lhsT=crow[0:1, k * D + kc * 128:k * D + (kc + 1) * 128],
                                                rhs=b.ones_row[0:1, 0:1], start=True, stop=True),
                  r=[crow, b.ones_row], w=[p])
    ccol = b.sb("ccol", [128, 2 * KC], F32)
    fw.act(lambda e: e.activation(out=ccol[:], in_=p[:, 0:2 * KC], func=AF.Silu), r=[p], w=[ccol])
    for k in range(2):
        fw.dve(lambda e, k=k: e.tensor_copy(out=b.scB[k][:],
                                           in_=ccol[:, k * KC:(k + 1) * KC].unsqueeze(2).to_broadcast([128, KC, 128])),
               r=[ccol], w=[b.scB[k]])


def mod_bcast(b, li, sec, dsts, add_one=False):
    fw = b.fw
    ada_w, ada_b = b.dram["ada_w"], b.dram["ada_b"]
    with b.scope():
        _mod_bcast(b, li, sec, dsts, add_one)


def _mod_bcast(b, li, sec, dsts, add_one):
    fw = b.fw
    ada_w, ada_b = b.dram["ada_w"], b.dram["ada_b"]
    brow = b.sb("brow", [1, D], F32)
    QW = 256
    wsts = [b.sb("wst%d" % j, [128, KC, QW], F32) for j in range(2)]
    fw.dma("sync", brow[:], ada_b[li:li + 1, sec * D:(sec + 1) * D], w=[brow])
    for q in range(D // QW):
        wst = wsts[q % 2]
        c0 = sec * D + q * QW
        fw.dma("sync", wst[:], ada_w[li, :, c0:c0 + QW].rearrange("(kc p) f -> p kc f", p=128), w=[wst])
        for k in range(2):
            if dsts[k] is None:
                continue
            p = b.psum()
            for kc in range(KC):
                fw.pe(lambda e, p=p, k=k, kc=kc, wst=wst: e.matmul(p[:, 0:QW], lhsT=b.scB[k][:, kc, :], rhs=wst[:, kc, :],
                                                                  start=(kc == 0), stop=False),
                      r=[b.scB[k], wst], w=[p])
            fw.pe(lambda e, p=p, q=q: e.matmul(p[:, 0:QW], lhsT=b.ones_row[0:1, 0:128], rhs=brow[0:1, q * QW:(q + 1) * QW],
                                              start=False, stop=True),
                  r=[b.ones_row, brow], w=[p])
            d = dsts[k]
            if add_one:
                fw.act(lambda e, p=p, d=d, q=q: e.activation(out=d[:, q * QW:(q + 1) * QW], in_=p[:, 0:QW],
                                                            func=AF.Identity, bias=b.one_col[:, 0:1], scale=1.0),
                       r=[p, b.one_col], w=[(d, q)])
            else:
                fw.act(lambda e, p=p, d=d, q=q: e.copy(out=d[:, q * QW:(q + 1) * QW], in_=p[:, 0:QW]),
                       r=[p], w=[(d, q)])


def norm_mod(b, li, which, tiles):
    fw, cfg = b.fw, b.cfg
    gname = "norm1_g" if which == 1 else "norm2_g"
    with b.scope():
        A = [b.sb("A%d" % k, [128, D], F32) for k in range(2)]
        S = [b.sb("S%d" % k, [128, D], F32) for k in range(2)]
        G = b.sb("Gn", [128, D], F32)
        grow = b.sb("grow", [1, D], F32)
        need_ctx = any(t < cfg.NTC for t in tiles)
        sec0 = 0 if which == 1 else 3
        fw.dma("sync", grow[:], b.dram[gname][li:li + 1, :], w=[grow])
        b.bcast_rows(G, 0, (grow, grow), D)
        dA = [A[0], A[1] if need_ctx else None]
        dS = [S[0], S[1] if need_ctx else None]
        mod_bcast(b, li, sec0 + 1, dA, add_one=True)
        mod_bcast(b, li, sec0 + 0, dS)
        for k in range(2):
            if dA[k] is not None:
                fw.dve(lambda e, k=k: e.tensor_mul(out=A[k][:], in0=A[k][:], in1=G[:]), r=[A[k], G], w=[A[k]])
        junk = b.sb("junk", [128, D], BF16)
        ss = b.sb("ss", [128, cfg.NT], F32)
        rstd = b.sb("rstd", [128, cfg.NT], F32)
        fw.pool(lambda e: e.memset(ss[:], 0.0), w=[ss])
        tmp = [b.sb("nt%d" % j, [128, D], F32) for j in range(2)]
        xnb = [b.sb("xnb%d" % j, [128, D], BF16) for j in range(2)]
        for j, t in enumerate(tiles):
            k = 1 if t < cfg.NTC else 0
            fw.act(lambda e, t=t: e.activation(out=junk[:], in_=b.h[:, t, :], func=AF.Square, accum_out=ss[:, t:t + 1]),
                   r=[(b.h, t)], w=[junk, (ss, t)])
            rstd_op(b, rstd[:, t:t + 1], ss[:, t:t + 1], 1.0 / D, [(ss, t)], [(rstd, t)])
            tm, xb = tmp[j % 2], xnb[j % 2]
            fw.dve(lambda e, t=t, tm=tm, k=k: e.scalar_tensor_tensor(out=tm[:], in0=b.h[:, t, :], scalar=rstd[:, t:t + 1],
                                                                    in1=A[k][:], op0=ALU.mult, op1=ALU.mult),
                   r=[(b.h, t), (rstd, t), A[k]], w=[tm])
            fw.pool(lambda e, tm=tm, xb=xb, k=k: e.tensor_tensor(out=xb[:], in0=tm[:], in1=S[k][:], op=ALU.add),
                    r=[tm, S[k]], w=[xb])
            pb = b.psumb()
            for kc in range(KC):
                fw.pe(lambda e, pb=pb, kc=kc, xb=xb: e.transpose(out=pb[:, kc * 128:(kc + 1) * 128],
                                                                in_=xb[:, kc * 128:(kc + 1) * 128], identity=b.identb[:]),
                      r=[xb, b.identb], w=[pb])
            fw.act(lambda e, pb=pb, t=t: e.copy(out=b.xnT[:, :, t * 128:(t + 1) * 128],
                                               in_=pb[:, :].rearrange("p (k q) -> p k q", k=KC)),
                   r=[pb], w=[(b.xnT, t)])


def gate_tiles(b, li, sec, need_ctx):
    Gt = [b.sb("Gt0", [128, D], F32), b.sb("Gt1", [128, D], F32) if need_ctx else None]
    mod_bcast(b, li, sec, Gt)
    return Gt


def resid_add(b, t, p, hf, Gk):
    fw = b.fw
    tm = b.rtmp[b.rtmp_rr]
    b.rtmp_rr = (b.rtmp_rr + 1) % len(b.rtmp)
    sl = slice(hf * 512, (hf + 1) * 512)
    fw.dve(lambda e: e.tensor_tensor(out=tm[:], in0=p[:, :], in1=Gk[:, sl], op=ALU.mult), r=[p, Gk], w=[tm])
    fw.pool(lambda e: e.tensor_tensor(out=b.h[:, t, sl], in0=b.h[:, t, sl], in1=tm[:], op=ALU.add),
            r=[tm, (b.h, t)], w=[(b.h, t)])


def moe(b, li, tiles):
    fw, cfg = b.fw, b.cfg
    E, NT = cfg.E, cfg.NT
    NG, PER = cfg.GROUPS, cfg.PER
    NL = NG + E
    need_ctx = any(t < cfg.NTC for t in tiles)
    with b.scope():
        Gt = gate_tiles(b, li, 5, need_ctx)
        gates = b.sb("gates", [128, NT, E], F32)
        with b.scope():
            _router(b, li, tiles, gates)
        _experts(b, li, tiles, gates, Gt)


def _router(b, li, tiles, gates):
    fw, cfg = b.fw, b.cfg
    E, NT = cfg.E, cfg.NT
    NG, PER = cfg.GROUPS, cfg.PER
    NL = NG + E
    if True:
        wgr = b.sb("wgr", [128, KC, NL], BF16)
        brow = b.sb("brow_r", [1, NL], F32)
        fw.dma("gpsimd", wgr[:, :, 0:NG], b.dram["moe_w_group"][li].rearrange("(kc p) f -> p kc f", p=128), w=[(wgr, 0)])
        fw.dma("gpsimd", wgr[:, :, NG:NL], b.dram["moe_w_router"][li].rearrange("(kc p) f -> p kc f", p=128), w=[(wgr, 1)])
        fw.dma("sync", brow[0:1, 0:NG], b.dram["moe_b_group"][li:li + 1, :], w=[(brow, 0)])
        fw.dma("sync", brow[0:1, NG:NL], b.dram["moe_b_router"][li:li + 1, :], w=[(brow, 1)])
        L = b.sb("L", [128, NT, NL], F32)
        fw.pool(lambda e: e.memset(L[:], 0.0), w=[L])
        for t in tiles:
            p = b.psum()
            for kc in range(KC):
                fw.pe(lambda e, p=p, kc=kc, t=t: e.matmul(p[:, 0:NL], lhsT=b.xnT[:, kc, t * 128:(t + 1) * 128], rhs=wgr[:, kc, :],
                                                         start=(kc == 0), stop=False), r=[(b.xnT, t), wgr], w=[p])
            fw.pe(lambda e, p=p: e.matmul(p[:, 0:NL], lhsT=b.ones_row[0:1, 0:128], rhs=brow[0:1, :], start=False, stop=True),
                  r=[b.ones_row, brow], w=[p])
            fw.act(lambda e, p=p, t=t: e.copy(out=L[:, t, :], in_=p[:, 0:NL]), r=[p], w=[L])
        def v(name, shape):
            return b.sb(name, shape, F32)
        gmax = v("gmax", [128, NT]); eg = v("eg", [128, NT, NG]); gsum = v("gsum", [128, NT]); pg = v("pg", [128, NT])
        goh = v("goh", [128, NT, NG]); Lm = v("Lm", [128, NT, E]); m1 = v("m1", [128, NT]); oh1 = v("oh1", [128, NT, E])
        m2 = v("m2", [128, NT]); oh2 = v("oh2", [128, NT, E]); w1 = v("w1", [128, NT]); w2 = v("w2", [128, NT])
        pen = v("pen", [128, NT, NG])
        Lg = L[:, :, 0:NG]
        Le = L[:, :, NG:NL]
        dv = fw.dve
        dv(lambda e: e.tensor_reduce(out=gmax[:], in_=Lg, axis=AX.X, op=ALU.max), r=[L], w=[gmax])
        dv(lambda e: e.tensor_tensor(out=eg[:], in0=Lg, in1=gmax[:].unsqueeze(2).to_broadcast([128, NT, NG]), op=ALU.subtract),
           r=[L, gmax], w=[eg])
        dv(lambda e: e.tensor_tensor(out=goh[:], in0=Lg, in1=gmax[:].unsqueeze(2).to_broadcast([128, NT, NG]), op=ALU.is_equal),
           r=[L, gmax], w=[goh])
        fw.act(lambda e: e.activation(out=eg[:], in_=eg[:], func=AF.Exp), r=[eg], w=[eg])
        dv(lambda e: e.tensor_reduce(out=gsum[:], in_=eg[:], axis=AX.X, op=ALU.add), r=[eg], w=[gsum])
        dv(lambda e: e.reciprocal(out=pg[:], in_=gsum[:]), r=[gsum], w=[pg])
        dv(lambda e: e.tensor_scalar(out=pen[:], in0=goh[:], scalar1=-1.0, scalar2=1e30, op0=ALU.add, op1=ALU.mult),
           r=[goh], w=[pen])
        dv(lambda e: e.tensor_tensor(out=Lm[:].rearrange("p t (g e) -> p t g e", g=NG),
                                     in0=Le.rearrange("p t (g e) -> p t g e", g=NG),
                                     in1=pen[:].unsqueeze(3).to_broadcast([128, NT, NG, PER]), op=ALU.add),
           r=[L, pen], w=[Lm])
        dv(lambda e: e.tensor_reduce(out=m1[:], in_=Lm[:], axis=AX.X, op=ALU.max), r=[Lm], w=[m1])
        dv(lambda e: e.tensor_tensor(out=oh1[:], in0=Lm[:], in1=m1[:].unsqueeze(2).to_broadcast([128, NT, E]), op=ALU.is_equal),
           r=[Lm, m1], w=[oh1])
        dv(lambda e: e.scalar_tensor_tensor(out=Lm[:], in0=oh1[:], scalar=-1e30, in1=Lm[:], op0=ALU.mult, op1=ALU.add),
           r=[oh1, Lm], w=[Lm])
        dv(lambda e: e.tensor_reduce(out=m2[:], in_=Lm[:], axis=AX.X, op=ALU.max), r=[Lm], w=[m2])
        dv(lambda e: e.tensor_tensor(out=oh2[:], in0=Lm[:], in1=m2[:].unsqueeze(2).to_broadcast([128, NT, E]), op=ALU.is_equal),
           r=[Lm, m2], w=[oh2])
        dv(lambda e: e.tensor_tensor(out=w2[:], in0=m2[:], in1=m1[:], op=ALU.subtract), r=[m1, m2], w=[w2])
        fw.act(lambda e: e.activation(out=w2[:], in_=w2[:], func=AF.Sigmoid), r=[w2], w=[w2])
        dv(lambda e: e.tensor_scalar(out=w1[:], in0=w2[:], scalar1=-1.0, scalar2=1.0, op0=ALU.mult, op1=ALU.add), r=[w2], w=[w1])
        dv(lambda e: e.tensor_mul(out=w1[:], in0=w1[:], in1=pg[:]), r=[w1, pg], w=[w1])
        dv(lambda e: e.tensor_mul(out=w2[:], in0=w2[:], in1=pg[:]), r=[w2, pg], w=[w2])
        dv(lambda e: e.tensor_tensor(out=oh1[:], in0=oh1[:], in1=w1[:].unsqueeze(2).to_broadcast([128, NT, E]), op=ALU.mult),
           r=[oh1, w1], w=[oh1])
        dv(lambda e: e.tensor_tensor(out=oh2[:], in0=oh2[:], in1=w2[:].unsqueeze(2).to_broadcast([128, NT, E]), op=ALU.mult),
           r=[oh2, w2], w=[oh2])
        dv(lambda e: e.tensor_add(out=gates[:], in0=oh1[:], in1=oh2[:]), r=[oh1, oh2], w=[gates])


def _experts(b, li, tiles, gates, Gt):
    fw, cfg = b.fw, b.cfg
    E, NT = cfg.E, cfg.NT
    if True:
        FF = 512
        w1b = [b.sb("w1b%d" % j, [128, KC, FF], BF16) for j in range(2)]
        w3b = [b.sb("w3b%d" % j, [128, KC, FF], BF16) for j in range(2)]
        w2b = [b.sb("w2b%d" % j, [128, 4, D], BF16) for j in range(2)]
        hid = [b.sb("hid%d" % j, [128, 4, 512], BF16) for j in range(2)]
        s1 = [b.sb("s1_%d" % j, [128, 512], BF16) for j in range(2)]
        mtmp = [b.sb("mtmp%d" % j, [128, 512], F32) for j in range(2)]
        blocks = []
        i0 = 0
        while i0 < len(tiles):
            blocks.append(tiles[i0:i0 + 4])
            i0 += 4
        def load_expert(ex):
            j = ex % 2
            fw.dma("gpsimd", w1b[j][:], b.dram["moe_w1"][li, ex].rearrange("(kc p) f -> p kc f", p=128), w=[w1b[j]])
            fw.dma("gpsimd", w3b[j][:], b.dram["moe_w3"][li, ex].rearrange("(kc p) f -> p kc f", p=128), w=[w3b[j]])
            fw.dma("gpsimd", w2b[j][:], b.dram["moe_w2"][li, ex].rearrange("(kc p) f -> p kc f", p=128), w=[w2b[j]])

        def up(ex, blk, hd):
            j = ex % 2
            nt = len(blk) * 128
            c0 = blk[0] * 128
            for fc in range(4):
                p1 = b.psum()
                p3 = b.psum()
                for kc in range(KC):
                    fw.pe(lambda e, p1=p1, kc=kc, fc=fc: e.matmul(
                        p1[:, 0:nt], lhsT=w1b[j][:, kc, fc * 128:(fc + 1) * 128], rhs=b.xnT[:, kc, c0:c0 + nt],
                        start=(kc == 0), stop=(kc == KC - 1)), r=[w1b[j], b.xnT], w=[p1])
                for kc in range(KC):
                    fw.pe(lambda e, p3=p3, kc=kc, fc=fc: e.matmul(
                        p3[:, 0:nt], lhsT=w3b[j][:, kc, fc * 128:(fc + 1) * 128], rhs=b.xnT[:, kc, c0:c0 + nt],
                        start=(kc == 0), stop=(kc == KC - 1)), r=[w3b[j], b.xnT], w=[p3])
                sj = s1[fc % 2]
                fw.act(lambda e, p1=p1, sj=sj: e.activation(out=sj[:, 0:nt], in_=p1[:, 0:nt], func=AF.Silu), r=[p1], w=[sj])
                fw.dve(lambda e, p3=p3, sj=sj, fc=fc: e.tensor_tensor(
                    out=hd[:, fc, 0:nt], in0=p3[:, 0:nt], in1=sj[:, 0:nt], op=ALU.mult), r=[p3, sj], w=[(hd, fc)])

        def down(ex, blk, hd):
            j = ex % 2
            for ti, t in enumerate(blk):
                k = 1 if t < cfg.NTC else 0
                for hf in range(2):
                    py = b.psum()
                    for fc in range(4):
                        fw.pe(lambda e, py=py, fc=fc, ti=ti, hf=hf: e.matmul(
                            py[:, :], lhsT=hd[:, fc, ti * 128:(ti + 1) * 128], rhs=w2b[j][:, fc, hf * 512:(hf + 1) * 512],
                            start=(fc == 0), stop=(fc == 3)), r=[hd, w2b[j]], w=[py])
                    tm = mtmp[(ti * 2 + hf) % 2]
                    sl = slice(hf * 512, (hf + 1) * 512)
                    fw.dve(lambda e, py=py, tm=tm, t=t, k=k, sl=sl: e.scalar_tensor_tensor(
                        out=tm[:], in0=py[:, :], scalar=gates[:, t, ex:ex + 1], in1=Gt[k][:, sl], op0=ALU.mult, op1=ALU.mult),
                        r=[py, gates, Gt[k]], w=[tm])
                    fw.pool(lambda e, tm=tm, t=t, sl=sl: e.tensor_tensor(out=b.h[:, t, sl], in0=b.h[:, t, sl], in1=tm[:], op=ALU.add),
                            r=[tm, (b.h, t)], w=[(b.h, t)])

        items = [(ex, blk) for ex in range(E) for blk in blocks]
        load_expert(0)
        if E > 1:
            load_expert(1)
        up(items[0][0], items[0][1], hid[0])
        for i, (ex, blk) in enumerate(items):
            if i + 1 < len(items):
                up(items[i + 1][0], items[i + 1][1], hid[(i + 1) % 2])
            down(ex, blk, hid[i % 2])
            if blk is blocks[-1] and ex >= 1 and ex + 1 < E:
                pass
            if blk is blocks[-1] and ex + 2 < E:
                load_expert(ex + 2)


MIXERS = {}


def build_program(cfg):
    nc = bass.Bass("TRN2", target_bir_lowering=False)
    b = B(nc, cfg)
    fw = b.fw
    NB, NT, NTC = cfg.NB, cfg.NT, cfg.NTC
    x_d = b.din("x", [NB, cfg.S_LAT, D])
    ctx_d = b.din("ctx", [NB, cfg.S_CTX, D])
    b.din("c", [NB, D])
    b.din("c_ctx", [1, D])
    for name, shape in cfg.wshapes.items():
        b.din(name, shape)
    out_d = nc.dram_tensor("out", [NB, cfg.S_LAT, D], F32, kind="ExternalOutput").ap()
    finals = []
    with b.scope():
        build_consts(b)
        b.one_col = b.ones_f
        b.h = b.sb("h", [128, NT, D], F32)
        b.scB = [b.sb("scB%d" % k, [128, KC, 128], F32) for k in range(2)]
        for bi in range(NB):
            with b.scope():
                fw.dma("sync", b.h[:, 0:NTC, :], ctx_d[bi].rearrange("(t p) d -> p t d", p=128), w=[b.h])
                fw.dma("sync", b.h[:, NTC:NT, :], x_d[bi].rearrange("(t p) d -> p t d", p=128), w=[b.h])
                load_cond(b, bi)
                for li in range(cfg.DEPTH):
                    kind = cfg.kinds[li]
                    wc = cfg.want_ctx[li]
                    with b.scope():
                        b.rtmp = [b.sb("rtmp%d" % j, [128, 512], F32) for j in range(4)]
                        b.rtmp_rr = 0
                        all_tiles = list(range(NT))
                        out_tiles = all_tiles if wc else list(range(NTC, NT))
                        if kind >= 0:
                            MIXERS[kind](b, li, all_tiles, out_tiles)
                        if cfg.moe:
                            with b.scope():
                                b.xnT = b.sb("xnT", [128, KC, NT * 128], BF16)
                                norm_mod(b, li, 2, out_tiles)
                                moe(b, li, out_tiles)
                finals.append(fw.dma("sync", out_d[bi].rearrange("(t p) d -> p t d", p=128), b.h[:, NTC:NT, :], r=[b.h]))
        fw.emit(final_wait_ops=finals)
    return nc, b


def host_tables(cfg):
    out = {}
    for name, rot in (("rope64", 64), ("rope32", 32)):
        n = cfg.S_LAT
        rows = n // 64
        row = np.repeat(np.arange(rows, dtype=np.float32), 64)
        col = np.tile(np.arange(64, dtype=np.float32), rows)
        nf = rot // 4
        inv = (10000.0 ** (-np.arange(nf, dtype=np.float32) / nf)).astype(np.float32)
        ang = np.concatenate([row[:, None] * inv, col[:, None] * inv], axis=-1)
        cs = np.concatenate([np.cos(ang), np.sin(ang)], axis=-1).astype(np.float32)
        ctxp = np.concatenate([np.ones((cfg.S_CTX, rot // 2), np.float32), np.zeros((cfg.S_CTX, rot // 2), np.float32)], axis=-1)
        out["c_" + name] = np.concatenate([ctxp, cs], axis=0)
    return out


def load_w(b, name, dram_ap, kchunks, n, dt=BF16, eng="gpsimd"):
    t = b.sb(name, [128, kchunks, n], dt)
    b.fw.dma(eng, t[:], dram_ap.rearrange("(kc p) f -> p kc f", p=128), w=[t])
    return t


def lin_tok(b, xT, t, W, n0, n, p, kchunks=KC, rx=None):
    fw = b.fw
    for kc in range(kchunks):
        fw.pe(lambda e, kc=kc: e.matmul(p[:, 0:n], lhsT=xT[:, kc, t * 128:(t + 1) * 128], rhs=W[:, kc, n0:n0 + n],
                                        start=(kc == 0), stop=(kc == kchunks - 1)),
              r=[(xT, t) if rx is None else rx, W], w=[p])


def transposes(b, src, nblk, dst_ap_fn, r, w):
    fw = b.fw
    pb = b.psumb()
    for k in range(nblk):
        fw.pe(lambda e, k=k: e.transpose(out=pb[:, k * 128:(k + 1) * 128], in_=src[:, k * 128:(k + 1) * 128], identity=b.identb[:]),
              r=[src, b.identb], w=[pb])
    fw.act(lambda e: e.copy(out=dst_ap_fn(), in_=pb[:, 0:nblk * 128].rearrange("p (k q) -> p k q", k=nblk)), r=[pb], w=w)


def row_bcast_tile(b, name, dram_row_ap, n, dt=F32):
    t = b.sb(name, [128, n], dt)
    with b.scope():
        row = b.sb(name + "_row", [1, n], F32)
        b.fw.dma("sync", row[:], dram_row_ap, w=[row])
        b.bcast_rows(t, 0, (row, row), n)
    return t


def out_proj_resid(b, li, t, srcT, W, kchunks, Gk, rsrc):
    fw = b.fw
    for hf in range(2):
        p = b.psum()
        for kc in range(kchunks):
            fw.pe(lambda e, kc=kc, p=p, hf=hf: e.matmul(p[:, :], lhsT=srcT[:, kc, :], rhs=W[:, kc, hf * 512:(hf + 1) * 512],
                                                       start=(kc == 0), stop=(kc == kchunks - 1)), r=[rsrc, W], w=[p])
        resid_add(b, t, p, hf, Gk)


def gmlp(b, li, all_tiles, out_tiles):
    fw, cfg = b.fw, b.cfg
    dr = b.dram
    with b.scope():
        b.xnT = b.sb("xnT", [128, KC, cfg.NT * 128], BF16)
        norm_mod(b, li, 1, out_tiles)
        need_ctx = any(t < cfg.NTC for t in out_tiles)
        Gt = gate_tiles(b, li, 2, need_ctx)
        w_in = load_w(b, "gm_win", dr["gm_w_in"][0], KC, 2 * D)
        w_out = load_w(b, "gm_wout", dr["gm_w_out"][0], KC, D)
        vg = row_bcast_tile(b, "gm_vg", dr["gm_v_g"][0:1, :], D)
        wsT = b.sb("wsT", [128, 8, 128], BF16)
        bsT = b.sb("bsT", [128, 8], F32)
        with b.scope():
            ws_raw = b.sb("ws_raw", [128, 8, 128], BF16)
            fw.dma("gpsimd", ws_raw[:], dr["gm_w_s"][0].rearrange("g p q -> p g q"), w=[ws_raw])
            transposes(b, ws_raw[:].rearrange("p g q -> p (g q)"), 8, lambda: wsT[:], [ws_raw], [wsT])
            bs_raw = b.sb("bs_raw", [8, 128], F32)
            fw.dma("sync", bs_raw[:], dr["gm_b_s"][0], w=[bs_raw])
            pbs = b.psum()
            fw.pe(lambda e: e.matmul(pbs[:, 0:8], lhsT=bs_raw[:, :], rhs=b.identf[0:8, 0:8], start=True, stop=True),
                  r=[bs_raw, b.identf], w=[pbs])
            fw.act(lambda e: e.copy(out=bsT[:], in_=pbs[:, 0:8]), r=[pbs], w=[bsT])
        u = [b.sb("gm_u%d" % j, [128, D], F32) for j in range(1)]
        v = [b.sb("gm_v%d" % j, [128, D], F32) for j in range(1)]
        vb = [b.sb("gm_vb%d" % j, [128, D], BF16) for j in range(1)]
        us = [b.sb("gm_us%d" % j, [128, D], BF16) for j in range(1)]
        usT = [b.sb("gm_usT%d" % j, [128, KC, 128], BF16) for j in range(1)]
        ss = b.sb("gm_ss", [128, cfg.NT], F32)
        fw.pool(lambda e: e.memset(ss[:], 0.0), w=[ss])
        for j, t in enumerate(out_tiles):
            k = 1 if t < cfg.NTC else 0
            uj, vj, vbj, usj, usTj = u[0], v[0], vb[0], us[0], usT[0]
            for blk in range(4):
                p = b.psum()
                lin_tok(b, b.xnT, t, w_in, blk * 512, 512, p)
                dst = uj if blk < 2 else vj
                c0 = (blk % 2) * 512
                fw.act(lambda e, p=p, dst=dst, c0=c0: e.activation(out=dst[:, c0:c0 + 512], in_=p[:, :], func=AF.Gelu),
                       r=[p], w=[(dst, blk % 2)])
            fw.act(lambda e, vj=vj, vbj=vbj, t=t: e.activation(out=vbj[:], in_=vj[:], func=AF.Square, accum_out=ss[:, t:t + 1]),
                   r=[vj], w=[vbj, (ss, t)])
            rstd_op(b, ss[:, t:t + 1], ss[:, t:t + 1], 1.0 / D, [(ss, t)], [(ss, t)])
            fw.dve(lambda e, vj=vj, vbj=vbj, t=t: e.scalar_tensor_tensor(out=vbj[:], in0=vj[:], scalar=ss[:, t:t + 1], in1=vg[:],
                                                                        op0=ALU.mult, op1=ALU.mult), r=[vj, (ss, t), vg], w=[vbj])
            for hf in range(2):
                p = b.psum()
                for g4 in range(4):
                    g = hf * 4 + g4
                    fw.pe(lambda e, p=p, g=g, g4=g4, vbj=vbj: e.matmul(p[:, g4 * 128:(g4 + 1) * 128], lhsT=wsT[:, g, :],
                                                                      rhs=vbj[:, g * 128:(g + 1) * 128], start=True, stop=True),
                          r=[wsT, vbj], w=[p])
                tmp = b.rtmp[b.rtmp_rr]
                b.rtmp_rr = (b.rtmp_rr + 1) % len(b.rtmp)
                fw.dve(lambda e, p=p, tmp=tmp, hf=hf: e.tensor_tensor(
                    out=tmp[:].rearrange("p (g d) -> p g d", g=4), in0=p[:, :].rearrange("p (g d) -> p g d", g=4),
                    in1=bsT[:, hf * 4:(hf + 1) * 4].unsqueeze(2).to_broadcast([128, 4, 128]), op=ALU.add), r=[p, bsT], w=[tmp])
                fw.pool(lambda e, tmp=tmp, uj=uj, usj=usj, hf=hf: e.tensor_tensor(
                    out=usj[:, hf * 512:(hf + 1) * 512], in0=tmp[:], in1=uj[:, hf * 512:(hf + 1) * 512], op=ALU.mult),
                    r=[tmp, uj], w=[(usj, hf)])
            transposes(b, usj, KC, lambda usTj=usTj: usTj[:], [usj], [usTj])
            out_proj_resid(b, li, t, usTj, w_out, KC, Gt[k], usTj)


MIXERS[1] = gmlp


def attn_core(b, qT, kT, v_aug, vd, qtiles, ktiles, scale, Osb, r_q, r_k, r_v):
    fw = b.fw
    nq = len(qtiles) * 128
    q0 = qtiles[0] * 128
    w1 = vd + 1
    per_bank = 1
    pexp = b.pexp
    obanks = [b.ps[0], b.ps[1], b.ps[2], b.ps[3]]
    assert len(qtiles) <= 4
    def score(ki):
        kt = ktiles[ki]
        ps_s = b.ps[4 + (b.sc_rr % 2)]
        b.sc_rr += 1
        fw.pe(lambda e: e.matmul(ps_s[:, 0:nq], lhsT=kT(kt * 128, 128), rhs=qT(q0, nq), start=True, stop=True),
              r=[r_q, r_k], w=[ps_s])
        pe_ = pexp[ki % 2]
        fw.act(lambda e: e.activation(out=pe_[:, 0:nq], in_=ps_s[:, 0:nq], func=AF.Exp, scale=scale), r=[ps_s], w=[pe_])

    def pv(ki):
        kt = ktiles[ki]
        pe_ = pexp[ki % 2]
        for qi in range(len(qtiles)):
            ob = obanks[qi // per_bank]
            oc = (qi % per_bank) * w1
            fw.pe(lambda e, ob=ob, oc=oc, qi=qi: e.matmul(ob[:, oc:oc + w1], lhsT=pe_[:, qi * 128:(qi + 1) * 128],
                                                        rhs=v_aug(kt), start=(ki == 0), stop=(ki == len(ktiles) - 1)),
                  r=[pe_, r_v], w=[ob])

    score(0)
    for ki in range(len(ktiles)):
        if ki + 1 < len(ktiles):
            score(ki + 1)
        pv(ki)
    for qi in range(len(qtiles)):
        ob = obanks[qi // per_bank]
        oc = (qi % per_bank) * w1
        fw.act(lambda e, ob=ob, oc=oc, qi=qi: e.copy(out=Osb[:, qi, 0:w1], in_=ob[:, oc:oc + w1]), r=[ob], w=[(Osb, qi)])


def qblocks(cfg, out_tiles):
    blocks = []
    ctx_q = [t for t in out_tiles if t < cfg.NTC]
    if ctx_q:
        blocks.append((ctx_q, list(range(cfg.NTC))))
    lat = [t for t in out_tiles if t >= cfg.NTC]
    return blocks, lat


def rope_apply(b, dst, src, cs_t, ngrp, half, r, w):
    fw = b.fw
    t1, t2 = b.rope_tmp
    cos = cs_t[:, 0:half].unsqueeze(1).to_broadcast([128, ngrp, half])
    sin = cs_t[:, half:2 * half].unsqueeze(1).to_broadcast([128, ngrp, half])
    x1 = src[:, :, 0, :]
    x2 = src[:, :, 1, :]
    v1 = t1[:, 0:ngrp * half].rearrange("p (g h) -> p g h", g=ngrp)
    v2 = t2[:, 0:ngrp * half].rearrange("p (g h) -> p g h", g=ngrp)
    fw.dve(lambda e: e.tensor_tensor(out=v1, in0=x1, in1=cos, op=ALU.mult), r=r, w=[t1])
    fw.pool(lambda e: e.tensor_tensor(out=v2, in0=x2, in1=sin, op=ALU.mult), r=r, w=[t2])
    fw.dve(lambda e: e.tensor_tensor(out=dst[:, :, 0, :], in0=v1, in1=v2, op=ALU.subtract), r=[t1, t2], w=w)
    fw.dve(lambda e: e.tensor_tensor(out=v1, in0=x1, in1=sin, op=ALU.mult), r=r, w=[t1])
    fw.pool(lambda e: e.tensor_tensor(out=v2, in0=x2, in1=cos, op=ALU.mult), r=r, w=[t2])
    fw.dve(lambda e: e.tensor_tensor(out=dst[:, :, 1, :], in0=v1, in1=v2, op=ALU.add), r=[t1, t2], w=w)


def group_rms(b, x, ngrp, gd, ssq, r, w_ssq):
    fw = b.fw
    sq = b.sq_tmp
    fw.pool(lambda e: e.tensor_tensor(out=sq[:, 0:ngrp * gd], in0=x[:, 0:ngrp * gd], in1=x[:, 0:ngrp * gd], op=ALU.mult), r=r, w=[sq])
    fw.dve(lambda e: e.tensor_reduce(out=ssq[:, 0:ngrp], in_=sq[:, 0:ngrp * gd].rearrange("p (g d) -> p g d", g=ngrp),
                                     axis=AX.X, op=ALU.add), r=[sq], w=w_ssq)
    rstd_op(b, ssq[:, 0:ngrp], ssq[:, 0:ngrp], 1.0 / gd, w_ssq, w_ssq)


def diff_attn(b, li, all_tiles, out_tiles):
    fw, cfg = b.fw, b.cfg
    dr = b.dram
    NT = cfg.NT
    lam_init = 0.8 - 0.6 * math.exp(-0.3 * cfg.layer_ids[li])
    with b.scope():
        b.xnT = b.sb("xnT", [128, KC, NT * 128], BF16)
        norm_mod(b, li, 1, all_tiles)
        need_ctx = any(t < cfg.NTC for t in out_tiles)
        Gt = gate_tiles(b, li, 2, need_ctx)
        lr = b.sb("lamrow", [1, 4, 64], F32)
        for j, nm in enumerate(("da_lam_q1", "da_lam_k1", "da_lam_q2", "da_lam_k2")):
            fw.dma("sync", lr[0:1, j, :], dr[nm][0:1, :], w=[(lr, j)])
        lp = b.sb("lamp", [1, 2, 64], F32)
        ls = b.sb("lams", [1, 4], F32)
        fw.dve(lambda e: e.tensor_tensor(out=lp[0:1, :, :], in0=lr[0:1, 0:4:2, :], in1=lr[0:1, 1:4:2, :], op=ALU.mult), r=[lr], w=[lp])
        fw.dve(lambda e: e.tensor_reduce(out=ls[0:1, 0:2], in_=lp[0:1, :, :], axis=AX.X, op=ALU.add), r=[lp], w=[ls])
        fw.act(lambda e: e.activation(out=ls[0:1, 0:2], in_=ls[0:1, 0:2], func=AF.Exp), r=[ls], w=[ls])
        fw.dve(lambda e: e.tensor_tensor(out=ls[0:1, 2:3], in0=ls[0:1, 0:1], in1=ls[0:1, 1:2], op=ALU.subtract), r=[ls], w=[ls])
        fw.dve(lambda e: e.tensor_scalar(out=ls[0:1, 3:4], in0=ls[0:1, 2:3], scalar1=lam_init, scalar2=-1.0, op0=ALU.add, op1=ALU.mult),
               r=[ls], w=[ls])
        nlam = b.sb("nlam", [128, 1], F32)
        b.bcast_rows(nlam, 0, (ls, T(ls[0:1, 3:4], ls.name)), 1)
        grow = b.sb("da_grow", [1, 256], F32)
        for j in range(4):
            fw.dma("sync", grow[0:1, j * 64:(j + 1) * 64], dr["da_q_g" if j < 2 else "da_k_g"][0:1, :], w=[(grow, j)])
        qkg = b.sb("da_qkg", [128, 256], F32)
        b.bcast_rows(qkg, 0, (grow, grow), 256)
        cs = b.sb("da_cs", [128, NT, 64], F32)
        fw.dma("sync", cs[:], dr["c_rope64"].rearrange("(t p) c -> p t c", p=128), w=[cs])
        w_out2 = [b.sb("da_wout%d" % j, [128, 1, D], BF16) for j in range(2)]
        sgrow = b.sb("da_sgrow", [1, 128], F32)
        fw.dma("sync", sgrow[:], dr["da_sub_g"][0:1, :], w=[sgrow])
        pc = b.psum()
        fw.pe(lambda e: e.matmul(pc[:, 0:1], lhsT=sgrow[0:1, :], rhs=b.ones_row[0:1, 0:1], start=True, stop=True),
              r=[sgrow, b.ones_row], w=[pc])
        sgcol = b.sb("da_sgcol", [128, 1], F32)
        fw.act(lambda e: e.activation(out=sgcol[:], in_=pc[:, 0:1], func=AF.Copy, scale=(1.0 - lam_init)), r=[pc], w=[sgcol])
        b.pexp = [b.sb("pexp%d" % j, [128, 512], BF16) for j in range(2)]
        b.sc_rr = 0
        NS = 2
        rope_tmps = [[b.sb("ropet%d_%d" % (sl, j), [128, 128], F32) for j in range(2)] for sl in range(NS)]
        sq_tmps = [b.sb("sqtmp%d" % sl, [128, 256], F32) for sl in range(NS)]
        qkT = b.sb("da_qkT", [128, 2, NT * 128], BF16)
        v_aug = b.sb("da_vaug", [128, NT, 132], BF16)
        fw.pool(lambda e: e.memset(v_aug[:, :, 128:129], 1.0), w=[v_aug])
        W_h = [b.sb("da_Wh%d" % j, [128, KC, 384], BF16) for j in range(1)]
        qk_s = [b.sb("da_qk%d" % sl, [128, 256], F32) for sl in range(NS)]
        qkn_s = [b.sb("da_qkn%d" % sl, [128, 256], F32) for sl in range(NS)]
        qkr_s = [b.sb("da_qkr%d" % sl, [128, 256], BF16) for sl in range(NS)]
        ssq_s = [b.sb("da_ssq%d" % sl, [128, 4], F32) for sl in range(NS)]
        Osb = [b.sb("da_O%d" % j, [128, 4, 132], F32) for j in range(2)]
        rr_s = [b.sb("da_rr%d" % sl, [128, 2], F32) for sl in range(2)]
        o_s = [b.sb("da_o%d" % sl, [128, 128], F32) for sl in range(4)]
        ob_s = [b.sb("da_ob%d" % sl, [128, 128], BF16) for sl in range(4)]
        oss_s = [b.sb("da_oss%d" % sl, [128, 1], F32) for sl in range(4)]
        junk_s = [b.sb("da_junk%d" % sl, [128, 128], BF16) for sl in range(4)]
        oT_s = [b.sb("da_oT%d" % sl, [128, 1, 128], BF16) for sl in range(4)]
        w_in = dr["da_w_in"][0]
        blocks, lat = qblocks(cfg, out_tiles)
        for i0 in range(0, len(lat), 4):
            blocks.append((lat[i0:i0 + 4], all_tiles))
        for hd in range(8):
            Wh = W_h[0]
            w_out = w_out2[hd % 2]
            for j in range(3):
                fw.dma("gpsimd", Wh[:, :, j * 128:(j + 1) * 128],
                       w_in[:, j * 1024 + hd * 128:j * 1024 + (hd + 1) * 128].rearrange("(kc p) f -> p kc f", p=128), w=[(Wh, j)])
            fw.dma("gpsimd", w_out[:], dr["da_w_out"][0][hd * 128:(hd + 1) * 128, :].rearrange("(kc p) f -> p kc f", p=128), w=[w_out])
            fw.dve(lambda e, w_out=w_out: e.tensor_scalar(out=w_out[:], in0=w_out[:], scalar1=sgcol[:, 0:1], scalar2=None, op0=ALU.mult),
                   r=[w_out, sgcol], w=[w_out])

            def pre(t, sl):
                qk, qkn, qkr, ssq = qk_s[sl], qkn_s[sl], qkr_s[sl], ssq_s[sl]
                b.sq_tmp = sq_tmps[sl]
                b.rope_tmp = rope_tmps[sl]
                p = b.psum()
                lin_tok(b, b.xnT, t, Wh, 0, 384, p)
                fw.act(lambda e: e.copy(out=qk[:], in_=p[:, 0:256]), r=[p], w=[qk])
                fw.act(lambda e: e.copy(out=v_aug[:, t, 0:128], in_=p[:, 256:384]), r=[p], w=[(v_aug, t)])
                group_rms(b, qk, 4, 64, ssq, [qk], [ssq])
                fw.dve(lambda e: e.tensor_tensor(out=qkn[:].rearrange("p (g d) -> p g d", g=4), in0=qk[:].rearrange("p (g d) -> p g d", g=4),
                                                 in1=ssq[:, 0:4].unsqueeze(2).to_broadcast([128, 4, 64]), op=ALU.mult), r=[qk, ssq], w=[qkn])
                fw.pool(lambda e: e.tensor_tensor(out=qkn[:], in0=qkn[:], in1=qkg[:], op=ALU.mult), r=[qkn, qkg], w=[qkn])
                rope_apply(b, qkr[:].rearrange("p (g two h) -> p g two h", g=4, two=2), qkn[:].rearrange("p (g two h) -> p g two h", g=4, two=2),
                           cs[:, t, :], 4, 32, [qkn, cs], [qkr])
                transposes(b, qkr, 2, lambda: qkT[:, :, t * 128:(t + 1) * 128], [qkr], [(qkT, t)])

            fw.lockstep(all_tiles, NS, pre)
            for (qts, kts) in blocks:
                for comp in range(2):
                    sl = slice(comp * 64, (comp + 1) * 64)
                    attn_core(b, lambda c0, n, sl=sl: qkT[sl, 0, c0:c0 + n], lambda c0, n, sl=sl: qkT[sl, 1, c0:c0 + n],
                              lambda kt: v_aug[:, kt, 0:129], 128, qts, kts, 0.125, Osb[comp], qkT, qkT, v_aug)

                def post(item, sl):
                    qi, t = item
                    rr, o, ob, oss, junk, oT = rr_s[sl], o_s[sl], ob_s[sl], oss_s[sl], junk_s[sl], oT_s[sl]
                    k = 1 if t < cfg.NTC else 0
                    b.rtmp_rr = sl * 2
                    fw.dve(lambda e: e.reciprocal(out=rr[:, 0:1], in_=Osb[0][:, qi, 128:129]), r=[(Osb[0], qi)], w=[rr])
                    fw.dve(lambda e: e.reciprocal(out=rr[:, 1:2], in_=Osb[1][:, qi, 128:129]), r=[(Osb[1], qi)], w=[rr])
                    fw.dve(lambda e: e.tensor_tensor(out=rr[:, 1:2], in0=rr[:, 1:2], in1=nlam[:, 0:1], op=ALU.mult), r=[rr, nlam], w=[rr])
                    fw.dve(lambda e: e.tensor_scalar(out=o[:], in0=Osb[0][:, qi, 0:128], scalar1=rr[:, 0:1], scalar2=None, op0=ALU.mult),
                           r=[(Osb[0], qi), rr], w=[o])
                    fw.dve(lambda e: e.scalar_tensor_tensor(out=o[:], in0=Osb[1][:, qi, 0:128], scalar=rr[:, 1:2], in1=o[:],
                                                            op0=ALU.mult, op1=ALU.add), r=[(Osb[1], qi), rr, o], w=[o])
                    fw.pool(lambda e: e.memset(oss[:], 0.0), w=[oss])
                    fw.act(lambda e: e.activation(out=junk[:], in_=o[:], func=AF.Square, accum_out=oss[:, 0:1]), r=[o, oss], w=[junk, oss])
                    rstd_op(b, oss[:, 0:1], oss[:, 0:1], 1.0 / 128, [oss], [oss])
                    fw.dve(lambda e: e.tensor_scalar(out=ob[:], in0=o[:], scalar1=oss[:, 0:1], scalar2=None, op0=ALU.mult), r=[o, oss], w=[ob])
                    transposes(b, ob, 1, lambda: oT[:], [ob], [oT])
                    out_proj_resid(b, li, t, oT, w_out, 1, Gt[k], oT)

                fw.lockstep(list(enumerate(qts)), 2, post)


MIXERS[0] = diff_attn


def mla(b, li, all_tiles, out_tiles):
    fw, cfg = b.fw, b.cfg
    dr = b.dram
    NT = cfg.NT
    with b.scope():
        need_ctx = any(t < cfg.NTC for t in out_tiles)
        Gt = gate_tiles(b, li, 2, need_ctx)
        cqnT = b.sb("cqnT", [128, 3, NT * 128], BF16)
        ckvnT = b.sb("ckvnT", [128, 2, NT * 128], BF16)
        kpe = b.sb("kpe", [128, NT, 32], F32)
        b.sq_tmp = b.sb("sqtmp", [128, 512], F32)
        ssq = b.sb("ml_ssq", [128, 8], F32)
        with b.scope():
            b.xnT = b.sb("xnT", [128, KC, NT * 128], BF16)
            norm_mod(b, li, 1, all_tiles)
            w_in = load_w(b, "ml_win", dr["mla_w_in"][0], KC, 672)
            qng = row_bcast_tile(b, "ml_qng", dr["mla_q_norm_g"][0:1, :], 384)
            kvng = row_bcast_tile(b, "ml_kvng", dr["mla_kv_norm_g"][0:1, :], 256)
            cq = b.sb("ml_cq", [128, 384], F32)
            ckv = b.sb("ml_ckv", [128, 288], F32)
            cqb = b.sb("ml_cqb", [128, 384], BF16)
            ckvb = b.sb("ml_ckvb", [128, 256], BF16)
            for t in all_tiles:
                p0 = b.psum()
                lin_tok(b, b.xnT, t, w_in, 0, 384, p0)
                p1 = b.psum()
                lin_tok(b, b.xnT, t, w_in, 384, 288, p1)
                fw.act(lambda e, p0=p0: e.copy(out=cq[:], in_=p0[:, 0:384]), r=[p0], w=[cq])
                fw.act(lambda e, p1=p1: e.copy(out=ckv[:], in_=p1[:, 0:288]), r=[p1], w=[ckv])
                fw.pool(lambda e, t=t: e.tensor_copy(out=kpe[:, t, :], in_=ckv[:, 256:288]), r=[ckv], w=[(kpe, t)])
                group_rms(b, cq, 1, 384, ssq, [cq], [ssq])
                fw.dve(lambda e: e.scalar_tensor_tensor(out=cqb[:], in0=cq[:], scalar=ssq[:, 0:1], in1=qng[:], op0=ALU.mult, op1=ALU.mult),
                       r=[cq, ssq, qng], w=[cqb])
                transposes(b, cqb, 3, lambda t=t: cqnT[:, :, t * 128:(t + 1) * 128], [cqb], [(cqnT, t)])
                group_rms(b, ckv, 1, 256, ssq, [ckv], [ssq])
                fw.dve(lambda e: e.scalar_tensor_tensor(out=ckvb[:], in0=ckv[:, 0:256], scalar=ssq[:, 0:1], in1=kvng[:], op0=ALU.mult, op1=ALU.mult),
                       r=[ckv, ssq, kvng], w=[ckvb])
                transposes(b, ckvb, 2, lambda t=t: ckvnT[:, :, t * 128:(t + 1) * 128], [ckvb], [(ckvnT, t)])
        with b.scope():
            qg = row_bcast_tile(b, "ml_qg", dr["mla_q_g"][0:1, :], 96)
            kg = row_bcast_tile(b, "ml_kg", dr["mla_k_g"][0:1, :], 96)
            cs = b.sb("ml_cs", [128, NT, 32], F32)
            fw.dma("sync", cs[:], dr["c_rope32"].rearrange("(t p) c -> p t c", p=128), w=[cs])
            b.pexp = [b.sb("pexp%d" % j, [128, 512], BF16) for j in range(2)]
            b.sc_rr = 0
            qT_g = b.sb("ml_qT", [128, 4, NT * 128], BF16)
            kT_g = b.sb("ml_kT", [128, 4, NT * 128], BF16)
            v_aug = b.sb("ml_vaug", [128, NT, 4, 66], BF16)
            fw.pool(lambda e: e.memset(v_aug[:, :, :, 64:65], 1.0), w=[v_aug])
            wuq = [b.sb("ml_wuq%d" % j, [128, 3, 384], BF16) for j in range(2)]
            wukv = [b.sb("ml_wukv%d" % j, [128, 2, 512], BF16) for j in range(2)]
            wout = [b.sb("ml_wout%d" % j, [128, 2, D], BF16) for j in range(2)]
            qs_s = [b.sb("ml_qs%d" % sl, [128, 384], F32) for sl in range(2)]
            ks_s = [b.sb("ml_ks%d" % sl, [128, 384], F32) for sl in range(2)]
            qr_s = [b.sb("ml_qr%d" % sl, [128, 384], BF16) for sl in range(2)]
            kr_s = [b.sb("ml_kr%d" % sl, [128, 384], BF16) for sl in range(2)]
            ssq_s = [b.sb("ml_ssq%d" % sl, [128, 8], F32) for sl in range(2)]
            sq_s = [b.sb("ml_sq%d" % sl, [128, 384], F32) for sl in range(2)]
            ropet_s = [[b.sb("ml_ropet%d_%d" % (sl, jj), [128, 64], F32) for jj in range(2)] for sl in range(2)]
            Osb = [b.sb("ml_O%d" % j, [128, 4, 68], F32) for j in range(2)]
            rr_s = [b.sb("ml_rr%d" % sl, [128, 2], F32) for sl in range(2)]
            opair_s = [b.sb("ml_opair%d" % sl, [128, 128], BF16) for sl in range(2)]
            oT_s = [b.sb("ml_oT%d" % sl, [128, 1, 128], BF16) for sl in range(2)]
            blocks, lat = qblocks(cfg, out_tiles)
            for i0 in range(0, len(lat), 4):
                blocks.append((lat[i0:i0 + 4], all_tiles))
            for hg in range(4):
                j = hg % 2
                fw.dma("gpsimd", wuq[j][:], dr["mla_w_uq"][0][:, hg * 384:(hg + 1) * 384].rearrange("(kc p) f -> p kc f", p=128), w=[wuq[j]])
                fw.dma("gpsimd", wukv[j][:], dr["mla_w_ukv"][0][:, hg * 512:(hg + 1) * 512].rearrange("(kc p) f -> p kc f", p=128), w=[wukv[j]])
                fw.dma("gpsimd", wout[j][:], dr["mla_w_out"][0][hg * 256:(hg + 1) * 256, :].rearrange("(kc p) f -> p kc f", p=128), w=[wout[j]])
                def pre2(t, sl):
                    qs, ks, qr, kr, ssq = qs_s[sl], ks_s[sl], qr_s[sl], kr_s[sl], ssq_s[sl]
                    b.sq_tmp = sq_s[sl]
                    b.rope_tmp = ropet_s[sl]
                    pb = b.psb[sl]
                    pq = b.psum()
                    lin_tok(b, cqnT, t, wuq[j], 0, 384, pq, kchunks=3)
                    pkv = b.psum()
                    lin_tok(b, ckvnT, t, wukv[j], 0, 512, pkv, kchunks=2)
                    fw.act(lambda e: e.copy(out=qs[:], in_=pq[:, 0:384]), r=[pq], w=[qs])
                    pkv4 = pkv[:, :].rearrange("p (h d) -> p h d", h=4)
                    fw.act(lambda e: e.copy(out=ks[:].rearrange("p (h d) -> p h d", h=4)[:, :, 0:64], in_=pkv4[:, :, 0:64]),
                           r=[pkv], w=[(ks, 0)])
                    fw.pool(lambda e: e.tensor_copy(out=ks[:].rearrange("p (h d) -> p h d", h=4)[:, :, 64:96],
                                                    in_=kpe[:, t, :].unsqueeze(1).to_broadcast([128, 4, 32])), r=[(kpe, t)], w=[(ks, 1)])
                    fw.act(lambda e: e.copy(out=v_aug[:, t, :, 0:64], in_=pkv4[:, :, 64:128]), r=[pkv], w=[(v_aug, t)])
                    for (src, gain, dst, dstT) in ((qs, qg, qr, qT_g), (ks, kg, kr, kT_g)):
                        group_rms(b, src, 4, 96, ssq, [src], [ssq])
                        s4 = src[:].rearrange("p (h d) -> p h d", h=4)
                        fw.dve(lambda e, s4=s4: e.tensor_tensor(out=s4, in0=s4, in1=ssq[:, 0:4].unsqueeze(2).to_broadcast([128, 4, 96]), op=ALU.mult),
                               r=[src, ssq], w=[src])
                        fw.pool(lambda e, s4=s4, gain=gain: e.tensor_tensor(out=s4, in0=s4, in1=gain[:].unsqueeze(1).to_broadcast([128, 4, 96]), op=ALU.mult),
                                r=[src, gain], w=[src])
                        d4 = dst[:].rearrange("p (h d) -> p h d", h=4)
                        fw.act(lambda e, s4=s4, d4=d4: e.copy(out=d4[:, :, 0:64], in_=s4[:, :, 0:64]), r=[src], w=[(dst, 0)])
                        rope_apply(b, d4[:, :, 64:96].rearrange("p h (two x) -> p h two x", two=2),
                                   s4[:, :, 64:96].rearrange("p h (two x) -> p h two x", two=2), cs[:, t, :], 4, 16, [src, cs], [(dst, 1)])
                        for h in range(4):
                            fw.pe(lambda e, h=h, dst=dst: e.transpose(out=pb[0:96, h * 128:(h + 1) * 128], in_=dst[:, h * 96:(h + 1) * 96],
                                                                     identity=b.identb[:]), r=[dst, b.identb], w=[pb])
                        fw.act(lambda e, dstT=dstT: e.copy(out=dstT[0:96, :, t * 128:(t + 1) * 128],
                                                           in_=pb[0:96, 0:512].rearrange("p (k q) -> p k q", k=4)), r=[pb], w=[(dstT, t)])

                fw.lockstep(all_tiles, 2, pre2)
                for pair in range(2):
                    for (qts, kts) in blocks:
                        for hh in range(2):
                            h = pair * 2 + hh
                            attn_core(b, lambda c0, n, h=h: qT_g[0:96, h, c0:c0 + n], lambda c0, n, h=h: kT_g[0:96, h, c0:c0 + n],
                                      lambda kt, h=h: v_aug[:, kt, h, 0:65], 64, qts, kts, 96 ** -0.5, Osb[hh], qT_g, kT_g, v_aug)

                        def post2(item, sl):
                            qi, t = item
                            rr, opair, oT = rr_s[sl], opair_s[sl], oT_s[sl]
                            k = 1 if t < cfg.NTC else 0
                            b.rtmp_rr = sl * 2
                            for hh in range(2):
                                fw.dve(lambda e, hh=hh: e.reciprocal(out=rr[:, hh:hh + 1], in_=Osb[hh][:, qi, 64:65]), r=[(Osb[hh], qi)], w=[(rr, hh)])
                                fw.dve(lambda e, hh=hh: e.tensor_scalar(out=opair[:, hh * 64:(hh + 1) * 64], in0=Osb[hh][:, qi, 0:64],
                                                                       scalar1=rr[:, hh:hh + 1], scalar2=None, op0=ALU.mult),
                                       r=[(Osb[hh], qi), (rr, hh)], w=[(opair, hh)])
                            pbs = b.psb[sl]
                            fw.pe(lambda e: e.transpose(out=pbs[:, 0:128], in_=opair[:, 0:128], identity=b.identb[:]), r=[opair, b.identb], w=[pbs])
                            fw.act(lambda e: e.copy(out=oT[:], in_=pbs[:, 0:128].rearrange("p (k q) -> p k q", k=1)), r=[pbs], w=[oT])
                            out_proj_resid(b, li, t, oT, T(wout[j][:, pair:pair + 1, :], wout[j].name), 1, Gt[k], oT)

                        fw.lockstep(list(enumerate(qts)), 2, post2)


MIXERS[3] = mla


def mamba(b, li, all_tiles, out_tiles):
    fw, cfg, nc = b.fw, b.cfg, b.nc
    dr = b.dram
    NT, NTC = cfg.NT, cfg.NTC
    S_CTX, S_LAT = cfg.S_CTX, cfg.S_LAT
    w_in = dr["ssm_w_in"][0]
    if not hasattr(b, "yf_d"):
        b.yf_d = nc.dram_tensor("ssm_yf_scratch", [NT * 128, 512], F32, kind="Internal").ap()
        b.yg_d = nc.dram_tensor("ssm_yg_scratch", [NT * 128, 2048], BF16, kind="Internal").ap()
    yf_d, yg_d = b.yf_d, b.yg_d
    with b.scope():
        need_ctx = any(t < NTC for t in out_tiles)
        ssqg = b.sb("sm_ssqg", [128, NT, 4], F32)
        fw.pool(lambda e: e.memset(ssqg[:], 0.0), w=[ssqg])
        with b.scope():
            b.xnT = b.sb("xnT", [128, KC, NT * 128], BF16)
            norm_mod(b, li, 1, all_tiles)
            dt_all = b.sb("sm_dt", [128, NT, 64], F32)
            a_all = b.sb("sm_a", [128, NT, 64], F32)
            dskB = b.sb("sm_dsk", [128, 32], F32)
            with b.scope():
                Wdt = load_w(b, "sm_wdt", w_in[:, 5120:5184], KC, 64)
                rows = b.sb("sm_rows", [1, 160], F32)
                fw.dma("sync", rows[0:1, 0:64], dr["ssm_dt_bias"][0:1].rearrange("a d h -> a (d h)"), w=[(rows, 0)])
                fw.dma("sync", rows[0:1, 64:128], dr["ssm_a_log"][0:1].rearrange("a d h -> a (d h)"), w=[(rows, 1)])
                fw.dma("sync", rows[0:1, 128:160], dr["ssm_d"][0:1, :], w=[(rows, 2)])
                fw.act(lambda e: e.activation(out=rows[0:1, 64:128], in_=rows[0:1, 64:128], func=AF.Exp), r=[(rows, 1)], w=[(rows, 1)])
                fw.dve(lambda e: e.tensor_scalar(out=rows[0:1, 64:128], in0=rows[0:1, 64:128], scalar1=-1.0, scalar2=None, op0=ALU.mult),
                       r=[(rows, 1)], w=[(rows, 1)])
                cB = b.sb("sm_cB", [128, 160], F32)
                b.bcast_rows(cB, 0, (rows, rows), 160)
                fw.dve(lambda e: e.tensor_copy(out=dskB[:], in_=cB[:, 128:160]), r=[cB], w=[dskB])
                xr = b.sb("sm_xr", [128, 64], F32)
                ax = b.sb("sm_ax", [128, 64], F32)
                for t in all_tiles:
                    p = b.psum()
                    lin_tok(b, b.xnT, t, Wdt, 0, 64, p)
                    fw.dve(lambda e, p=p: e.tensor_tensor(out=xr[:], in0=p[:, 0:64], in1=cB[:, 0:64], op=ALU.add), r=[p, cB], w=[xr])
                    fw.act(lambda e: e.activation(out=ax[:], in_=xr[:], func=AF.Abs), r=[xr], w=[ax])
                    fw.act(lambda e: e.activation(out=ax[:], in_=ax[:], func=AF.Exp, scale=-1.0), r=[ax], w=[ax])
                    fw.act(lambda e: e.activation(out=ax[:], in_=ax[:], func=AF.Ln, bias=b.ones_f[:, 0:1], scale=1.0), r=[ax, b.ones_f], w=[ax])
                    fw.dve(lambda e, t=t: e.scalar_tensor_tensor(out=dt_all[:, t, :], in0=xr[:], scalar=0.0, in1=ax[:], op0=ALU.max, op1=ALU.add),
                           r=[xr, ax], w=[(dt_all, t)])
                    fw.pool(lambda e, t=t: e.tensor_tensor(out=a_all[:, t, :], in0=dt_all[:, t, :], in1=cB[:, 64:128], op=ALU.mult),
                            r=[(dt_all, t), cB], w=[(a_all, t)])
            for g in range(4):
                with b.scope():
                    _mamba_group(b, li, g, all_tiles, dt_all, a_all, dskB, ssqg, w_in, yf_d, yg_d)
        with b.scope():
            Gt = gate_tiles(b, li, 2, need_ctx)
            w_out = load_w(b, "sm_wout", dr["ssm_w_out"][0], 16, D)
            OG = row_bcast_tile(b, "sm_og", dr["ssm_out_g"][0:1, :], 2048)
            rstd = b.sb("sm_rstd", [128, NT], F32)
            fw.dve(lambda e: e.tensor_reduce(out=rstd[:], in_=ssqg[:], axis=AX.X, op=ALU.add), r=[ssqg], w=[rstd])
            rstd_op(b, rstd[:], rstd[:], 1.0 / 2048, [rstd], [rstd])
            ygt = [b.sb("sm_ygt%d" % j, [128, 2048], BF16) for j in range(2)]
            ygn = b.sb("sm_ygn", [128, 2048], BF16)
            ygT = [b.sb("sm_ygT%d" % j, [128, 16, 128], BF16) for j in range(2)]
            for j, t in enumerate(out_tiles):
                k = 1 if t < NTC else 0
                yt, yT = ygt[j % 2], ygT[j % 2]
                fw.dma("sync", yt[:], yg_d[t * 128:(t + 1) * 128, :], r=[("yg_d", t)], w=[yt])
                fw.dve(lambda e, yt=yt, t=t: e.scalar_tensor_tensor(out=ygn[:], in0=yt[:], scalar=rstd[:, t:t + 1], in1=OG[:],
                                                                  op0=ALU.mult, op1=ALU.mult), r=[yt, rstd, OG], w=[ygn])
                for hf in range(2):
                    transposes(b, T(ygn[:, hf * 1024:(hf + 1) * 1024], ygn.name), 8,
                               lambda yT=yT, hf=hf: yT[:, hf * 8:(hf + 1) * 8, :], [ygn], [(yT, hf)])
                out_proj_resid(b, li, t, yT, w_out, 16, Gt[k], yT)


def _mamba_group(b, li, g, all_tiles, dt_all, a_all, dskB, ssqg, w_in, yf_d, yg_d):
    fw, cfg = b.fw, b.cfg
    dr = b.dram
    NT, NTC = cfg.NT, cfg.NTC
    S_CTX, S_LAT = cfg.S_CTX, cfg.S_LAT
    NTOK = NT * 128
    xs_tok = b.sb("sm_xs", [128, NT, 512], BF16)
    BT = b.sb("sm_BT", [128, NTOK], BF16)
    CT = b.sb("sm_CT", [128, NTOK], BF16)
    B_tok = b.sb("sm_Btok", [128, NT, 128], BF16)
    with b.scope():
        XR = S_CTX + S_LAT + 8
        xrow = b.sb("sm_xrow", [128, XR], F32)
        acc = b.sb("sm_acc", [128, NTOK], F32)
        conv_o = b.sb("sm_convo", [128, NTOK], BF16)
        fw.pool(lambda e: e.memset(xrow[:], 0.0), w=[xrow])
        segs = [(0, S_CTX, 2), (S_CTX, S_LAT, S_CTX + 6)]
        Wcs = [b.sb("sm_Wc%d" % j, [128, KC, 128], BF16) for j in range(2)]
        cwrow = b.sb("sm_cwrow", [6, 128], F32)
        cw = b.sb("sm_cw", [128, 6], F32)
        chunks = [("xs", g * 512 + cc * 128, cc) for cc in range(4)] + [("B", 2048 + g * 128, 0), ("C", 2560 + g * 128, 0)]
        for ci, (kind, cch, cc) in enumerate(chunks):
            Wc = Wcs[ci % 2]
            fw.dma("gpsimd", Wc[:], w_in[:, 2048 + cch:2048 + cch + 128].rearrange("(kc p) f -> p kc f", p=128), w=[Wc])
            fw.dma("sync", cwrow[0:5, :], dr["ssm_conv_w"][0][:, cch:cch + 128], w=[(cwrow, 0)])
            fw.dma("sync", cwrow[5:6, :], dr["ssm_conv_b"][0:1, cch:cch + 128], w=[(cwrow, 1)])
            pc = b.psum()
            fw.pe(lambda e, pc=pc: e.matmul(pc[:, 0:6], lhsT=cwrow[:, :], rhs=b.identf[0:6, 0:6], start=True, stop=True),
                  r=[cwrow, b.identf], w=[pc])
            fw.act(lambda e, pc=pc: e.copy(out=cw[:], in_=pc[:, 0:6]), r=[pc], w=[cw])
            dst = conv_o if kind == "xs" else (BT if kind == "B" else CT)
            for (tok0, ln, xoff) in segs:
                for c0 in range(0, ln, 512):
                    n = min(512, ln - c0)
                    p = b.psum()
                    for kc in range(KC):
                        fw.pe(lambda e, p=p, kc=kc, Wc=Wc, a0=tok0 + c0, n=n: e.matmul(p[:, 0:n], lhsT=Wc[:, kc, :], rhs=b.xnT[:, kc, a0:a0 + n],
                                                                                     start=(kc == 0), stop=(kc == KC - 1)), r=[Wc, b.xnT], w=[p])
                    fw.act(lambda e, p=p, x0=xoff + c0, n=n: e.copy(out=xrow[:, x0:x0 + n], in_=p[:, 0:n]), r=[p], w=[xrow])
                fw.dve(lambda e, tok0=tok0, ln=ln, xoff=xoff: e.tensor_scalar(out=acc[:, tok0:tok0 + ln], in0=xrow[:, xoff - 2:xoff - 2 + ln],
                                                                             scalar1=cw[:, 0:1], scalar2=None, op0=ALU.mult),
                       r=[xrow, cw], w=[acc])
                for j in range(1, 5):
                    fw.dve(lambda e, j=j, tok0=tok0, ln=ln, xoff=xoff: e.scalar_tensor_tensor(
                        out=acc[:, tok0:tok0 + ln], in0=xrow[:, xoff - 2 + j:xoff - 2 + j + ln], scalar=cw[:, j:j + 1],
                        in1=acc[:, tok0:tok0 + ln], op0=ALU.mult, op1=ALU.add), r=[xrow, cw, acc], w=[acc])
                fw.act(lambda e, tok0=tok0, ln=ln, dst=dst: e.activation(out=dst[:, tok0:tok0 + ln], in_=acc[:, tok0:tok0 + ln], func=AF.Silu,
                                                                        bias=cw[:, 5:6], scale=1.0), r=[acc, cw], w=[dst])
            if kind == "xs":
                for t in all_tiles:
                    transposes(b, T(conv_o[:, t * 128:(t + 1) * 128], conv_o.name), 1,
                               lambda t=t, cc=cc: xs_tok[:, t:t + 1, cc * 128:(cc + 1) * 128], [conv_o], [(xs_tok, t)])
            elif kind == "B":
                for t in all_tiles:
                    transposes(b, T(BT[:, t * 128:(t + 1) * 128], BT.name), 1, lambda t=t: B_tok[:, t:t + 1, :], [BT], [(B_tok, t)])
    with b.scope():
        Wz = load_w(b, "sm_wz", w_in[:, g * 512:(g + 1) * 512], KC, 512)
        S = b.sb("sm_S", [128, 512], F32)
        S_bf = b.sb("sm_Sbf", [128, 512], BF16)
        CBm = b.sb("sm_CBm", [128, 128], F32)
        aU = b.sb("sm_aU", [128, 8, 128], F32)
        Ex = b.sb("sm_Ex", [128, 8, 128], BF16)
        MT = b.sb("sm_MT", [128, 8, 128], BF16)
        xdt = b.sb("sm_xdt", [128, 512], BF16)
        ytmp = b.sb("sm_ytmp", [128, 512], F32)
        ygb = b.sb("sm_ygb", [128, 512], BF16)
        ct2 = [b.sb("sm_ct%d" % j, [128, 24], F32) for j in range(2)]
        ex2 = [b.sb("sm_ex%d" % j, [128, 24], F32) for j in range(2)]
        xdtd2 = [b.sb("sm_xdtd%d" % j, [128, 512], BF16) for j in range(2)]
        ydg2 = [b.sb("sm_ydg%d" % j, [128, 512], F32) for j in range(2)]
        zs2 = [b.sb("sm_zs%d" % j, [128, 512], BF16) for j in range(2)]
        yfl2 = [b.sb("sm_yfl%d" % j, [128, 512], F32) for j in range(2)]
        ctx_t = list(range(NTC))
        lat_t = list(range(NTC, NT))
        v8 = lambda ap: ap.rearrange("p (e q) -> p e q", e=8)

        def stageA(d, t, k):
            iU_r, iU_l = (0, 1) if d == 0 else (2, 3)
            c0 = d * 32 + g * 8
            tsl = slice(t * 128, (t + 1) * 128)
            a8 = a_all[:, t, c0:c0 + 8]
            dt8 = dt_all[:, t, c0:c0 + 8]
            ct, ex, xdtd, ydg, zs, yfl = ct2[k], ex2[k], xdtd2[k], ydg2[k], zs2[k], yfl2[k]
            pct = b.psum()
            fw.pe(lambda e: e.matmul(pct[:, 0:8], lhsT=b.tri[:, iU_r, :], rhs=a8, start=True, stop=True), r=[b.tri, (a_all, t)], w=[pct])
            fw.pe(lambda e: e.matmul(pct[:, 8:16], lhsT=b.ones_f[:, :], rhs=a8, start=True, stop=True), r=[b.ones_f, (a_all, t)], w=[pct])
            fw.act(lambda e: e.copy(out=ct[:, 0:16], in_=pct[:, 0:16]), r=[pct], w=[ct])
            fw.dve(lambda e: e.tensor_tensor(out=ct[:, 16:24], in0=ct[:, 8:16], in1=ct[:, 0:8], op=ALU.subtract), r=[ct], w=[ct])
            fw.act(lambda e: e.activation(out=ex[:], in_=ct[:], func=AF.Exp), r=[ct], w=[ex])
            pcb = b.psum()
            fw.pe(lambda e: e.matmul(pcb[:, 0:128], lhsT=BT[:, tsl], rhs=CT[:, tsl], start=True, stop=True), r=[BT, CT], w=[pcb])
            fw.dve(lambda e: e.tensor_tensor(out=CBm[:], in0=pcb[:, 0:128], in1=b.tri[:, iU_r, :], op=ALU.mult), r=[pcb, b.tri], w=[CBm])
            fw.pool(lambda e: e.tensor_tensor(out=aU[:], in0=a8.unsqueeze(2).to_broadcast([128, 8, 128]),
                                              in1=b.tri[:, iU_r, :].unsqueeze(1).to_broadcast([128, 8, 128]), op=ALU.mult),
                    r=[(a_all, t), b.tri], w=[aU])
            for hf in range(2):
                pd = b.psum()
                fw.pe(lambda e, pd=pd, hf=hf: e.matmul(pd[:, :], lhsT=b.tri[:, iU_l, :],
                                                      rhs=aU[:, hf * 4:(hf + 1) * 4, :].rearrange("p e t -> p (e t)"), start=True, stop=True),
                      r=[b.tri, aU], w=[pd])
                fw.act(lambda e, pd=pd, hf=hf: e.activation(out=Ex[:, hf * 4:(hf + 1) * 4, :].rearrange("p e t -> p (e t)"), in_=pd[:, :], func=AF.Exp),
                       r=[pd], w=[(Ex, hf)])
            fw.dve(lambda e: e.tensor_tensor(out=MT[:], in0=Ex[:], in1=CBm[:].unsqueeze(1).to_broadcast([128, 8, 128]), op=ALU.mult),
                   r=[Ex, CBm], w=[MT])
            fw.pool(lambda e: e.tensor_tensor(out=v8(xdt[:]), in0=v8(xs_tok[:, t, :]), in1=dt8.unsqueeze(2).to_broadcast([128, 8, 64]), op=ALU.mult),
                    r=[(xs_tok, t), (dt_all, t)], w=[xdt])
            fw.pool(lambda e: e.tensor_tensor(out=v8(xdtd[:]), in0=v8(xdt[:]), in1=ex[:, 16:24].unsqueeze(2).to_broadcast([128, 8, 64]), op=ALU.mult),
                    r=[xdt, ex], w=[xdtd])
            pyd = b.psum()
            for e8 in range(8):
                fw.pe(lambda e, e8=e8: e.matmul(pyd[:, e8 * 64:(e8 + 1) * 64], lhsT=MT[:, e8, :], rhs=xdt[:, e8 * 64:(e8 + 1) * 64],
                                                start=True, stop=True), r=[MT, xdt], w=[pyd])
            if d == 0:
                fw.act(lambda e: e.copy(out=ydg[:], in_=pyd[:, :]), r=[pyd], w=[ydg])
            else:
                fw.dma("sync", yfl[:], yf_d[tsl, :], r=[("yf_d", t)], w=[yfl])
                fw.dve(lambda e: e.tensor_tensor(out=ydg[:], in0=pyd[:, :], in1=yfl[:], op=ALU.add), r=[pyd, yfl], w=[ydg])
                fw.pool(lambda e: e.tensor_tensor(out=v8(yfl[:]), in0=v8(xs_tok[:, t, :]),
                                                  in1=dskB[:, g * 8:(g + 1) * 8].unsqueeze(2).to_broadcast([128, 8, 64]), op=ALU.mult),
                        r=[(xs_tok, t), dskB, yfl, ydg], w=[yfl])
                fw.pool(lambda e: e.tensor_tensor(out=ydg[:], in0=ydg[:], in1=yfl[:], op=ALU.add), r=[ydg, yfl], w=[ydg])
                pz = b.psum()
                lin_tok(b, b.xnT, t, Wz, 0, 512, pz)
                fw.act(lambda e: e.activation(out=zs[:], in_=pz[:, :], func=AF.Silu), r=[pz], w=[zs])

        def stageB(d, t, k):
            tsl = slice(t * 128, (t + 1) * 128)
            ex, xdtd, ydg, zs = ex2[k], xdtd2[k], ydg2[k], zs2[k]
            pyo = b.psum()
            fw.pe(lambda e: e.matmul(pyo[:, :], lhsT=CT[:, tsl], rhs=S_bf[:, :], start=True, stop=True), r=[CT, S_bf], w=[pyo])
            psn = b.psum()
            fw.pe(lambda e: e.matmul(psn[:, :], lhsT=B_tok[:, t, :], rhs=xdtd[:, :], start=True, stop=True), r=[(B_tok, t), xdtd], w=[psn])
            fw.dve(lambda e: e.tensor_tensor(out=v8(ytmp[:]), in0=v8(pyo[:, :]), in1=ex[:, 0:8].unsqueeze(2).to_broadcast([128, 8, 64]), op=ALU.mult),
                   r=[pyo, ex], w=[ytmp])
            fw.pool(lambda e: e.tensor_tensor(out=v8(S[:]), in0=v8(S[:]), in1=ex[:, 8:16].unsqueeze(2).to_broadcast([128, 8, 64]), op=ALU.mult),
                    r=[S, ex], w=[S])
            fw.dve(lambda e: e.tensor_tensor(out=S[:], in0=psn[:, :], in1=S[:], op=ALU.add), r=[psn, S], w=[S])
            fw.act(lambda e: e.copy(out=S_bf[:], in_=S[:]), r=[S], w=[S_bf])
            fw.dve(lambda e: e.tensor_tensor(out=ydg[:], in0=ydg[:], in1=ytmp[:], op=ALU.add), r=[ydg, ytmp], w=[ydg])
            if d == 0:
                fw.dma("sync", yf_d[tsl, :], ydg[:], r=[ydg], w=[("yf_d", t)])
            else:
                fw.dve(lambda e: e.tensor_tensor(out=ydg[:], in0=ydg[:], in1=zs[:], op=ALU.mult), r=[ydg, zs], w=[ydg])
                fw.act(lambda e: e.activation(out=ygb[:], in_=ydg[:], func=AF.Square, accum_out=ssqg[:, t, g:g + 1]),
                       r=[ydg], w=[ygb, (ssqg, t)])
                fw.dve(lambda e: e.tensor_copy(out=ygb[:], in_=ydg[:]), r=[ydg], w=[ygb])
                fw.dma("sync", yg_d[tsl, g * 512:(g + 1) * 512], ygb[:], r=[ygb], w=[("yg_d", t)])

        for d in range(2):
            order = (ctx_t + lat_t) if d == 0 else (ctx_t[::-1] + lat_t[::-1])
            fw.pool(lambda e: e.memset(S[:], 0.0), w=[S])
            fw.pool(lambda e: e.memset(S_bf[:], 0.0), w=[S_bf])
            stageA(d, order[0], 0)
            for i, t in enumerate(order):
                if i + 1 < len(order):
                    stageA(d, order[i + 1], (i + 1) % 2)
                stageB(d, t, i % 2)


MIXERS[2] = mamba


def _host_consts(cfg):
    ident = np.eye(128, dtype=np.float32)
    k = np.arange(128)[:, None]
    t = np.arange(128)[None, :]
    tri = np.stack([(k <= t), (k > t), (k >= t), (k < t)]).astype(np.float32)
    out = {"c_ident": ident, "c_tri": tri}
    out.update(host_tables(cfg))
    return out


def kernel(**inputs):
    from concourse.bass_utils import run_bass_kernel_spmd
    inp = {k: np.ascontiguousarray(np.asarray(v)) for k, v in inputs.items()}
    n_cores = 8
    NB = inp["x"].shape[0] // n_cores
    cfg = Cfg(S_LAT=inp["x"].shape[1], S_CTX=inp["ctx"].shape[1], NB=NB, kinds=[0, 1, 2, 3])
    wnames = [k for k in inp if k not in ("x", "c", "ctx", "c_ctx")]
    cfg.wshapes = {k: inp[k].shape for k in wnames}
    nc, b = build_program(cfg)
    consts = _host_consts(cfg)
    maps = []
    for core in range(n_cores):
        m = {k: inp[k] for k in wnames if k in b.dram}
        m.update({k: v for k, v in consts.items() if k in b.dram})
        m["x"] = inp["x"][core * NB:(core + 1) * NB]
        m["ctx"] = inp["ctx"][core * NB:(core + 1) * NB]
        m["c"] = inp["c"][core * NB:(core + 1) * NB]
        m["c_ctx"] = inp["c_ctx"][None, :]
        maps.append(m)
    res = run_bass_kernel_spmd(nc, maps, core_ids=list(range(n_cores)))
    return np.concatenate([r["out"] for r in res.results], axis=0).astype(np.float32)
```
